# Optimizing a Trainium2 kernel written in Bass

```python
import math
import jax
import jax.numpy as jnp
from jax import lax
import numpy as np

D_MODEL = 1024
BATCH = 1
SEQ = 16384
DEPTH = 4

GRID_W = 64
CTX_LEN = 256
Q_BLOCK = 128
ROPE_BASE = 10000.0
NORM_EPS = 1e-6

W_DIFF = 256
W_SSD = 256
W_S5 = 256
W_GQA = 256
D_MIX = W_DIFF + W_SSD + W_S5 + W_GQA

DIFF_HEADS = 4
DIFF_QK_DIM = 32
DIFF_V_DIM = 2 * DIFF_QK_DIM

SSD_HEAD_DIM = 64
SSD_HEADS = W_SSD // SSD_HEAD_DIM
SSD_GROUPS = 2
SSD_STATE = 128
SSD_CONV = 3
SSD_CHUNK = 128
SSD_XBC = W_SSD + 2 * SSD_GROUPS * SSD_STATE

S5_GROUP = 16
S5_GROUPS = W_S5 // S5_GROUP
S5_STATE = 64

GQA_HEADS = 4
GQA_KV_HEADS = 2
GQA_REP = GQA_HEADS // GQA_KV_HEADS
GQA_HEAD_DIM = W_GQA // GQA_HEADS

N_DIFF_IN = 3 * W_DIFF
N_SSD_IN = W_SSD + SSD_XBC + 2 * SSD_HEADS
N_S5_IN = W_S5
N_GQA_IN = (GQA_HEADS + 2 * GQA_KV_HEADS) * GQA_HEAD_DIM
D_IN = N_DIFF_IN + N_SSD_IN + N_S5_IN + N_GQA_IN

N_EXPERTS = 32
TOP_K = 4
D_EXPERT = D_MODEL
SWIGLU_LIMIT = 7.0
SWIGLU_ALPHA = 1.702
MOE_BLOCK = 128

kernel_name = 'hybrid_parallel_heads_diffusion_moe'


def rms_norm(x, w):
    xf = x.astype(jnp.float32)
    y = xf * lax.rsqrt(jnp.mean(xf * xf, axis=-1, keepdims=True) + NORM_EPS)
    return (y * w.astype(jnp.float32)).astype(x.dtype)


def modulate(h, shift, scale):
    return h * (1.0 + scale) + shift


def axial_rope_tables(row, col, head_dim):
    axis_dim = head_dim // 2
    inv_freq = ROPE_BASE ** (-jnp.arange(0, axis_dim, 2, dtype=jnp.float32) / axis_dim)
    ang_r = row.astype(jnp.float32)[:, None] * inv_freq
    ang_c = col.astype(jnp.float32)[:, None] * inv_freq
    return (jnp.cos(ang_r), jnp.sin(ang_r), jnp.cos(ang_c), jnp.sin(ang_c))


def _rotate_half(x, cos, sin):
    x1, x2 = jnp.split(x.astype(jnp.float32), 2, axis=-1)
    return jnp.concatenate([x1 * cos - x2 * sin, x1 * sin + x2 * cos], axis=-1)


def apply_axial_rope(x, rope):
    cos_r, sin_r, cos_c, sin_c = rope

    def expand(t):
        return t.reshape((1, t.shape[0]) + (1,) * (x.ndim - 3) + (t.shape[1],))

    x_row, x_col = jnp.split(x, 2, axis=-1)
    out = jnp.concatenate([_rotate_half(x_row, expand(cos_r), expand(sin_r)),
                           _rotate_half(x_col, expand(cos_c), expand(sin_c))], axis=-1)
    return out.astype(x.dtype)


def sweep_query_blocks(fn, q):
    b, t = q.shape[:2]
    nb = t // Q_BLOCK
    qb = jnp.moveaxis(q.reshape((b, nb, Q_BLOCK) + q.shape[2:]), 1, 0)
    ob = lax.map(fn, qb)
    return jnp.moveaxis(ob, 0, 1).reshape((b, t) + ob.shape[3:])


def diff_attention_mixer(uc, ul, rope, lq1, lk1, lq2, lk2, subln_w, lam_init, with_ctx):
    f32 = jnp.float32
    lam = (jnp.exp(jnp.sum(lq1.astype(f32) * lk1.astype(f32)))
           - jnp.exp(jnp.sum(lq2.astype(f32) * lk2.astype(f32))) + lam_init)
    scale = DIFF_QK_DIM ** -0.5

    def split_heads(u):
        b, t = u.shape[:2]
        q, k, v = jnp.split(u, 3, axis=-1)
        shp = (b, t, DIFF_HEADS, 2, DIFF_QK_DIM)
        return q.reshape(shp), k.reshape(shp), v.reshape(b, t, DIFF_HEADS, DIFF_V_DIM)

    qc, kc, vc = split_heads(uc)
    ql, kl, vl = split_heads(ul)
    ql = apply_axial_rope(ql, rope)
    kl = apply_axial_rope(kl, rope)
    k_all = jnp.concatenate([kc, kl], axis=1)
    v_all = jnp.concatenate([vc, vl], axis=1)

    def attend(q, k, v):
        s = jnp.einsum('bqhmd,bkhmd->bhmqk', q, k).astype(f32) * scale
        p = jax.nn.softmax(s, axis=-1)
        a = p[:, :, 0] - lam * p[:, :, 1]
        o = jnp.einsum('bhqk,bkhe->bqhe', a.astype(v.dtype), v)
        o = rms_norm(o, subln_w) * (1.0 - lam_init)
        return o.reshape(o.shape[:2] + (W_DIFF,))

    lat = sweep_query_blocks(lambda q: attend(q, k_all, v_all), ql)
    ctx_out = attend(qc, kc, vc) if with_ctx else None
    return ctx_out, lat


def depthwise_conv(x, w, bias):
    width = w.shape[0]
    y = lax.conv_general_dilated(x, w[:, None, :].astype(x.dtype), window_strides=(1,),
                                 padding=[(width // 2, width // 2)],
                                 dimension_numbers=('NWC', 'WIO', 'NWC'),
                                 feature_group_count=x.shape[-1])
    return y + bias.astype(x.dtype)


def segsum(x):
    t = x.shape[-1]
    cs = jnp.cumsum(x, axis=-1)
    diff = cs[..., :, None] - cs[..., None, :]
    return jnp.where(jnp.tril(jnp.ones((t, t), dtype=bool)), diff, -jnp.inf)


def ssd_scan(x, dt, a, bm, cm, init_state):
    b, t, h, p = x.shape
    nc = t // SSD_CHUNK
    rep = h // bm.shape[2]
    bm = jnp.repeat(bm, rep, axis=2)
    cm = jnp.repeat(cm, rep, axis=2)

    def chunk(v):
        return v.reshape((b, nc, SSD_CHUNK) + v.shape[2:])

    xdt = chunk(x * dt[..., None])
    bm, cm = chunk(bm), chunk(cm)
    adt = jnp.moveaxis(chunk(dt * a), -1, 1)
    a_cum = jnp.cumsum(adt, axis=-1)
    l_mat = jnp.exp(segsum(adt))
    y_diag = jnp.einsum('bclhn,bcshn,bhcls,bcshp->bclhp', cm, bm, l_mat, xdt)
    decay_states = jnp.exp(a_cum[..., -1:] - a_cum)
    states = jnp.einsum('bclhn,bhcl,bclhp->bchpn', bm, decay_states, xdt)
    states = jnp.concatenate([init_state[:, None], states], axis=1)
    decay_chunk = jnp.exp(segsum(jnp.pad(a_cum[..., -1], ((0, 0), (0, 0), (1, 0)))))
    new_states = jnp.einsum('bhzc,bchpn->bzhpn', decay_chunk, states)
    y_off = jnp.einsum('bclhn,bchpn,bhcl->bclhp', cm, new_states[:, :-1], jnp.exp(a_cum))
    return (y_diag + y_off).reshape(b, t, h, p), new_states[:, -1]


def ssd_mixer(uc, ul, conv_w, conv_b, dt_bias, a_log, d_skip, norm_w, with_ctx):
    f32 = jnp.float32
    a = -jnp.exp(a_log.astype(f32))
    gn = SSD_GROUPS * SSD_STATE

    def prep(u):
        b, t = u.shape[:2]
        z = u[..., :W_SSD]
        xbc = jax.nn.silu(depthwise_conv(u[..., W_SSD:W_SSD + SSD_XBC], conv_w, conv_b)).astype(f32)
        xs = xbc[..., :W_SSD].reshape(b, t, SSD_HEADS, SSD_HEAD_DIM)
        bm = xbc[..., W_SSD:W_SSD + gn].reshape(b, t, SSD_GROUPS, SSD_STATE)
        cm = xbc[..., W_SSD + gn:].reshape(b, t, SSD_GROUPS, SSD_STATE)
        dt = jax.nn.softplus(u[..., W_SSD + SSD_XBC:].astype(f32).reshape(b, t, 2, SSD_HEADS)
                             + dt_bias.astype(f32))
        return z, xs, bm, cm, dt

    def flip(v):
        return jnp.flip(v, axis=1)

    def bidir(xs, bm, cm, dt, s_f, s_b):
        y_f, fin_f = ssd_scan(xs, dt[:, :, 0], a[0], bm, cm, s_f)
        y_b, fin_b = ssd_scan(flip(xs), flip(dt[:, :, 1]), a[1], flip(bm), flip(cm), s_b)
        return y_f + flip(y_b), fin_f, fin_b

    def finish(y, xs, z):
        y = y + d_skip.astype(f32)[:, None] * xs
        y = y.reshape(y.shape[:2] + (W_SSD,))
        return rms_norm(y * jax.nn.silu(z.astype(f32)), norm_w).astype(z.dtype)

    zc, xc, bc, cc, dtc = prep(uc)
    zl, xl, bl, cl, dtl = prep(ul)
    zero = jnp.zeros((uc.shape[0], SSD_HEADS, SSD_HEAD_DIM, SSD_STATE), f32)
    yc, sc_f, sc_b = bidir(xc, bc, cc, dtc, zero, zero)
    yl, _, _ = bidir(xl, bl, cl, dtl, sc_f, sc_b)
    lat = finish(yl, xl, zl)
    ctx_out = finish(yc, xc, zc) if with_ctx else None
    return ctx_out, lat


def _linear_recurrence(bu, a, h0, reverse):
    edge = -1 if reverse else 0
    bu = bu.at[:, edge].add(a * h0)
    a_seq = jnp.broadcast_to(a, bu.shape)

    def combine(e1, e2):
        a1, b1 = e1
        a2, b2 = e2
        return a1 * a2, a2 * b1 + b2

    _, h = lax.associative_scan(combine, (a_seq, bu), reverse=reverse, axis=1)
    return h


def s5_mixer(uc, ul, lam_re, lam_im, log_dt, b_re, b_im, c_re, c_im, d_skip, w_glu, b_glu, with_ctx):
    f32 = jnp.float32
    lam = lax.complex(lam_re.astype(f32), lam_im.astype(f32))
    step = jnp.exp(log_dt.astype(f32))[..., None]
    a_bar = jnp.exp(lam * step)
    b_mat = lax.complex(b_re.astype(f32), b_im.astype(f32))
    b_bar = ((a_bar - 1.0) / lam)[..., None] * b_mat[None]
    c_mat = lax.complex(c_re.astype(f32), c_im.astype(f32))
    d_grp = d_skip.astype(f32).reshape(S5_GROUPS, S5_GROUP)

    def states(u, h0_f, h0_b):
        b, t = u.shape[:2]
        ug = u.astype(f32).reshape(b, t, S5_GROUPS, S5_GROUP)
        ucx = ug.astype(jnp.complex64)
        bu_f = jnp.einsum('gpc,btgc->btgp', b_bar[0], ucx)
        bu_b = jnp.einsum('gpc,btgc->btgp', b_bar[1], ucx)
        h_f = _linear_recurrence(bu_f, a_bar[0], h0_f, reverse=False)
        h_b = _linear_recurrence(bu_b, a_bar[1], h0_b, reverse=True)
        return ug, h_f, h_b

    def readout(ug, h_f, h_b, dtype):
        y = (jnp.real(jnp.einsum('gcp,btgp->btgc', c_mat[0], h_f))
             + jnp.real(jnp.einsum('gcp,btgp->btgc', c_mat[1], h_b)) + d_grp * ug)
        y = jax.nn.gelu(y.reshape(y.shape[:2] + (W_S5,)))
        return (y * jax.nn.sigmoid(y @ w_glu.astype(f32) + b_glu.astype(f32))).astype(dtype)

    zero = jnp.zeros((uc.shape[0], S5_GROUPS, S5_STATE), jnp.complex64)
    ugc, hc_f, hc_b = states(uc, zero, zero)
    ugl, hl_f, hl_b = states(ul, hc_f[:, -1], hc_b[:, 0])
    lat = readout(ugl, hl_f, hl_b, ul.dtype)
    ctx_out = readout(ugc, hc_f, hc_b, uc.dtype) if with_ctx else None
    return ctx_out, lat


def gqa_mixer(uc, ul, rope, q_norm_w, k_norm_w, with_ctx):
    nq = GQA_HEADS * GQA_HEAD_DIM
    nkv = GQA_KV_HEADS * GQA_HEAD_DIM
    scale = GQA_HEAD_DIM ** -0.5

    def split_heads(u):
        b, t = u.shape[:2]
        q = u[..., :nq].reshape(b, t, GQA_KV_HEADS, GQA_REP, GQA_HEAD_DIM)
        k = u[..., nq:nq + nkv].reshape(b, t, GQA_KV_HEADS, GQA_HEAD_DIM)
        v = u[..., nq + nkv:].reshape(b, t, GQA_KV_HEADS, GQA_HEAD_DIM)
        return rms_norm(q, q_norm_w), rms_norm(k, k_norm_w), v

    qc, kc, vc = split_heads(uc)
    ql, kl, vl = split_heads(ul)
    ql = apply_axial_rope(ql, rope)
    kl = apply_axial_rope(kl, rope)
    k_all = jnp.concatenate([kc, kl], axis=1)
    v_all = jnp.concatenate([vc, vl], axis=1)

    def attend(q, k, v):
        s = jnp.einsum('bqgrd,bkgd->bgrqk', q, k).astype(jnp.float32) * scale
        p = jax.nn.softmax(s, axis=-1)
        o = jnp.einsum('bgrqk,bkgd->bqgrd', p.astype(v.dtype), v)
        return o.reshape(o.shape[:2] + (W_GQA,))

    lat = sweep_query_blocks(lambda q: attend(q, k_all, v_all), ql)
    ctx_out = attend(qc, kc, vc) if with_ctx else None
    return ctx_out, lat


def moe_ffn(h, w_router, b_router, w_gate_up, b_gate_up, w_down, b_down):
    n, d = h.shape
    logits = (h @ w_router).astype(jnp.float32) + b_router.astype(jnp.float32)
    top_val, top_idx = lax.top_k(logits, TOP_K)
    gates = jax.nn.softmax(top_val, axis=-1)
    m = n * TOP_K
    expert = top_idx.reshape(-1)
    token = jnp.repeat(jnp.arange(n, dtype=jnp.int32), TOP_K)
    gate = gates.reshape(-1)
    order = jnp.argsort(expert)
    e_sorted = expert[order]
    counts = jnp.bincount(expert, length=N_EXPERTS)
    padded = (counts + MOE_BLOCK - 1) // MOE_BLOCK * MOE_BLOCK
    start = jnp.cumsum(counts) - counts
    pend = jnp.cumsum(padded)
    pstart = pend - padded
    dest = pstart[e_sorted] + jnp.arange(m, dtype=jnp.int32) - start[e_sorted]
    n_blocks = -(-(m + N_EXPERTS * (MOE_BLOCK - 1)) // MOE_BLOCK)
    cap = n_blocks * MOE_BLOCK
    slot_token = jnp.zeros((cap,), jnp.int32).at[dest].set(token[order])
    slot_gate = jnp.zeros((cap,), jnp.float32).at[dest].set(gate[order])
    block_expert = jnp.minimum(
        jnp.searchsorted(pend, jnp.arange(n_blocks, dtype=jnp.int32) * MOE_BLOCK, side='right'),
        N_EXPERTS - 1)
    xb = h[slot_token].reshape(n_blocks, MOE_BLOCK, d)

    def expert_block(args):
        xblk, e = args
        gu = xblk @ w_gate_up[e] + b_gate_up[e]
        g, u = gu[..., ::2], gu[..., 1::2]
        g = jnp.minimum(g, SWIGLU_LIMIT)
        u = jnp.clip(u, -SWIGLU_LIMIT, SWIGLU_LIMIT)
        act = g * jax.nn.sigmoid(SWIGLU_ALPHA * g) * (u + 1.0)
        return act @ w_down[e] + b_down[e]

    yb = lax.map(expert_block, (xb, block_expert))
    contrib = yb.reshape(cap, d) * slot_gate[:, None].astype(yb.dtype)
    return jnp.zeros_like(h).at[slot_token].add(contrib.astype(h.dtype))


def trunk_layer(xc, xl, c, c_ctx, rope_diff, rope_gqa, p, layer_idx, with_ctx):
    mod_l = (jax.nn.silu(c) @ p['w_ada'] + p['b_ada'])[:, None, :]
    mod_c = (jax.nn.silu(c_ctx) @ p['w_ada'] + p['b_ada'])[None, None, :]
    sh1_l, sc1_l, g1_l, sh2_l, sc2_l, g2_l = jnp.split(mod_l, 6, axis=-1)
    sh1_c, sc1_c, g1_c, sh2_c, sc2_c, g2_c = jnp.split(mod_c, 6, axis=-1)

    uc = modulate(rms_norm(xc, p['norm1_w']), sh1_c, sc1_c) @ p['w_in']
    ul = modulate(rms_norm(xl, p['norm1_w']), sh1_l, sc1_l) @ p['w_in']
    cuts = [N_DIFF_IN, N_DIFF_IN + N_SSD_IN, N_DIFF_IN + N_SSD_IN + N_S5_IN]
    uc_a, uc_b, uc_c, uc_d = jnp.split(uc, cuts, axis=-1)
    ul_a, ul_b, ul_c, ul_d = jnp.split(ul, cuts, axis=-1)

    lam_init = 0.8 - 0.6 * math.exp(-0.3 * layer_idx)
    ca, la = diff_attention_mixer(uc_a, ul_a, rope_diff, p['diff_lq1'], p['diff_lk1'], p['diff_lq2'],
                                  p['diff_lk2'], p['diff_subln_w'], lam_init, with_ctx)
    cb, lb = ssd_mixer(uc_b, ul_b, p['ssd_conv_w'], p['ssd_conv_b'], p['ssd_dt_bias'], p['ssd_a_log'],
                       p['ssd_d'], p['ssd_norm_w'], with_ctx)
    cc, lc = s5_mixer(uc_c, ul_c, p['s5_lam_re'], p['s5_lam_im'], p['s5_log_dt'], p['s5_b_re'], p['s5_b_im'],
                      p['s5_c_re'], p['s5_c_im'], p['s5_d'], p['s5_w_glu'], p['s5_b_glu'], with_ctx)
    cd, ld = gqa_mixer(uc_d, ul_d, rope_gqa, p['gqa_q_norm_w'], p['gqa_k_norm_w'], with_ctx)

    xl = xl + g1_l * (jnp.concatenate([la, lb, lc, ld], axis=-1) @ p['w_out'])
    hl = modulate(rms_norm(xl, p['norm2_w']), sh2_l, sc2_l)
    moe_args = (p['moe_w_router'], p['moe_b_router'], p['moe_w_gate_up'], p['moe_b_gate_up'],
                p['moe_w_down'], p['moe_b_down'])
    if not with_ctx:
        y = moe_ffn(hl.reshape(-1, hl.shape[-1]), *moe_args)
        return xc, xl + g2_l * y.reshape(xl.shape)
    xc = xc + g1_c * (jnp.concatenate([ca, cb, cc, cd], axis=-1) @ p['w_out'])
    hc = modulate(rms_norm(xc, p['norm2_w']), sh2_c, sc2_c)
    n_ctx = hc.shape[0] * hc.shape[1]
    y = moe_ffn(jnp.concatenate([hc.reshape(-1, hc.shape[-1]), hl.reshape(-1, hl.shape[-1])], axis=0),
                *moe_args)
    return xc + g2_c * y[:n_ctx].reshape(xc.shape), xl + g2_l * y[n_ctx:].reshape(xl.shape)


def setup_inputs(seed: int = 0) -> dict:
    key = jax.random.key(seed)
    keys = iter([jax.random.fold_in(key, i) for i in range(64)])
    f32 = jnp.float32
    L = DEPTH

    def normal(shape, std):
        return std * jax.random.normal(next(keys), shape, f32)

    def gain(shape):
        return 1.0 + normal(shape, 0.02)

    def log_uniform(shape, lo, hi):
        return jax.random.uniform(next(keys), shape, f32, math.log(lo), math.log(hi))

    ssd_dt = jnp.exp(log_uniform((L, 2, SSD_HEADS), 1e-3, 1e-1))
    ssd_a = jax.random.uniform(next(keys), (L, 2, SSD_HEADS), f32, 1.0, 16.0)
    s5_n = jnp.arange(S5_STATE, dtype=f32)
    return {
        'x': normal((BATCH, SEQ, D_MODEL), 1.0),
        'c': normal((BATCH, D_MODEL), 1.0),
        'ctx': normal((BATCH, CTX_LEN, D_MODEL), 1.0),
        'c_ctx': normal((D_MODEL,), 1.0),
        'w_ada': normal((L, D_MODEL, 6 * D_MODEL), 0.5 * D_MODEL ** -0.5),
        'b_ada': normal((L, 6 * D_MODEL), 0.02),
        'norm1_w': gain((L, D_MODEL)),
        'norm2_w': gain((L, D_MODEL)),
        'w_in': normal((L, D_MODEL, D_IN), D_MODEL ** -0.5),
        'w_out': normal((L, D_MIX, D_MODEL), D_MIX ** -0.5),
        'diff_lq1': normal((L, DIFF_QK_DIM), 0.1),
        'diff_lk1': normal((L, DIFF_QK_DIM), 0.1),
        'diff_lq2': normal((L, DIFF_QK_DIM), 0.1),
        'diff_lk2': normal((L, DIFF_QK_DIM), 0.1),
        'diff_subln_w': gain((L, DIFF_V_DIM)),
        'ssd_conv_w': normal((L, SSD_CONV, SSD_XBC), SSD_CONV ** -0.5),
        'ssd_conv_b': normal((L, SSD_XBC), 0.02),
        'ssd_dt_bias': ssd_dt + jnp.log(-jnp.expm1(-ssd_dt)),
        'ssd_a_log': jnp.log(ssd_a),
        'ssd_d': gain((L, SSD_HEADS)),
        'ssd_norm_w': gain((L, W_SSD)),
        's5_lam_re': -0.5 + normal((L, 2, S5_GROUPS, S5_STATE), 0.01),
        's5_lam_im': jnp.pi * s5_n + normal((L, 2, S5_GROUPS, S5_STATE), 0.01),
        's5_log_dt': log_uniform((L, 2, S5_GROUPS), 1e-3, 1e-1),
        's5_b_re': normal((L, S5_GROUPS, S5_STATE, S5_GROUP), (2 * S5_GROUP) ** -0.5),
        's5_b_im': normal((L, S5_GROUPS, S5_STATE, S5_GROUP), (2 * S5_GROUP) ** -0.5),
        's5_c_re': normal((L, 2, S5_GROUPS, S5_GROUP, S5_STATE), S5_STATE ** -0.5),
        's5_c_im': normal((L, 2, S5_GROUPS, S5_GROUP, S5_STATE), S5_STATE ** -0.5),
        's5_d': gain((L, W_S5)),
        's5_w_glu': normal((L, W_S5, W_S5), W_S5 ** -0.5),
        's5_b_glu': normal((L, W_S5), 0.02),
        'gqa_q_norm_w': gain((L, GQA_HEAD_DIM)),
        'gqa_k_norm_w': gain((L, GQA_HEAD_DIM)),
        'moe_w_router': normal((L, D_MODEL, N_EXPERTS), D_MODEL ** -0.5),
        'moe_b_router': normal((L, N_EXPERTS), 0.01),
        'moe_w_gate_up': normal((L, N_EXPERTS, D_MODEL, 2 * D_EXPERT), D_MODEL ** -0.5),
        'moe_b_gate_up': normal((L, N_EXPERTS, 2 * D_EXPERT), 0.01),
        'moe_w_down': normal((L, N_EXPERTS, D_EXPERT, D_MODEL), D_EXPERT ** -0.5),
        'moe_b_down': normal((L, N_EXPERTS, D_MODEL), 0.01),
        'final_norm_w': gain((D_MODEL,)),
    }


def reference(x, c, ctx, c_ctx, w_ada, b_ada, norm1_w, norm2_w, w_in, w_out,
              diff_lq1, diff_lk1, diff_lq2, diff_lk2, diff_subln_w,
              ssd_conv_w, ssd_conv_b, ssd_dt_bias, ssd_a_log, ssd_d, ssd_norm_w,
              s5_lam_re, s5_lam_im, s5_log_dt, s5_b_re, s5_b_im, s5_c_re, s5_c_im, s5_d, s5_w_glu, s5_b_glu,
              gqa_q_norm_w, gqa_k_norm_w,
              moe_w_router, moe_b_router, moe_w_gate_up, moe_b_gate_up, moe_w_down, moe_b_down,
              final_norm_w):
    seq = x.shape[1]
    rows = seq // GRID_W
    row = jnp.repeat(jnp.arange(rows, dtype=jnp.int32), GRID_W)
    col = jnp.tile(jnp.arange(GRID_W, dtype=jnp.int32), rows)
    rope_diff = axial_rope_tables(row, col, DIFF_QK_DIM)
    rope_gqa = axial_rope_tables(row, col, GQA_HEAD_DIM)
    xc, xl = ctx, x
    for i in range(DEPTH):
        p = {
            'w_ada': w_ada[i], 'b_ada': b_ada[i], 'norm1_w': norm1_w[i], 'norm2_w': norm2_w[i],
            'w_in': w_in[i], 'w_out': w_out[i],
            'diff_lq1': diff_lq1[i], 'diff_lk1': diff_lk1[i], 'diff_lq2': diff_lq2[i], 'diff_lk2': diff_lk2[i],
            'diff_subln_w': diff_subln_w[i],
            'ssd_conv_w': ssd_conv_w[i], 'ssd_conv_b': ssd_conv_b[i], 'ssd_dt_bias': ssd_dt_bias[i],
            'ssd_a_log': ssd_a_log[i], 'ssd_d': ssd_d[i], 'ssd_norm_w': ssd_norm_w[i],
            's5_lam_re': s5_lam_re[i], 's5_lam_im': s5_lam_im[i], 's5_log_dt': s5_log_dt[i],
            's5_b_re': s5_b_re[i], 's5_b_im': s5_b_im[i], 's5_c_re': s5_c_re[i], 's5_c_im': s5_c_im[i],
            's5_d': s5_d[i], 's5_w_glu': s5_w_glu[i], 's5_b_glu': s5_b_glu[i],
            'gqa_q_norm_w': gqa_q_norm_w[i], 'gqa_k_norm_w': gqa_k_norm_w[i],
            'moe_w_router': moe_w_router[i], 'moe_b_router': moe_b_router[i],
            'moe_w_gate_up': moe_w_gate_up[i], 'moe_b_gate_up': moe_b_gate_up[i],
            'moe_w_down': moe_w_down[i], 'moe_b_down': moe_b_down[i],
        }
        xc, xl = trunk_layer(xc, xl, c, c_ctx, rope_diff, rope_gqa, p, i, i < DEPTH - 1)
    return rms_norm(xl, final_norm_w)
```

```python
from contextlib import ExitStack
import math
import numpy as np
import ml_dtypes
import concourse.bass as bass
import concourse.mybir as mybir
from concourse.bass_utils import run_bass_kernel_spmd

F32 = mybir.dt.float32
BF16 = mybir.dt.bfloat16
U32 = mybir.dt.uint32
AF = mybir.ActivationFunctionType
ALU = mybir.AluOpType
AX = mybir.AxisListType

NCORES = 8
D = 1024
SEQ = 16384
CTX = 256
NTOK = SEQ + CTX
LAT_PC = SEQ // NCORES
CTX_PC = CTX // NCORES
TOK_PC = LAT_PC + CTX_PC
NT = 17
D_IN = 2568
EPS = 1e-6
DEPTH = 4


class _Buf:
    __slots__ = ("w", "r")

    def __init__(self):
        self.w = None
        self.r = {}


class Tile:
    def __init__(self, h, psum=False):
        self.h = h
        self.psum = psum
        self.bufs = {None: _Buf()}

    def __getitem__(self, idx):
        return self.h[idx]


class _Eng:
    def __init__(self, name, h, sem, sem_id):
        self.name, self.h, self.sem, self.sem_id = name, h, sem, sem_id
        self.count = 0
        self.seen = {}


class Prog:
    def __init__(self, nc, stack, n_dma_sems=6):
        self.nc = nc
        self.stack = stack
        self.sems = []
        self.engs = {}
        for name, h in (("pe", nc.tensor), ("act", nc.scalar), ("dve", nc.vector),
                        ("pool", nc.gpsimd), ("sp", nc.sync)):
            s = stack.enter_context(nc.semaphore("sem_" + name))
            self.sems.append(s)
            self.engs[name] = _Eng(name, h, s, len(self.sems) - 1)
        self.dma_sems = {}
        for q in ("sp", "pool", "act"):
            lst = []
            for i in range(n_dma_sems):
                s = stack.enter_context(nc.semaphore(f"dsem_{q}{i}"))
                self.sems.append(s)
                lst.append([len(self.sems) - 1, 0])
            self.dma_sems[q] = [lst, 0]
        self.out_events = []
        self.ntiles = 0

    def sb(self, shape, dtype, name=None):
        self.ntiles += 1
        h = self.stack.enter_context(self.nc.sbuf_tensor(name or f"t{self.ntiles}", list(shape), dtype))
        return Tile(h)

    def ps(self, shape, dtype=F32, name=None):
        self.ntiles += 1
        h = self.stack.enter_context(self.nc.psum_tensor(name or f"p{self.ntiles}", list(shape), dtype))
        return Tile(h, psum=True)

    @staticmethod
    def _split(ref):
        if isinstance(ref, Tile):
            return ref, None
        return ref

    def _check_bufs(self, ref):
        t, k = self._split(ref)
        if k is None:
            return list(t.bufs.values())
        b = t.bufs.get(k)
        if b is None:
            b = t.bufs[k] = _Buf()
        return [t.bufs[None], b]

    def _rec_buf(self, ref):
        t, k = self._split(ref)
        b = t.bufs.get(k)
        if b is None:
            b = t.bufs[k] = _Buf()
        return b

    def _collect(self, reads, writes):
        waits = {}

        def add(ev):
            if ev is None:
                return
            s, v = ev
            if waits.get(s, 0) < v:
                waits[s] = v
        for ref in reads:
            for b in self._check_bufs(ref):
                add(b.w)
        for ref in writes:
            for b in self._check_bufs(ref):
                add(b.w)
                for ev in b.r.items():
                    add(ev)
        return waits

    def _emit_waits(self, E, waits, skip_own=False):
        for s, v in waits.items():
            if E.seen.get(s, 0) >= v:
                continue
            if skip_own and s == E.sem_id:
                continue
            E.h.wait_ge(self.sems[s], v)
            E.seen[s] = v

    def _record(self, ev, reads, writes):
        s, v = ev
        for ref in reads:
            b = self._rec_buf(ref)
            if b.r.get(s, 0) < v:
                b.r[s] = v
        for ref in writes:
            b = self._rec_buf(ref)
            b.w = ev
            b.r = {}
            t, k = self._split(ref)
            if k is None:
                for kk, bb in t.bufs.items():
                    if kk is not None:
                        bb.w = ev
                        bb.r = {}

    def op(self, e, fn, reads=(), writes=()):
        E = self.engs[e]
        pr = [r for r in reads if self._split(r)[0].psum]
        if pr:
            reads = [r for r in reads if not self._split(r)[0].psum]
            writes = list(writes) + pr
        waits = self._collect(reads, writes)
        self._emit_waits(E, waits, skip_own=(e == "pe"))
        inst = fn(E.h)
        E.count += 1
        inst.then_inc(E.sem, 1)
        self._record((E.sem_id, E.count), reads, writes)
        return inst

    def dma(self, q, out, in_, reads=(), writes=(), is_output=False, **kw):
        E = self.engs[q]
        lst, rr = self.dma_sems[q]
        slot = lst[rr]
        self.dma_sems[q][1] = (rr + 1) % len(lst)
        waits = self._collect(reads, writes)
        if slot[1] > 0:
            if waits.get(slot[0], 0) < slot[1]:
                waits[slot[0]] = slot[1]
        self._emit_waits(E, waits)
        inst = E.h.dma_start(out=out, in_=in_, **kw)
        slot[1] += 16
        inst.then_inc(self.sems[slot[0]], 16)
        ev = (slot[0], slot[1])
        self._record(ev, reads, writes)
        if is_output:
            self.out_events.append(ev)
        return inst

    def finish(self, e="sp"):
        E = self.engs[e]
        waits = {}
        for s, v in self.out_events:
            if waits.get(s, 0) < v:
                waits[s] = v
        for q, (lst, _) in self.dma_sems.items():
            for sid, tot in lst:
                if tot > 0 and waits.get(sid, 0) < tot:
                    waits[sid] = tot
        for name, EE in self.engs.items():
            if EE.count > 0 and name != e:
                waits[EE.sem_id] = EE.count
        self._emit_waits(E, waits)


def _bf16(a):
    return np.asarray(a).astype(ml_dtypes.bfloat16)


def _new_nc():
    return bass.Bass("TRN2", target_bir_lowering=False)


def _din(nc, name, shape, dtype=F32):
    return nc.dram_tensor(name, list(shape), dtype, kind="ExternalInput").ap()


def _dout(nc, name, shape, dtype=F32):
    return nc.dram_tensor(name, list(shape), dtype, kind="ExternalOutput").ap()


def build_phase_m():
    nc = _new_nc()
    cT = _din(nc, "cT", [128, 8, 2])
    wada = _din(nc, "wada", [DEPTH, 1024, 768])
    bada = _din(nc, "bada", [DEPTH, 2, 768])
    mod = _dout(nc, "mod", [DEPTH, 2, 768])
    with ExitStack() as st:
        P = Prog(nc, st)
        ct = P.sb([128, 8, 2], F32)
        sc = P.sb([128, 8, 2], F32)
        P.dma("sp", ct[:, :, :], cT[:, :, :], writes=[ct])
        P.op("act", lambda a: a.activation(out=sc[:, :, :], in_=ct[:, :, :], func=AF.Sigmoid), reads=[ct], writes=[sc])
        P.op("dve", lambda v: v.tensor_tensor(out=sc[:, :, :], in0=sc[:, :, :], in1=ct[:, :, :], op=ALU.mult),
             reads=[sc, ct], writes=[sc])
        wt = [P.sb([128, 8, 768], F32) for _ in range(2)]
        bt = [P.sb([2, 768], F32) for _ in range(2)]
        ot = [P.sb([2, 768], F32) for _ in range(2)]
        pss = [P.ps([128, 512]) for _ in range(2)]
        for l in range(DEPTH):
            w = wt[l % 2]
            P.dma("sp", w[:, :, :], wada[l].rearrange("(k p) n -> p k n", p=128), writes=[w])
            P.dma("sp", bt[l % 2][:, :], bada[l], writes=[bt[l % 2]])
            for h in range(2):
                ps = pss[h]
                for k in range(8):
                    P.op("pe", lambda t, k=k, h=h, w=w, ps=ps: t.matmul(ps[0:2, 0:384], lhsT=sc[:, k, :], rhs=w[:, k, h * 384:(h + 1) * 384],
                                                                     start=(k == 0), stop=(k == 7)),
                         reads=[sc, w], writes=[ps])
                P.op("dve", lambda v, h=h, ps=ps, l=l: v.tensor_tensor(out=ot[l % 2][:, h * 384:(h + 1) * 384], in0=ps[0:2, 0:384],
                                                                      in1=bt[l % 2][:, h * 384:(h + 1) * 384], op=ALU.add),
                     reads=[ps, bt[l % 2]], writes=[ot[l % 2]])
            P.dma("sp", mod[l], ot[l % 2][:, :], reads=[ot[l % 2]], is_output=True)
        P.finish()
    return nc


C_DQ, C_DK, C_DV = 0, 256, 512
C_SZ, C_SX, C_SDT = 768, 1024, 1792
C_S5 = 1800
C_GQ, C_GK, C_GV = 2056, 2312, 2440


def _load_bcast(P, q, dst, src_ap, n):
    P.dma(q, dst[:, 0:n], src_ap.partition_broadcast(128), writes=[dst])


def _rms_rstd(P, ss, rstd, r, n, k=1):
    P.op("dve", lambda v: v.tensor_scalar(out=rstd[:r, 0:k], in0=ss[:r, 0:k], scalar1=1.0 / n, scalar2=EPS,
                                          op0=ALU.mult, op1=ALU.add), reads=[ss], writes=[rstd])
    P.op("act", lambda a: a.activation(out=rstd[:r, 0:k], in_=rstd[:r, 0:k], func=AF.Sqrt), reads=[rstd], writes=[rstd])
    P.op("dve", lambda v: v.reciprocal(out=rstd[:r, 0:k], in_=rstd[:r, 0:k]), reads=[rstd], writes=[rstd])


def _rope(P, eng, src, dst, tmp, c0, ngrp, hd, cos_ap, sin_ap, r):
    j = hd // 4
    n = ngrp * hd
    sv = src[:r, c0:c0 + n].rearrange("p (g a h j) -> p g a h j", g=ngrp, a=2, h=2, j=j)
    dv = dst[:r, c0:c0 + n].rearrange("p (g a h j) -> p g a h j", g=ngrp, a=2, h=2, j=j)
    t1 = tmp[:r, 0:n // 2].rearrange("p (g a j) -> p g a j", g=ngrp, a=2, j=j)
    t2 = tmp[:r, n // 2:n].rearrange("p (g a j) -> p g a j", g=ngrp, a=2, j=j)
    cb = cos_ap.unsqueeze(1).broadcast_to([r, ngrp, 2, j])
    sb_ = sin_ap.unsqueeze(1).broadcast_to([r, ngrp, 2, j])
    x0 = sv[:, :, :, 0, :]
    x1 = sv[:, :, :, 1, :]
    tt = lambda o, a, b, op: P.op(eng, lambda v: v.tensor_tensor(out=o, in0=a, in1=b, op=op),
                                  reads=[src, tmp], writes=[tmp, dst])
    tt(t1, x0, cb, ALU.mult)
    tt(t2, x1, sb_, ALU.mult)
    tt(dv[:, :, :, 0, :], t1, t2, ALU.subtract)
    tt(t1, x0, sb_, ALU.mult)
    tt(t2, x1, cb, ALU.mult)
    tt(dv[:, :, :, 1, :], t1, t2, ALU.add)


def build_phase_a():
    nc = _new_nc()
    x = _din(nc, "x", [TOK_PC, D])
    modl = _din(nc, "modl", [6 * D])
    modc = _din(nc, "modc", [6 * D])
    n1w = _din(nc, "n1w", [D])
    w_in = _din(nc, "w_in", [D, D_IN])
    ident = _din(nc, "ident", [128, 128], BF16)
    ropd = _din(nc, "ropd", [LAT_PC, 2, 16])
    ropg = _din(nc, "ropg", [LAT_PC, 2, 32])
    qkw = _din(nc, "qkw", [384])
    u_out = _dout(nc, "u", [TOK_PC, C_GQ - C_SZ])
    qkd_out = _dout(nc, "qkd", [TOK_PC, 512], BF16)
    qkg_out = _dout(nc, "qkg", [TOK_PC, 384], BF16)
    vv_out = _dout(nc, "vv", [TOK_PC, 384], BF16)
    with ExitStack() as st:
        P = Prog(nc, st)
        idt = P.sb([128, 128], BF16)
        P.dma("sp", idt[:, :], ident, writes=[idt])
        w1 = [P.sb([128, D], F32) for _ in range(2)]
        sh1 = [P.sb([128, D], F32) for _ in range(2)]
        n1 = P.sb([128, D], F32)
        _load_bcast(P, "sp", n1, n1w, D)
        for i, m in enumerate((modl, modc)):
            _load_bcast(P, "sp", sh1[i], m[0:D], D)
            _load_bcast(P, "sp", w1[i], m[D:2 * D], D)
            P.op("dve", lambda v, i=i: v.scalar_tensor_tensor(out=w1[i][:, :], in0=w1[i][:, :], scalar=1.0, in1=n1[:, :],
                                                              op0=ALU.add, op1=ALU.mult), reads=[w1[i], n1], writes=[w1[i]])
        qkwt = P.sb([128, 384], F32)
        _load_bcast(P, "sp", qkwt, qkw, 384)
        rd = P.sb([128, 16, 32], F32)
        rg = P.sb([128, 16, 64], F32)
        P.dma("sp", rd[:, :, :], ropd.rearrange("(t p) c j -> p t (c j)", p=128), writes=[rd])
        P.dma("sp", rg[:, :, :], ropg.rearrange("(t p) c j -> p t (c j)", p=128), writes=[rg])
        wbf = P.sb([128, 8, D_IN], BF16)
        stg = [P.sb([128, D_IN], F32) for _ in range(2)]
        for k in range(8):
            s = stg[k % 2]
            P.dma("sp", s[:, :], w_in[k * 128:(k + 1) * 128, :], writes=[s])
            P.op("pool", lambda g, k=k, s=s: g.tensor_copy(out=wbf[:, k, :], in_=s[:, :]), reads=[s], writes=[(wbf, k)])

        xt = [P.sb([128, D], F32) for _ in range(3)]
        junk = P.sb([128, D], F32)
        ss = [P.sb([128, 8], F32) for _ in range(2)]
        rstd = [P.sb([128, 8], F32) for _ in range(2)]
        hf = [P.sb([128, D], F32) for _ in range(2)]
        hb = [P.sb([128, D], BF16) for _ in range(2)]
        hT = [P.sb([128, 8, 128], BF16) for _ in range(2)]
        pT = [P.ps([128, 8, 128], BF16) for _ in range(2)]
        pu = [P.ps([128, 512]) for _ in range(4)]
        ut = [P.sb([128, D_IN], F32) for _ in range(2)]
        qd = [P.sb([128, 512], BF16) for _ in range(2)]
        qg = [P.sb([128, 384], BF16) for _ in range(2)]
        gn = [P.sb([128, 384], F32) for _ in range(2)]
        vvt = [P.sb([128, 384], BF16) for _ in range(2)]
        tmp = [P.sb([128, 512], F32) for _ in range(2)]
        coltiles = [(c, min(512, D_IN - c)) for c in range(0, D_IN, 512)]
        npu = 0
        for t in range(NT):
            r = 128 if t < 16 else CTX_PC
            ic = 0 if t < 16 else 1
            X = xt[t % 3]
            P.dma("sp", X[:r, :], x[t * 128:t * 128 + r, :], writes=[X])
            S, R = ss[t % 2], rstd[t % 2]
            P.op("act", lambda a, X=X, S=S, r=r: a.activation(out=junk[:r, :], in_=X[:r, :], func=AF.Square, accum_out=S[:r, 0:1]),
                 reads=[X], writes=[junk, S])
            _rms_rstd(P, S, R, r, D)
            H, HB = hf[t % 2], hb[t % 2]
            P.op("dve", lambda v, X=X, R=R, H=H, r=r, ic=ic: v.scalar_tensor_tensor(
                out=H[:r, :], in0=X[:r, :], scalar=R[:r, 0:1], in1=w1[ic][:r, :], op0=ALU.mult, op1=ALU.mult),
                reads=[X, R, w1[ic]], writes=[H])
            P.op("pool", lambda g, H=H, HB=HB, r=r, ic=ic: g.tensor_tensor(out=HB[:r, :], in0=H[:r, :], in1=sh1[ic][:r, :], op=ALU.add),
                 reads=[H, sh1[ic]], writes=[HB])
            PT, HT = pT[t % 2], hT[t % 2]
            for k in range(8):
                P.op("pe", lambda pe, k=k, HB=HB, PT=PT, r=r: pe.transpose(PT[:, k, :r], HB[:r, k * 128:(k + 1) * 128], idt[:r, :r]),
                     reads=[HB, idt], writes=[PT])
            P.op("act", lambda a, PT=PT, HT=HT, r=r: a.copy(out=HT[:, :, :r], in_=PT[:, :, :r]), reads=[PT], writes=[HT])
            U = ut[t % 2]
            for ci, (c0, cw) in enumerate(coltiles):
                ps = pu[npu % 4]
                npu += 1
                for k in range(8):
                    P.op("pe", lambda pe, k=k, ps=ps, HT=HT, r=r, c0=c0, cw=cw: pe.matmul(
                        ps[:r, :cw], lhsT=HT[:, k, :r], rhs=wbf[:, k, c0:c0 + cw], start=(k == 0), stop=(k == 7)),
                        reads=[HT, (wbf, k)], writes=[ps])
                e = "act" if ci % 2 == 0 else "dve"
                if e == "act":
                    P.op("act", lambda a, ps=ps, U=U, r=r, c0=c0, cw=cw: a.copy(out=U[:r, c0:c0 + cw], in_=ps[:r, :cw]),
                         reads=[ps], writes=[(U, ci)])
                else:
                    P.op("dve", lambda v, ps=ps, U=U, r=r, c0=c0, cw=cw: v.tensor_copy(out=U[:r, c0:c0 + cw], in_=ps[:r, :cw]),
                         reads=[ps], writes=[(U, ci)])
            P.dma("sp", u_out[t * 128:t * 128 + r, :], U[:r, C_SZ:C_GQ], reads=[U], is_output=True)
            VV = vvt[t % 2]
            P.op("pool", lambda g, U=U, VV=VV, r=r: g.tensor_copy(out=VV[:r, 0:256], in_=U[:r, C_DV:C_DV + 256]), reads=[U], writes=[VV])
            P.op("pool", lambda g, U=U, VV=VV, r=r: g.tensor_copy(out=VV[:r, 256:384], in_=U[:r, C_GV:C_GV + 128]), reads=[U], writes=[VV])
            P.dma("sp", vv_out[t * 128:t * 128 + r, :], VV[:r, :], reads=[VV], is_output=True)
            QD, QG, GN, TM = qd[t % 2], qg[t % 2], gn[t % 2], tmp[t % 2]
            if t < 16:
                cosd = rd[:r, t, 0:16].rearrange("p (a j) -> p a j", a=2)
                sind = rd[:r, t, 16:32].rearrange("p (a j) -> p a j", a=2)
                _rope(P, "dve", U, QD, TM, 0, 16, 32, cosd, sind, r)
            else:
                P.op("dve", lambda v, U=U, QD=QD, r=r: v.tensor_copy(out=QD[:r, :], in_=U[:r, 0:512]), reads=[U], writes=[QD])
            P.dma("sp", qkd_out[t * 128:t * 128 + r, :], QD[:r, :], reads=[QD], is_output=True)
            S2, R2 = ss[t % 2], rstd[t % 2]
            P.op("pool", lambda g, U=U, GN=GN, r=r: g.tensor_tensor(out=GN[:r, :], in0=U[:r, C_GQ:C_GQ + 384], in1=U[:r, C_GQ:C_GQ + 384],
                                                                  op=ALU.mult), reads=[U], writes=[GN])
            P.op("dve", lambda v, GN=GN, S2=S2, r=r: v.tensor_reduce(out=S2[:r, 1:7], in_=GN[:r, :].rearrange("p (g d) -> p g d", g=6),
                                                                    op=ALU.add, axis=AX.X), reads=[GN], writes=[S2])
            P.op("dve", lambda v, S2=S2, R2=R2, r=r: v.tensor_scalar(out=R2[:r, 1:7], in0=S2[:r, 1:7], scalar1=1.0 / 64, scalar2=EPS,
                                                                    op0=ALU.mult, op1=ALU.add), reads=[S2], writes=[R2])
            P.op("act", lambda a, R2=R2, r=r: a.activation(out=R2[:r, 1:7], in_=R2[:r, 1:7], func=AF.Sqrt), reads=[R2], writes=[R2])
            P.op("dve", lambda v, R2=R2, r=r: v.reciprocal(out=R2[:r, 1:7], in_=R2[:r, 1:7]), reads=[R2], writes=[R2])
            P.op("dve", lambda v, U=U, GN=GN, R2=R2, r=r: v.tensor_tensor(
                out=GN[:r, :].rearrange("p (g d) -> p g d", g=6), in0=U[:r, C_GQ:C_GQ + 384].rearrange("p (g d) -> p g d", g=6),
                in1=R2[:r, 1:7].unsqueeze(2).broadcast_to([r, 6, 64]), op=ALU.mult), reads=[U, R2], writes=[GN])
            if t < 16:
                P.op("pool", lambda g, GN=GN, r=r: g.tensor_tensor(out=GN[:r, :], in0=GN[:r, :], in1=qkwt[:r, :], op=ALU.mult),
                     reads=[GN, qkwt], writes=[GN])
                cosg = rg[:r, t, 0:32].rearrange("p (a j) -> p a j", a=2)
                sing = rg[:r, t, 32:64].rearrange("p (a j) -> p a j", a=2)
                _rope(P, "dve", GN, QG, TM, 0, 6, 64, cosg, sing, r)
            else:
                P.op("pool", lambda g, GN=GN, QG=QG, r=r: g.tensor_tensor(out=QG[:r, :], in0=GN[:r, :], in1=qkwt[:r, :], op=ALU.mult),
                     reads=[GN, qkwt], writes=[QG])
            P.dma("sp", qkg_out[t * 128:t * 128 + r, :], QG[:r, :], reads=[QG], is_output=True)
        P.finish()
    return nc


def rope_tables(head_dim):
    axis_dim = head_dim // 2
    inv = (10000.0 ** (-np.arange(0, axis_dim, 2, dtype=np.float32) / axis_dim)).astype(np.float32)
    t = np.arange(SEQ)
    row = (t // 64).astype(np.float32)[:, None] * inv
    col = (t % 64).astype(np.float32)[:, None] * inv
    ang = np.stack([row, col], axis=1).astype(np.float32)
    return np.stack([np.cos(ang), np.sin(ang)], axis=1).reshape(SEQ, 2, -1).astype(np.float32)


NKC = NTOK // 128


def emit_attention(P, io):
    lamv = P.sb([128, 4, 32], F32)
    P.dma("sp", lamv[:, :, :].rearrange("p a b -> p (a b)"), io["lamv"].rearrange("a b -> (a b)").partition_broadcast(128), writes=[lamv])
    lami = P.sb([128, 1], F32)
    P.dma("sp", lami[:, :], io["laminit"].partition_broadcast(128), writes=[lami])
    subw = P.sb([128, 64], F32)
    P.dma("sp", subw[:, :], io["subw"].partition_broadcast(128), writes=[subw])
    sm = P.sb([128, 8], F32)
    lj = P.sb([128, 32], F32)
    for i in range(2):
        P.op("dve", lambda v, i=i: v.tensor_tensor(out=lj[:, :], in0=lamv[:, 2 * i, :], in1=lamv[:, 2 * i + 1, :], op=ALU.mult),
             reads=[lamv], writes=[lj])
        P.op("dve", lambda v, i=i: v.tensor_reduce(out=sm[:, i:i + 1], in_=lj[:, :], op=ALU.add, axis=AX.X), reads=[lj], writes=[sm])
    P.op("act", lambda a: a.activation(out=sm[:, 0:2], in_=sm[:, 0:2], func=AF.Exp), reads=[sm], writes=[sm])
    P.op("dve", lambda v: v.tensor_tensor(out=sm[:, 2:3], in0=sm[:, 0:1], in1=sm[:, 1:2], op=ALU.subtract), reads=[sm], writes=[sm])
    P.op("dve", lambda v: v.tensor_tensor(out=sm[:, 2:3], in0=sm[:, 2:3], in1=lami[:, 0:1], op=ALU.add), reads=[sm, lami], writes=[sm])
    P.op("dve", lambda v: v.tensor_scalar(out=sm[:, 3:4], in0=sm[:, 2:3], scalar1=-1.0, scalar2=None, op0=ALU.mult), reads=[sm], writes=[sm])
    P.op("dve", lambda v: v.tensor_scalar(out=sm[:, 4:5], in0=lami[:, 0:1], scalar1=-1.0, scalar2=1.0, op0=ALU.mult, op1=ALU.add),
         reads=[lami], writes=[sm])
    P.op("dve", lambda v: v.tensor_scalar(out=subw[:, :], in0=subw[:, :], scalar1=sm[:, 4:5], scalar2=None, op0=ALU.mult),
         reads=[subw, sm], writes=[subw])

    kt = [P.sb([128, NTOK + 256], BF16) for _ in range(2)]
    vt = [P.sb([128, NKC, 65], BF16) for _ in range(2)]
    qt = [P.sb([128, TOK_PC], BF16) for _ in range(2)]
    pt = [P.sb([128, 1024], BF16) for _ in range(4)]
    pss = [P.ps([128, 1024]) for _ in range(3)]
    pso2 = [P.ps([128, 512]) for _ in range(2)]
    class _Acc:
        def __init__(self, t, off):
            self.t, self.off = t, off
        def ap(self, w, c0, c1):
            return self.t[:w, self.off + c0:self.off + c1]
    pso = [_Acc(pso2[j // 2], 128 * (j % 2)) for j in range(4)]
    n0 = P.sb([128, NT, 64], F32)
    o1 = [P.sb([128, 64], F32) for _ in range(2)]
    rz = P.sb([128, 8], F32)
    s2 = [P.sb([128, 2], F32) for _ in range(2)]
    res = [P.sb([128, NT, 64], F32) for _ in range(2)]
    junk = P.sb([128, 64], F32)
    qtiles = [(i * 512, 512, 0, NKC) for i in range(4)] + [(LAT_PC, CTX_PC, 0, 2)]

    jobs = []
    for h in range(4):
        for m in range(2):
            jobs.append(("d", h, m, io["qTd"][2 * h + m], io["kTd"][2 * h + m], io["vd"][h], 32, 32 ** -0.5))
    for h in range(4):
        jobs.append(("g", h, 0, io["qTg"][h], io["kTg"][h // 2], io["vg"][h // 2], 64, 64 ** -0.5))
    nS = 0
    nres = 0
    nv = 0
    ne = 0
    VT = None
    RES = None
    for ji, (kind, h, m, qsrc, ksrc, vsrc, dk, scale) in enumerate(jobs):
        KT, QT = kt[ji % 2], qt[ji % 2]
        G = _rg(dk)
        NJ = (NKC + G - 1) // G
        P.dma("sp", KT[:, 0:NJ * 128], ksrc, writes=[KT])
        for g in range(G):
            P.dma("sp", QT[64 * g:64 * g + dk, :], qsrc, writes=[QT])
        if (kind == "d" and m == 0) or (kind == "g" and h % 2 == 0):
            VT = vt[nv % 2]
            nv += 1
            P.dma("sp", VT[:, :, :].rearrange("p k e -> p (k e)"), vsrc, writes=[VT])
        if (kind == "d" and m == 1) or kind == "g":
            RES = res[nres % 2]
            nres += 1
        for (q0, qw, c_lo, c_hi) in qtiles:
            nsub = (qw + 127) // 128
            pend = []
            npairs = (c_hi - c_lo + 1) // 2
            LOOKP = 2
            for pstep in range(npairs + LOOKP):
                if pstep < npairs:
                    ps = pss[nS % 3]
                    PT = pt[nS % 4]
                    nS += 1
                    cs_ = [c for c in (c_lo + 2 * pstep, c_lo + 2 * pstep + 1) if c < c_hi]
                    for hf, c in enumerate(cs_):
                        P.op("pe", lambda pe, ps=ps, KT=KT, QT=QT, c=c, q0=q0, qw=qw, dk=dk, G=G, hf=hf: pe.matmul(
                            ps[:, hf * 512:hf * 512 + qw], lhsT=KT[64 * (c % G):64 * (c % G) + dk, (c // G) * 128:(c // G + 1) * 128],
                            rhs=QT[64 * (c % G):64 * (c % G) + dk, q0:q0 + qw], start=True, stop=True),
                            reads=[KT, QT], writes=[ps])
                    if qw == 512 and len(cs_) == 2:
                        P.op("act", lambda a, ps=ps, PT=PT, scale=scale: a.activation(out=PT[:, :], in_=ps[:, :], func=AF.Exp, scale=scale),
                             reads=[ps], writes=[PT])
                    else:
                        for hf, c in enumerate(cs_):
                            P.op("act", lambda a, ps=ps, PT=PT, qw=qw, scale=scale, hf=hf: a.activation(
                                out=PT[:, hf * 512:hf * 512 + qw], in_=ps[:, hf * 512:hf * 512 + qw], func=AF.Exp, scale=scale),
                                reads=[ps], writes=[PT])
                    pend.append([(c, PT, hf) for hf, c in enumerate(cs_)])
                if pstep >= LOOKP:
                    for (c, PT, hf) in pend.pop(0):
                        for j in range(nsub):
                            w = min(128, qw - j * 128)
                            first = (c == c_lo)
                            P.op("pe", lambda pe, j=j, w=w, PT=PT, VT=VT, c=c, hf=hf, first=first, c_hi=c_hi: pe.matmul(
                                pso[j].ap(w, 0, 65), lhsT=PT[:, hf * 512 + j * 128:hf * 512 + j * 128 + w], rhs=VT[:, c, :],
                                start=(first and j % 2 == 0), stop=(c == c_hi - 1), skip_group_check=True),
                                reads=[PT, VT], writes=[pso[j].t])
            assert not pend
            for j in range(nsub):
                w = min(128, qw - j * 128)
                tix = (q0 // 128) + j
                P.op("dve", lambda v, j=j, w=w: v.reciprocal(out=rz[:w, j:j + 1], in_=pso[j].ap(w, 64, 65)), reads=[pso[j].t], writes=[(rz, j)])
                if kind == "g":
                    P.op("dve", lambda v, j=j, w=w, RES=RES, tix=tix: v.tensor_scalar(
                        out=RES[:w, tix, :], in0=pso[j].ap(w, 0, 64), scalar1=rz[:w, j:j + 1], scalar2=None, op0=ALU.mult),
                        reads=[pso[j].t, (rz, j)], writes=[(RES, tix)])
                elif m == 0:
                    P.op("dve", lambda v, j=j, w=w, tix=tix: v.tensor_scalar(
                        out=n0[:w, tix, :], in0=pso[j].ap(w, 0, 64), scalar1=rz[:w, j:j + 1], scalar2=None, op0=ALU.mult),
                        reads=[pso[j].t, (rz, j)], writes=[(n0, tix)])
                else:
                    O1 = o1[ne % 2]
                    S2 = s2[ne % 2]
                    ne += 1
                    P.op("dve", lambda v, j=j, w=w, O1=O1: v.tensor_scalar(
                        out=O1[:w, :], in0=pso[j].ap(w, 0, 64), scalar1=rz[:w, j:j + 1], scalar2=None, op0=ALU.mult),
                        reads=[pso[j].t, (rz, j)], writes=[O1])
                    P.op("dve", lambda v, w=w, O1=O1, tix=tix: v.scalar_tensor_tensor(
                        out=O1[:w, :], in0=O1[:w, :], scalar=sm[:w, 3:4], in1=n0[:w, tix, :], op0=ALU.mult, op1=ALU.add),
                        reads=[O1, sm, (n0, tix)], writes=[O1])
                    P.op("pool", lambda g, w=w, O1=O1: g.tensor_tensor(out=junk[:w, :], in0=O1[:w, :], in1=O1[:w, :], op=ALU.mult),
                         reads=[O1], writes=[junk])
                    P.op("dve", lambda v, w=w, S2=S2: v.tensor_reduce(out=S2[:w, 0:1], in_=junk[:w, :], op=ALU.add, axis=AX.X),
                         reads=[junk], writes=[S2])
                    P.op("dve", lambda v, w=w, S2=S2: v.tensor_scalar(out=S2[:w, 1:2], in0=S2[:w, 0:1], scalar1=1.0 / 64, scalar2=EPS,
                                                                     op0=ALU.mult, op1=ALU.add), reads=[S2], writes=[S2])
                    P.op("act", lambda a, w=w, S2=S2: a.activation(out=S2[:w, 1:2], in_=S2[:w, 1:2], func=AF.Sqrt), reads=[S2], writes=[S2])
                    P.op("dve", lambda v, w=w, S2=S2: v.reciprocal(out=S2[:w, 1:2], in_=S2[:w, 1:2]), reads=[S2], writes=[S2])
                    P.op("dve", lambda v, w=w, S2=S2, O1=O1, RES=RES, tix=tix: v.scalar_tensor_tensor(
                        out=RES[:w, tix, :], in0=O1[:w, :], scalar=S2[:w, 1:2], in1=subw[:w, :], op0=ALU.mult, op1=ALU.mult),
                        reads=[O1, S2, subw], writes=[(RES, tix)])
        if (kind == "d" and m == 1) or kind == "g":
            dst = io["od"] if kind == "d" else io["og"]
            P.dma("pool", dst[0:LAT_PC, h * 64:(h + 1) * 64].rearrange("(t p) e -> p t e", p=128), RES[:, 0:16, :],
                  reads=[RES], is_output=True)
            P.dma("pool", dst[LAT_PC:TOK_PC, h * 64:(h + 1) * 64], RES[:CTX_PC, 16, :], reads=[RES], is_output=True)


def build_phase_b_attn():
    nc = _new_nc()
    io = {
        "qTd": _din(nc, "qTd", [8, 32, TOK_PC], BF16), "kTd": _din(nc, "kTd", [8, 128, 65 * 128], BF16),
        "vd": _din(nc, "vd", [4, 128, NKC * 65], BF16),
        "qTg": _din(nc, "qTg", [4, 64, TOK_PC], BF16), "kTg": _din(nc, "kTg", [2, 128, 65 * 128], BF16),
        "vg": _din(nc, "vg", [2, 128, NKC * 65], BF16),
        "lamv": _din(nc, "lamv", [4, 32]), "laminit": _din(nc, "laminit", [1]), "subw": _din(nc, "subw", [64]),
        "od": _dout(nc, "od", [TOK_PC, 256]), "og": _dout(nc, "og", [TOK_PC, 256]),
    }
    with ExitStack() as st:
        P = Prog(nc, st)
        emit_attention(P, io)
        P.finish()
    return nc


def _gather_tok(per_core):
    lat = np.concatenate([a[:LAT_PC] for a in per_core], axis=0)
    ctx = np.concatenate([a[LAT_PC:] for a in per_core], axis=0)
    return np.concatenate([ctx, lat], axis=0)


def _core_rows(glob, i):
    return np.concatenate([glob[CTX + LAT_PC * i:CTX + LAT_PC * (i + 1)], glob[CTX_PC * i:CTX_PC * (i + 1)]], axis=0)


def _v_aug(v):
    n, e = v.shape
    va = np.concatenate([v, np.ones((n, 1), v.dtype)], axis=1)
    return np.ascontiguousarray(va.reshape(NKC, 128, e + 1).transpose(1, 0, 2).reshape(128, NKC * (e + 1)))


RG = {32: 2, 64: 2}


def _rg(dk):
    return RG[dk]


def _row_group_layout(kT, dk):
    nm = kT.shape[0]
    G = _rg(dk)
    NJ = (NKC + G - 1) // G
    pad = np.zeros((nm, dk, NJ * G * 128), kT.dtype)
    pad[:, :, :NTOK] = kT
    v = pad.reshape(nm, dk, NJ, G, 128).transpose(0, 3, 1, 2, 4)
    out = np.zeros((nm, 128, NJ * 128), kT.dtype)
    for g in range(G):
        out[:, 64 * g:64 * g + dk] = v[:, g].reshape(nm, dk, NJ * 128)
    return out


def attn_inputs(qkd, qkg, vv, lamv, laminit, subw):
    gd = _gather_tok(qkd)
    gg = _gather_tok(qkg)
    gv = _gather_tok(vv)
    kTd = _row_group_layout(gd[:, 256:512].reshape(NTOK, 8, 32).transpose(1, 2, 0), 32)
    kTg = _row_group_layout(gg[:, 256:384].reshape(NTOK, 2, 64).transpose(1, 2, 0), 64)
    vd = np.stack([_v_aug(gv[:, h * 64:(h + 1) * 64]) for h in range(4)])
    vg = np.stack([_v_aug(gv[:, 256 + h * 64:256 + (h + 1) * 64]) for h in range(2)])
    maps = []
    for i in range(NCORES):
        qTd = np.ascontiguousarray(qkd[i][:, 0:256].reshape(TOK_PC, 8, 32).transpose(1, 2, 0))
        qTg = np.ascontiguousarray(qkg[i][:, 0:256].reshape(TOK_PC, 4, 64).transpose(1, 2, 0))
        maps.append({"qTd": qTd, "kTd": kTd, "vd": vd, "qTg": qTg, "kTg": kTg, "vg": vg,
                     "lamv": lamv, "laminit": laminit, "subw": subw})
    return maps


XPAD = NTOK + 4


def _xpad_col(tok):
    return tok if tok < CTX else tok + 2


def emit_ssd(P, io):
    nc = P.nc
    triu = P.sb([128, 128], F32)
    ones = P.sb([128, 128], F32)
    identf = P.sb([128, 128], F32)
    negm = P.sb([128, 128], F32)
    identb = P.sb([128, 128], BF16)
    for tl, nm in ((triu, "triu"), (ones, "ones"), (identf, "identf"), (negm, "negm"), (identb, "identb")):
        P.dma("sp", tl[:, :], io[nm], writes=[tl])
    cw = P.sb([128, 3, 4], F32)
    P.dma("sp", cw[0:64, 0, :], io["convw"][0:64, :], writes=[cw])
    P.dma("sp", cw[:, 1, :], io["convw"][64:192, :], writes=[cw])
    P.dma("sp", cw[:, 2, :], io["convw"][192:320, :], writes=[cw])
    import os
    _pre = int(os.environ.get("SSD_PRE", "99"))
    if _pre <= 0:
        return
    scal = P.sb([128, 8], F32)
    P.dma("sp", scal[:, 0:3], io["scal"].partition_broadcast(128), writes=[scal])
    P.op("act", lambda a: a.activation(out=scal[:, 3:4], in_=scal[:, 1:2], func=AF.Exp), reads=[scal], writes=[scal])
    P.op("dve", lambda v: v.tensor_scalar(out=scal[:, 3:4], in0=scal[:, 3:4], scalar1=-1.0, scalar2=None, op0=ALU.mult),
         reads=[scal], writes=[scal])
    P.op("dve", lambda v: v.tensor_scalar(out=scal[:, 4:5], in0=scal[:, 2:3], scalar1=0.5, scalar2=None, op0=ALU.mult),
         reads=[scal], writes=[scal])
    if _pre <= 1:
        return
    dt = P.sb([128, NKC], F32)
    adt = P.sb([128, NKC], F32)
    P.dma("sp", dt[:, :], io["dtraw"], writes=[dt])
    P.op("act", lambda a: a.activation(out=dt[:, :], in_=dt[:, :], func=AF.Exp, bias=scal[:, 0:1]), reads=[dt, scal], writes=[dt])
    P.op("dve", lambda v: v.tensor_scalar(out=dt[:, :], in0=dt[:, :], scalar1=1.0, scalar2=None, op0=ALU.add), reads=[dt], writes=[dt])
    P.op("act", lambda a: a.activation(out=dt[:, :], in_=dt[:, :], func=AF.Ln), reads=[dt], writes=[dt])
    P.op("dve", lambda v: v.tensor_scalar(out=adt[:, :], in0=dt[:, :], scalar1=scal[:, 3:4], scalar2=None, op0=ALU.mult),
         reads=[dt, scal], writes=[adt])
    if _pre <= 2:
        return
    pX, pB, pA, pG, pY, pS, pO = (P.ps([128, 512]) for _ in range(7))
    pcs, ptot = pA, pG
    P.op("pe", lambda pe: pe.matmul(pcs[:, 0:NKC], lhsT=triu[:, :], rhs=adt[:, :], start=True, stop=True), reads=[triu, adt], writes=[pcs])
    P.op("pe", lambda pe: pe.matmul(ptot[:, 0:NKC], lhsT=ones[:, :], rhs=adt[:, :], start=True, stop=True), reads=[ones, adt], writes=[ptot])
    if _pre <= 3:
        return
    nacum = P.sb([128, NKC], F32)
    eacum = P.sb([128, NKC], F32)
    dtd = P.sb([128, NKC], F32)
    etot = P.sb([128, NKC], F32)
    P.op("dve", lambda v: v.tensor_scalar(out=nacum[:, :], in0=pcs[:, 0:NKC], scalar1=-1.0, scalar2=None, op0=ALU.mult), reads=[pcs], writes=[nacum])
    P.op("act", lambda a: a.activation(out=eacum[:, :], in_=pcs[:, 0:NKC], func=AF.Exp), reads=[pcs], writes=[eacum])
    P.op("dve", lambda v: v.tensor_tensor(out=dtd[:, :], in0=ptot[:, 0:NKC], in1=nacum[:, :], op=ALU.add), reads=[ptot, nacum], writes=[dtd])
    P.op("act", lambda a: a.activation(out=dtd[:, :], in_=dtd[:, :], func=AF.Exp), reads=[dtd], writes=[dtd])
    P.op("dve", lambda v: v.tensor_tensor(out=dtd[:, :], in0=dtd[:, :], in1=dt[:, :], op=ALU.mult), reads=[dtd, dt], writes=[dtd])
    P.op("act", lambda a: a.activation(out=etot[:, :], in_=ptot[:, 0:NKC], func=AF.Exp), reads=[ptot], writes=[etot])

    BLK = 8
    raw = [[P.sb([128, BLK * 128 + 2], F32) for _ in range(3)] for _ in range(2)]
    ctmp = [P.sb([128, BLK * 128], F32) for _ in range(2)]
    xT = [P.sb([64, BLK * 128], F32) for _ in range(2)]
    BT = [P.sb([128, BLK * 128], BF16) for _ in range(2)]
    CT = [P.sb([128, BLK * 128], BF16) for _ in range(2)]
    Yb = [P.sb([128, BLK, 64], F32) for _ in range(2)]
    xtm = [P.sb([128, 64], F32) for _ in range(2)]
    xdt = [P.sb([128, 64], BF16) for _ in range(2)]
    xdd = [P.sb([128, 64], BF16) for _ in range(2)]
    btm = [P.sb([128, 128], BF16) for _ in range(2)]
    adtb = [P.sb([128, 128], F32) for _ in range(2)]
    Lt = [P.sb([128, 128], F32) for _ in range(2)]
    Mt = [P.sb([128, 128], BF16) for _ in range(2)]
    ysb = [P.sb([128, 64], F32) for _ in range(2)]
    Hf = P.sb([128, 64], F32)
    Hb = [P.sb([128, 64], BF16) for _ in range(3)]
    P.op("dve", lambda v: v.memset(Hf[:, :], 0.0), writes=[Hf])

    blocks = [(0, 2)] + [(2 + i * BLK, BLK) for i in range(16)]
    import os
    _lvl = int(os.environ.get("SSD_LVL", "9"))
    if _lvl <= 1:
        blocks = []
    for bi, (c0, nch) in enumerate(blocks):
        n = nch * 128
        R = raw[bi % 2]
        col = _xpad_col(c0 * 128)
        src = io["xbcT"]
        P.dma("sp", R[0][0:64, 0:n + 2], src[0:64, col:col + n + 2], writes=[R[0]])
        P.dma("sp", R[1][:, 0:n + 2], src[64:192, col:col + n + 2], writes=[R[1]])
        P.dma("sp", R[2][:, 0:n + 2], src[192:320, col:col + n + 2], writes=[R[2]])
        T = ctmp[bi % 2]
        outs = (xT[bi % 2], BT[bi % 2], CT[bi % 2])
        for gi in range(3):
            rows = 64 if gi == 0 else 128
            e = "dve" if gi != 1 else "pool"
            Rg = R[gi]
            if e == "dve":
                P.op("dve", lambda v, Rg=Rg, T=T, rows=rows, n=n, gi=gi: v.tensor_scalar(
                    out=T[:rows, 0:n], in0=Rg[:rows, 0:n], scalar1=cw[:rows, gi, 0:1], scalar2=cw[:rows, gi, 3:4], op0=ALU.mult, op1=ALU.add),
                    reads=[Rg, cw], writes=[T])
                for tap in (1, 2):
                    P.op("dve", lambda v, Rg=Rg, T=T, rows=rows, n=n, gi=gi, tap=tap: v.scalar_tensor_tensor(
                        out=T[:rows, 0:n], in0=Rg[:rows, tap:tap + n], scalar=cw[:rows, gi, tap:tap + 1], in1=T[:rows, 0:n],
                        op0=ALU.mult, op1=ALU.add), reads=[Rg, cw, T], writes=[T])
            else:
                P.op("pool", lambda g, Rg=Rg, T=T, rows=rows, n=n, gi=gi: g.tensor_scalar(
                    out=T[:rows, 0:n], in0=Rg[:rows, 0:n], scalar1=cw[:rows, gi, 0:1], scalar2=cw[:rows, gi, 3:4], op0=ALU.mult, op1=ALU.add),
                    reads=[Rg, cw], writes=[T])
                for tap in (1, 2):
                    P.op("dve", lambda v, Rg=Rg, T=T, rows=rows, n=n, gi=gi, tap=tap: v.scalar_tensor_tensor(
                        out=T[:rows, 0:n], in0=Rg[:rows, tap:tap + n], scalar=cw[:rows, gi, tap:tap + 1], in1=T[:rows, 0:n],
                        op0=ALU.mult, op1=ALU.add), reads=[Rg, cw, T], writes=[T])
            O = outs[gi]
            P.op("act", lambda a, O=O, T=T, rows=rows, n=n: a.activation(out=O[:rows, 0:n], in_=T[:rows, 0:n], func=AF.Silu),
                 reads=[T], writes=[O])
        XT, BTt, CTt, Y = outs[0], outs[1], outs[2], Yb[bi % 2]
        if _lvl <= 2 or (_lvl == 3 and bi > 0) or (_lvl == 4 and bi > 2):
            continue
        for k in range(nch):
            c = c0 + k
            sl = slice(k * 128, (k + 1) * 128)
            X, XD, XDD, BM = xtm[c % 2], xdt[c % 2], xdd[c % 2], btm[c % 2]
            P.op("pe", lambda pe, XT=XT, sl=sl: pe.transpose(pX[:, 0:64], XT[0:64, sl], identf[0:64, 0:64]), reads=[XT, identf], writes=[pX])
            P.op("act", lambda a, X=X: a.copy(out=X[:, :], in_=pX[:, 0:64]), reads=[pX], writes=[X])
            P.op("dve", lambda v, X=X, XD=XD, c=c: v.tensor_scalar(out=XD[:, :], in0=X[:, :], scalar1=dt[:, c:c + 1], scalar2=None, op0=ALU.mult),
                 reads=[X, dt], writes=[XD])
            P.op("dve", lambda v, X=X, XDD=XDD, c=c: v.tensor_scalar(out=XDD[:, :], in0=X[:, :], scalar1=dtd[:, c:c + 1], scalar2=None, op0=ALU.mult),
                 reads=[X, dtd], writes=[XDD])
            pBv = pB.h[:, 0:256].bitcast(BF16)
            P.op("pe", lambda pe, BTt=BTt, sl=sl, pBv=pBv: pe.transpose(pBv[:, 0:128], BTt[:, sl], identb[:, :]), reads=[BTt, identb], writes=[pB])
            P.op("act", lambda a, BM=BM, pBv=pBv: a.copy(out=BM[:, :], in_=pBv[:, 0:128]), reads=[pB], writes=[BM])
            AB = adtb[c % 2]
            P.op("pool", lambda g, AB=AB, c=c: g.tensor_copy(out=AB[:, :], in_=adt[:, c:c + 1].broadcast_to([128, 128])), reads=[adt], writes=[AB])
            P.op("pe", lambda pe, AB=AB: pe.matmul(pA[:, 0:128], lhsT=AB[:, :], rhs=triu[:, :], start=True, stop=False), reads=[AB, triu], writes=[pA])
            P.op("pe", lambda pe: pe.matmul(pA[:, 0:128], lhsT=identf[:, :], rhs=negm[:, :], start=False, stop=True), reads=[identf, negm], writes=[pA])
            L, M = Lt[c % 2], Mt[c % 2]
            P.op("act", lambda a, L=L, c=c: a.activation(out=L[:, :], in_=pA[:, 0:128], func=AF.Exp, bias=nacum[:, c:c + 1]),
                 reads=[pA, nacum], writes=[L])
            P.op("pe", lambda pe, BTt=BTt, CTt=CTt, sl=sl: pe.matmul(pG[:, 0:128], lhsT=BTt[:, sl], rhs=CTt[:, sl], start=True, stop=True),
                 reads=[BTt, CTt], writes=[pG])
            P.op("dve", lambda v, L=L, M=M: v.tensor_tensor(out=M[:, :], in0=pG[:, 0:128], in1=L[:, :], op=ALU.mult), reads=[pG, L], writes=[M])
            P.op("pe", lambda pe, M=M, XD=XD: pe.matmul(pY[:, 0:64], lhsT=M[:, :], rhs=XD[:, :], start=True, stop=True), reads=[M, XD], writes=[pY])
            YS = ysb[c % 2]
            P.op("act", lambda a, YS=YS: a.copy(out=YS[:, :], in_=pY[:, 0:64]), reads=[pY], writes=[YS])
            if c > 0:
                HP = Hb[(c - 1) % 3]
                P.op("pe", lambda pe, CTt=CTt, sl=sl, HP=HP: pe.matmul(pO[:, 0:64], lhsT=CTt[:, sl], rhs=HP[:, :], start=True, stop=True),
                     reads=[CTt, HP], writes=[pO])
                P.op("dve", lambda v, YS=YS, c=c: v.scalar_tensor_tensor(out=YS[:, :], in0=pO[:, 0:64], scalar=eacum[:, c:c + 1], in1=YS[:, :],
                                                                        op0=ALU.mult, op1=ALU.add), reads=[pO, eacum, YS], writes=[YS])
            P.op("dve", lambda v, YS=YS, X=X, Y=Y, k=k: v.scalar_tensor_tensor(out=Y[:, k, :], in0=X[:, :], scalar=scal[:, 4:5], in1=YS[:, :],
                                                                              op0=ALU.mult, op1=ALU.add), reads=[X, scal, YS], writes=[(Y, k)])
            P.op("pe", lambda pe, BM=BM, XDD=XDD: pe.matmul(pS[:, 0:64], lhsT=BM[:, :], rhs=XDD[:, :], start=True, stop=True),
                 reads=[BM, XDD], writes=[pS])
            P.op("dve", lambda v, c=c: v.scalar_tensor_tensor(out=Hf[:, :], in0=Hf[:, :], scalar=etot[:, c:c + 1], in1=pS[:, 0:64],
                                                             op0=ALU.mult, op1=ALU.add), reads=[Hf, etot, pS], writes=[Hf])
            HN = Hb[c % 3]
            P.op("pool", lambda g, HN=HN: g.tensor_copy(out=HN[:, :], in_=Hf[:, :]), reads=[Hf], writes=[HN])
        P.dma("pool", io["y"][c0 * 128:(c0 + nch) * 128, :].rearrange("(k p) e -> p k e", p=128), Y[:, 0:nch, :], reads=[Y], is_output=True)


def ssd_consts():
    s = np.arange(128)[:, None]
    l = np.arange(128)[None, :]
    return {"triu": (s <= l).astype(np.float32), "ones": np.ones((128, 128), np.float32),
            "identf": np.eye(128, dtype=np.float32), "negm": np.where(s > l, -30000.0, 0.0).astype(np.float32),
            "identb": np.eye(128).astype(ml_dtypes.bfloat16)}


def ssd_io(nc):
    io = {"xbcT": _din(nc, "s_xbcT", [320, XPAD]), "convw": _din(nc, "s_convw", [320, 4]), "dtraw": _din(nc, "s_dtraw", [128, NKC]),
          "scal": _din(nc, "s_scal", [3]), "y": _dout(nc, "s_y", [NTOK, 64])}
    for nm in ("triu", "ones", "identf", "negm"):
        io[nm] = _din(nc, "s_" + nm, [128, 128])
    io["identb"] = _din(nc, "s_identb", [128, 128], BF16)
    return io


def ssd_inputs(u_glob, p, i):
    h, d = i % 4, i // 4
    g = h // 2
    cols = np.concatenate([np.arange(C_SX + h * 64, C_SX + (h + 1) * 64),
                           np.arange(C_SX + 256 + g * 128, C_SX + 256 + (g + 1) * 128),
                           np.arange(C_SX + 512 + g * 128, C_SX + 512 + (g + 1) * 128)])
    uc, ul = u_glob[:CTX], u_glob[CTX:]
    taps = [0, 1, 2]
    if d == 1:
        uc, ul = uc[::-1], ul[::-1]
        taps = [2, 1, 0]
    xp = np.zeros((320, XPAD), np.float32)
    xp[:, 1:1 + CTX] = uc[:, cols].T
    xp[:, 3 + CTX:3 + CTX + SEQ] = ul[:, cols].T
    ccols = cols - C_SX
    convw = np.stack([p["ssd_conv_w"][taps[0], ccols], p["ssd_conv_w"][taps[1], ccols], p["ssd_conv_w"][taps[2], ccols],
                      p["ssd_conv_b"][ccols]], axis=1).astype(np.float32)
    dcol = C_SDT + d * 4 + h
    dts = np.concatenate([uc[:, dcol], ul[:, dcol]])
    dtraw = np.ascontiguousarray(dts.reshape(NKC, 128).T)
    scal = np.array([p["ssd_dt_bias"][d, h], p["ssd_a_log"][d, h], p["ssd_d"][h]], np.float32)
    m = {"s_xbcT": xp, "s_convw": np.ascontiguousarray(convw), "s_dtraw": dtraw, "s_scal": scal}
    for k, v in ssd_consts().items():
        m["s_" + k] = v
    return m


def build_phase_b_ssd():
    nc = _new_nc()
    io = ssd_io(nc)
    with ExitStack() as st:
        P = Prog(nc, st)
        emit_ssd(P, io)
        P.finish()
    return nc


I32 = mybir.dt.int32
TWO_PI = 2.0 * math.pi
PI_SAFE = 3.1415925


def _sincos(P, ang, n, sin_t, cos_t, tmpf, tmpi, msk):
    A = ang
    P.op("dve", lambda v: v.tensor_scalar(out=tmpf[:, :n], in0=A[:, :n], scalar1=1.0 / TWO_PI, scalar2=None, op0=ALU.mult),
         reads=[A], writes=[tmpf])
    P.op("dve", lambda v: v.tensor_copy(out=tmpi[:, :n], in_=tmpf[:, :n]), reads=[tmpf], writes=[tmpi])
    P.op("dve", lambda v: v.tensor_copy(out=tmpf[:, :n], in_=tmpi[:, :n]), reads=[tmpi], writes=[tmpf])
    P.op("dve", lambda v: v.scalar_tensor_tensor(out=sin_t[:, :n], in0=tmpf[:, :n], scalar=-TWO_PI, in1=A[:, :n], op0=ALU.mult, op1=ALU.add),
         reads=[tmpf, A], writes=[sin_t])

    def wrap(T):
        P.op("dve", lambda v: v.tensor_scalar(out=msk[:, :n], in0=T[:, :n], scalar1=math.pi, scalar2=-TWO_PI, op0=ALU.is_gt, op1=ALU.mult),
             reads=[T], writes=[msk])
        P.op("dve", lambda v: v.tensor_tensor(out=T[:, :n], in0=T[:, :n], in1=msk[:, :n], op=ALU.add), reads=[T, msk], writes=[T])
        P.op("dve", lambda v: v.tensor_scalar(out=msk[:, :n], in0=T[:, :n], scalar1=-math.pi, scalar2=TWO_PI, op0=ALU.is_lt, op1=ALU.mult),
             reads=[T], writes=[msk])
        P.op("dve", lambda v: v.tensor_tensor(out=T[:, :n], in0=T[:, :n], in1=msk[:, :n], op=ALU.add), reads=[T, msk], writes=[T])
        P.op("dve", lambda v: v.tensor_scalar(out=T[:, :n], in0=T[:, :n], scalar1=PI_SAFE, scalar2=-PI_SAFE, op0=ALU.min, op1=ALU.max),
             reads=[T], writes=[T])
    wrap(sin_t)
    P.op("dve", lambda v: v.tensor_scalar(out=cos_t[:, :n], in0=sin_t[:, :n], scalar1=math.pi / 2, scalar2=None, op0=ALU.add),
         reads=[sin_t], writes=[cos_t])
    wrap(cos_t)
    P.op("act", lambda a: a.activation(out=sin_t[:, :n], in_=sin_t[:, :n], func=AF.Sin), reads=[sin_t], writes=[sin_t])
    P.op("act", lambda a: a.activation(out=cos_t[:, :n], in_=cos_t[:, :n], func=AF.Sin), reads=[cos_t], writes=[cos_t])


S5L = 512


def emit_s5(P, io):
    identf = P.sb([128, 128], F32)
    P.dma("sp", identf[:, :], io["identf"], writes=[identf])
    iot = P.sb([128, S5L], F32)
    P.dma("sp", iot[:, :], io["iota"].partition_broadcast(128), writes=[iot])
    dsk = P.sb([32, 1], F32)
    P.dma("sp", dsk[:, :], io["dskip"], writes=[dsk])
    Yacc = P.sb([32, NTOK], F32)
    prm = P.sb([128, 2, 4], F32)
    P.dma("sp", prm[:, :, :], io["prm"], writes=[prm])
    Bre = P.sb([128, 32], F32)
    Bim = P.sb([128, 32], F32)
    P.dma("sp", Bre[:, :], io["bre"], writes=[Bre])
    P.dma("sp", Bim[:, :], io["bim"], writes=[Bim])
    tf = P.sb([128, S5L], F32)
    ti = P.sb([128, S5L], I32)
    mk = P.sb([128, S5L], F32)
    sc = P.sb([128, 24], F32)
    ang = P.sb([128, S5L], F32)
    pbu = [[P.ps([128, 512]) for _ in range(2)] for _ in range(2)]
    py = [P.ps([128, 512]) for _ in range(2)]
    pT = P.ps([128, 512])
    uT = [P.sb([32, S5L], F32) for _ in range(3)]
    t1 = [P.sb([128, S5L], F32) for _ in range(2)]
    t2 = [P.sb([128, S5L], F32) for _ in range(2)]
    bre_t = [P.sb([128, S5L], F32) for _ in range(2)]
    bim_t = [P.sb([128, S5L], F32) for _ in range(2)]
    gre = [P.sb([128, S5L], F32) for _ in range(2)]
    gim = [P.sb([128, S5L], F32) for _ in range(2)]
    hre = [P.sb([128, S5L], F32) for _ in range(2)]
    him = [P.sb([128, S5L], F32) for _ in range(2)]
    Rt = P.sb([128, S5L], F32)
    COS = P.sb([128, S5L], F32)
    SIN = P.sb([128, S5L], F32)
    BbT = [P.sb([32, 128], F32) for _ in range(2)]
    Bb = [P.sb([128, 32], F32) for _ in range(2)]
    CT = [P.sb([128, 32], F32) for _ in range(2)]
    h0 = P.sb([128, 2], F32)
    th = P.sb([128, 1], F32)
    sn = P.sb([128, 1], F32)
    cs = P.sb([128, 1], F32)
    ts = lambda o, i, s1, s2, o0, o1=None, rd=(), wr=(): P.op(
        "dve", lambda v: v.tensor_scalar(out=o, in0=i, scalar1=s1, scalar2=s2, op0=o0, **({"op1": o1} if o1 is not None else {})),
        reads=list(rd), writes=list(wr))
    tt = lambda o, a, b, op, rd=(), wr=(), e="dve": P.op(e, lambda v: v.tensor_tensor(out=o, in0=a, in1=b, op=op), reads=list(rd), writes=list(wr))
    chunks = [(0, CTX)] + [(CTX + i * S5L, S5L) for i in range(SEQ // S5L)]
    nchunk = 0
    for d in range(2):
        c = lambda j: sc[:, j:j + 1]
        P.op("act", lambda a: a.activation(out=c(0), in_=prm[:, d, 2:3], func=AF.Exp), reads=[prm], writes=[sc])
        tt(c(1), prm[:, d, 0:1], c(0), ALU.mult, [prm, sc], [sc])
        P.op("act", lambda a: a.activation(out=c(2), in_=c(1), func=AF.Exp), reads=[sc], writes=[sc])
        tt(th[:, 0:1], prm[:, d, 1:2], c(0), ALU.mult, [prm, sc], [th])
        _sincos(P, th, 1, sn, cs, tf, ti, mk)
        tt(c(6), c(2), cs[:, 0:1], ALU.mult, [sc, cs], [sc])
        tt(c(7), c(2), sn[:, 0:1], ALU.mult, [sc, sn], [sc])
        ts(c(6), c(6), -1.0, None, ALU.add, rd=[sc], wr=[sc])
        tt(c(8), prm[:, d, 0:1], prm[:, d, 0:1], ALU.mult, [prm], [sc])
        tt(c(9), prm[:, d, 1:2], prm[:, d, 1:2], ALU.mult, [prm], [sc])
        tt(c(8), c(8), c(9), ALU.add, [sc], [sc])
        P.op("dve", lambda v: v.reciprocal(out=c(8), in_=c(8)), reads=[sc], writes=[sc])
        tt(c(10), c(6), prm[:, d, 0:1], ALU.mult, [sc, prm], [sc])
        tt(c(11), c(7), prm[:, d, 1:2], ALU.mult, [sc, prm], [sc])
        tt(c(10), c(10), c(11), ALU.add, [sc], [sc])
        tt(c(10), c(10), c(8), ALU.mult, [sc], [sc])
        tt(c(12), c(7), prm[:, d, 0:1], ALU.mult, [sc, prm], [sc])
        tt(c(13), c(6), prm[:, d, 1:2], ALU.mult, [sc, prm], [sc])
        tt(c(12), c(12), c(13), ALU.subtract, [sc], [sc])
        tt(c(12), c(12), c(8), ALU.mult, [sc], [sc])
        ts(c(14), c(12), -1.0, None, ALU.mult, rd=[sc], wr=[sc])
        ts(Bb[0][:, :], Bre[:, :], c(10), None, ALU.mult, rd=[Bre, sc], wr=[Bb[0]])
        P.op("dve", lambda v: v.scalar_tensor_tensor(out=Bb[0][:, :], in0=Bim[:, :], scalar=c(14), in1=Bb[0][:, :], op0=ALU.mult, op1=ALU.add),
             reads=[Bim, sc, Bb[0]], writes=[Bb[0]])
        ts(Bb[1][:, :], Bim[:, :], c(10), None, ALU.mult, rd=[Bim, sc], wr=[Bb[1]])
        P.op("dve", lambda v: v.scalar_tensor_tensor(out=Bb[1][:, :], in0=Bre[:, :], scalar=c(12), in1=Bb[1][:, :], op0=ALU.mult, op1=ALU.add),
             reads=[Bre, sc, Bb[1]], writes=[Bb[1]])
        for q in range(2):
            P.op("pe", lambda pe, q=q: pe.transpose(pT[0:32, 0:128], Bb[q][:, :], identf[:, :]), reads=[Bb[q], identf], writes=[pT])
            P.op("act", lambda a, q=q: a.copy(out=BbT[q][:, :], in_=pT[0:32, 0:128]), reads=[pT], writes=[BbT[q]])
        P.dma("sp", CT[0][:, :], io["ctre"][d], writes=[CT[0]])
        P.dma("sp", CT[1][:, :], io["ctim"][d], writes=[CT[1]])
        ts(CT[1][:, :], CT[1][:, :], -1.0, None, ALU.mult, rd=[CT[1]], wr=[CT[1]])
        ts(ang[:, :], iot[:, :], th[:, 0:1], None, ALU.mult, rd=[iot, th], wr=[ang])
        _sincos(P, ang, S5L, SIN, COS, tf, ti, mk)
        ts(Rt[:, :], iot[:, :], 0.0, c(2), ALU.mult, ALU.add, rd=[iot, sc], wr=[Rt])
        P.op("dve", lambda v: v.memset(h0[:, :], 0.0), writes=[h0])
        prev = (h0[:, 0:1], h0[:, 1:2], [h0])
        src = io["uT"][d]
        for ci, (t0, n) in enumerate(chunks):
            k = nchunk % 2
            nchunk += 1
            U = uT[nchunk % 3]
            P.dma("sp", U[:, :n], src[:, t0:t0 + n], writes=[U])
            pr, pi = pbu[k]
            P.op("pe", lambda pe, U=U, pr=pr, n=n: pe.matmul(pr[:, :n], lhsT=BbT[0][:, :], rhs=U[:, :n], start=True, stop=True),
                 reads=[BbT[0], U], writes=[pr])
            P.op("pe", lambda pe, U=U, pi=pi, n=n: pe.matmul(pi[:, :n], lhsT=BbT[1][:, :], rhs=U[:, :n], start=True, stop=True),
                 reads=[BbT[1], U], writes=[pi])
            T1, T2, BR, BI, GR, GI, HR, HI = t1[k], t2[k], bre_t[k], bim_t[k], gre[k], gim[k], hre[k], him[k]
            tt(T1[:, :n], pr[:, :n], COS[:, :n], ALU.mult, [pr, COS], [T1])
            tt(T2[:, :n], pi[:, :n], SIN[:, :n], ALU.mult, [pi, SIN], [T2])
            tt(BR[:, :n], T1[:, :n], T2[:, :n], ALU.add, [T1, T2], [BR])
            tt(T1[:, :n], pi[:, :n], COS[:, :n], ALU.mult, [pi, COS], [T1])
            tt(T2[:, :n], pr[:, :n], SIN[:, :n], ALU.mult, [pr, SIN], [T2])
            tt(BI[:, :n], T1[:, :n], T2[:, :n], ALU.subtract, [T1, T2], [BI])
            P.op("dve", lambda v, GR=GR, BR=BR, n=n, prev=prev: v.tensor_tensor_scan(
                out=GR[:, :n], data0=Rt[:, :n], data1=BR[:, :n], initial=prev[0], op0=ALU.mult, op1=ALU.add),
                reads=[Rt, BR] + prev[2], writes=[GR])
            P.op("dve", lambda v, GI=GI, BI=BI, n=n, prev=prev: v.tensor_tensor_scan(
                out=GI[:, :n], data0=Rt[:, :n], data1=BI[:, :n], initial=prev[1], op0=ALU.mult, op1=ALU.add),
                reads=[Rt, BI] + prev[2], writes=[GI])
            tt(T1[:, :n], GR[:, :n], COS[:, :n], ALU.mult, [GR, COS], [T1])
            tt(T2[:, :n], GI[:, :n], SIN[:, :n], ALU.mult, [GI, SIN], [T2])
            tt(HR[:, :n], T1[:, :n], T2[:, :n], ALU.subtract, [T1, T2], [HR])
            tt(T1[:, :n], GR[:, :n], SIN[:, :n], ALU.mult, [GR, SIN], [T1])
            tt(T2[:, :n], GI[:, :n], COS[:, :n], ALU.mult, [GI, COS], [T2])
            tt(HI[:, :n], T1[:, :n], T2[:, :n], ALU.add, [T1, T2], [HI])
            PY = py[k]
            P.op("pe", lambda pe, PY=PY, HR=HR, n=n: pe.matmul(PY[0:32, :n], lhsT=CT[0][:, :], rhs=HR[:, :n], start=True, stop=False),
                 reads=[CT[0], HR], writes=[PY])
            P.op("pe", lambda pe, PY=PY, HI=HI, n=n: pe.matmul(PY[0:32, :n], lhsT=CT[1][:, :], rhs=HI[:, :n], start=False, stop=True),
                 reads=[CT[1], HI], writes=[PY])
            if d == 0:
                P.op("act", lambda a, PY=PY, t0=t0, n=n: a.copy(out=Yacc[:, t0:t0 + n], in_=PY[0:32, :n]), reads=[PY], writes=[(Yacc, ci)])
            else:
                if ci == 0:
                    lo = 0
                else:
                    lo = CTX + SEQ - (t0 - CTX) - n
                dst = Yacc[:, lo:lo + n][:, ::-1]
                cj = 0 if ci == 0 else len(chunks) - ci
                P.op("dve", lambda v, PY=PY, dst=dst, n=n: v.tensor_tensor(out=dst, in0=PY[0:32, :n], in1=dst, op=ALU.add),
                     reads=[PY, (Yacc, cj)], writes=[(Yacc, cj)])
            prev = (HR[:, n - 1:n], HI[:, n - 1:n], [HR, HI])
    GW = 2048
    ub = [P.sb([32, GW], F32) for _ in range(2)]
    g1 = [P.sb([32, GW], F32) for _ in range(2)]
    g2 = [P.sb([32, GW], F32) for _ in range(2)]
    for gi, t0 in enumerate(range(0, NTOK, GW)):
        n = min(GW, NTOK - t0)
        UB, G1, G2 = ub[gi % 2], g1[gi % 2], g2[gi % 2]
        P.dma("sp", UB[:, :n], io["uT"][0][:, t0:t0 + n], writes=[UB])
        Y = Yacc[:, t0:t0 + n]
        P.op("dve", lambda v, UB=UB, Y=Y, n=n: v.scalar_tensor_tensor(out=Y, in0=UB[:, :n], scalar=dsk[:, 0:1], in1=Y, op0=ALU.mult, op1=ALU.add),
             reads=[UB, dsk, Yacc], writes=[Yacc])
        P.op("dve", lambda g, Y=Y, G1=G1, n=n: g.tensor_tensor(out=G1[:, :n], in0=Y, in1=Y, op=ALU.mult), reads=[Yacc], writes=[G1])
        P.op("dve", lambda v, G1=G1, n=n: v.tensor_scalar(out=G1[:, :n], in0=G1[:, :n], scalar1=0.044715, scalar2=1.0, op0=ALU.mult, op1=ALU.add),
             reads=[G1], writes=[G1])
        P.op("dve", lambda g, Y=Y, G1=G1, n=n: g.tensor_tensor(out=G1[:, :n], in0=G1[:, :n], in1=Y, op=ALU.mult), reads=[Yacc, G1], writes=[G1])
        P.op("act", lambda a, G1=G1, G2=G2, n=n: a.activation(out=G2[:, :n], in_=G1[:, :n], func=AF.Sigmoid, scale=1.5957691216057308),
             reads=[G1], writes=[G2])
        P.op("dve", lambda v, Y=Y, G2=G2, n=n: v.tensor_tensor(out=G2[:, :n], in0=G2[:, :n], in1=Y, op=ALU.mult), reads=[Yacc, G2], writes=[G2])
        P.dma("pool", io["yT"][:, t0:t0 + n], G2[:, :n], reads=[G2], is_output=True)


def s5_io(nc):
    return {"identf": _din(nc, "c_identf", [128, 128]), "iota": _din(nc, "c_iota", [S5L]), "dskip": _din(nc, "c_dskip", [32, 1]),
            "prm": _din(nc, "c_prm", [128, 2, 4]), "bre": _din(nc, "c_bre", [128, 32]), "bim": _din(nc, "c_bim", [128, 32]),
            "ctre": _din(nc, "c_ctre", [2, 128, 32]), "ctim": _din(nc, "c_ctim", [2, 128, 32]),
            "uT": _din(nc, "c_uT", [2, 32, NTOK]), "yT": _dout(nc, "c_yT", [32, NTOK])}


def s5_inputs(u_glob, p, i):
    g0 = 2 * i
    cols = np.arange(C_S5 + g0 * 16, C_S5 + g0 * 16 + 32)
    uf = np.ascontiguousarray(u_glob[:, cols].T)
    ub = np.concatenate([uf[:, :CTX][:, ::-1], uf[:, CTX:][:, ::-1]], axis=1)
    prm = np.zeros((128, 2, 4), np.float32)
    bre = np.zeros((128, 32), np.float32)
    bim = np.zeros((128, 32), np.float32)
    ctre = np.zeros((2, 128, 32), np.float32)
    ctim = np.zeros((2, 128, 32), np.float32)
    for uidx in range(2):
        g = g0 + uidx
        rows = slice(uidx * 64, (uidx + 1) * 64)
        cc = slice(uidx * 16, (uidx + 1) * 16)
        bre[rows, cc] = p["s5_b_re"][g]
        bim[rows, cc] = p["s5_b_im"][g]
        for d in range(2):
            prm[rows, d, 0] = p["s5_lam_re"][d, g]
            prm[rows, d, 1] = p["s5_lam_im"][d, g]
            prm[rows, d, 2] = p["s5_log_dt"][d, g]
            ctre[d, rows, cc] = p["s5_c_re"][d, g].T
            ctim[d, rows, cc] = p["s5_c_im"][d, g].T
    return {"c_identf": np.eye(128, dtype=np.float32), "c_iota": np.arange(1, S5L + 1, dtype=np.float32),
            "c_dskip": np.ascontiguousarray(p["s5_d"][g0 * 16:g0 * 16 + 32].reshape(32, 1)), "c_prm": prm, "c_bre": bre, "c_bim": bim,
            "c_ctre": ctre, "c_ctim": ctim, "c_uT": np.ascontiguousarray(np.stack([uf, ub]))}


def build_phase_b_s5():
    nc = _new_nc()
    io = s5_io(nc)
    with ExitStack() as st:
        P = Prog(nc, st)
        emit_s5(P, io)
        P.finish()
    return nc


def build_phase_c():
    nc = _new_nc()
    x = _din(nc, "x", [TOK_PC, D])
    ad = _din(nc, "ad", [TOK_PC, 256])
    ag = _din(nc, "ag", [TOK_PC, 256])
    ysf = _din(nc, "ysf", [TOK_PC, 256])
    ysb = _din(nc, "ysb", [TOK_PC, 256])
    zz = _din(nc, "zz", [TOK_PC, 256])
    s5T = _din(nc, "s5T", [256, TOK_PC])
    modl = _din(nc, "modl", [6 * D])
    modc = _din(nc, "modc", [6 * D])
    n2w = _din(nc, "n2w", [D])
    snw = _din(nc, "snw", [256])
    wglu = _din(nc, "wglu", [256, 256])
    bglu = _din(nc, "bglu", [128, 2])
    wout = _din(nc, "wout", [D, D])
    wr = _din(nc, "wr", [D, 32])
    br = _din(nc, "br", [32])
    identb_d = _din(nc, "identb", [128, 128], BF16)
    identf_d = _din(nc, "identf", [128, 128])
    x1_out = _dout(nc, "x1", [TOK_PC, D])
    hmT_out = _dout(nc, "hmT", [128, 8, TOK_PC], BF16)
    gates_out = _dout(nc, "gates", [TOK_PC, 32])
    with ExitStack() as st:
        P = Prog(nc, st)
        idb = P.sb([128, 128], BF16)
        idf = P.sb([128, 128], F32)
        P.dma("sp", idb[:, :], identb_d, writes=[idb])
        P.dma("sp", idf[:, :], identf_d, writes=[idf])
        g1 = [P.sb([128, D], F32) for _ in range(2)]
        w2 = [P.sb([128, D], F32) for _ in range(2)]
        sh2 = [P.sb([128, D], F32) for _ in range(2)]
        n2 = P.sb([128, D], F32)
        _load_bcast(P, "sp", n2, n2w, D)
        for i, m in enumerate((modl, modc)):
            _load_bcast(P, "sp", g1[i], m[2 * D:3 * D], D)
            _load_bcast(P, "sp", sh2[i], m[3 * D:4 * D], D)
            _load_bcast(P, "sp", w2[i], m[4 * D:5 * D], D)
            P.op("dve", lambda v, i=i: v.scalar_tensor_tensor(out=w2[i][:, :], in0=w2[i][:, :], scalar=1.0, in1=n2[:, :],
                                                              op0=ALU.add, op1=ALU.mult), reads=[w2[i], n2], writes=[w2[i]])
        snwt = P.sb([128, 256], F32)
        _load_bcast(P, "sp", snwt, snw, 256)
        brt = P.sb([128, 32], F32)
        _load_bcast(P, "sp", brt, br, 32)
        wg = P.sb([128, 2, 256], F32)
        P.dma("sp", wg[:, :, :], wglu.rearrange("(k p) n -> p k n", p=128), writes=[wg])
        bg = P.sb([128, 2], F32)
        P.dma("sp", bg[:, :], bglu, writes=[bg])
        wrt = P.sb([128, 8, 32], F32)
        P.dma("sp", wrt[:, :, :], wr.rearrange("(k p) n -> p k n", p=128), writes=[wrt])
        wob = P.sb([128, 8, D], BF16)
        stg = [P.sb([128, D], F32) for _ in range(2)]
        for k in range(8):
            s = stg[k % 2]
            P.dma("sp", s[:, :], wout[k * 128:(k + 1) * 128, :], writes=[s])
            P.op("pool", lambda g, k=k, s=s: g.tensor_copy(out=wob[:, k, :], in_=s[:, :]), reads=[s], writes=[(wob, k)])

        NB = 2
        xt = [P.sb([128, D], F32) for _ in range(NB)]
        mixin = [P.sb([128, 5, 256], F32) for _ in range(NB)]
        s5t = [P.sb([128, 2, 128], F32) for _ in range(NB)]
        mix = [P.sb([128, 768], BF16) for _ in range(NB)]
        mixT = [P.sb([128, 8, 128], BF16) for _ in range(NB)]
        sg = [P.sb([128, 256], F32) for _ in range(NB)]
        sq = [P.sb([128, 256], F32) for _ in range(NB)]
        st8 = [P.sb([128, 8], F32) for _ in range(NB)]
        x1t = [P.sb([128, D], F32) for _ in range(NB)]
        junk = P.sb([128, D], F32)
        hmf = [P.sb([128, D], F32) for _ in range(NB)]
        hmb = [P.sb([128, D], BF16) for _ in range(NB)]
        hmTb = [P.sb([128, 8, 128], BF16) for _ in range(NB)]
        hmTf = [P.sb([128, 8, 128], F32) for _ in range(NB)]
        sgl = [P.sb([128, 2, 128], F32) for _ in range(NB)]
        lg = [P.sb([128, 32], F32) for _ in range(NB)]
        t8 = [P.sb([128, 8], F32) for _ in range(NB)]
        msk = [P.sb([128, 32], F32) for _ in range(NB)]
        gt = [P.sb([128, 32], F32) for _ in range(NB)]
        pT = P.ps([128, 8, 128], BF16)
        pTf = [P.ps([128, 4, 128]) for _ in range(2)]
        pgl = P.ps([128, 512])
        po = [P.ps([128, 512]) for _ in range(2)]
        plg = P.ps([128, 512])
        for t in range(NT):
            r = 128 if t < 16 else CTX_PC
            ic = 0 if t < 16 else 1
            b = t % NB
            rows = slice(t * 128, t * 128 + r)
            X, MI, S5, MX, MT = xt[b], mixin[b], s5t[b], mix[b], mixT[b]
            P.dma("sp", X[:r, :], x[rows, :], writes=[X])
            for j, src in enumerate((ad, ag, ysf, ysb, zz)):
                P.dma("sp", MI[:r, j, :], src[rows, :], writes=[(MI, j)])
            P.dma("sp", S5[:, :, :r], s5T[:, t * 128:t * 128 + r].rearrange("(k p) n -> p k n", p=128), writes=[S5])
            SG, SQ, S8 = sg[b], sq[b], st8[b]
            P.op("act", lambda a, MI=MI, SG=SG, r=r: a.activation(out=SG[:r, :], in_=MI[:r, 4, :], func=AF.Silu), reads=[(MI, 4)], writes=[SG])
            P.op("pool", lambda g, MI=MI, SQ=SQ, r=r: g.tensor_tensor(out=SQ[:r, :], in0=MI[:r, 2, :], in1=MI[:r, 3, :], op=ALU.add),
                 reads=[(MI, 2), (MI, 3)], writes=[SQ])
            P.op("dve", lambda v, SG=SG, SQ=SQ, r=r: v.tensor_tensor(out=SG[:r, :], in0=SG[:r, :], in1=SQ[:r, :], op=ALU.mult),
                 reads=[SG, SQ], writes=[SG])
            P.op("act", lambda a, SG=SG, SQ=SQ, S8=S8, r=r: a.activation(out=SQ[:r, :], in_=SG[:r, :], func=AF.Square, accum_out=S8[:r, 0:1]),
                 reads=[SG], writes=[SQ, S8])
            P.op("dve", lambda v, S8=S8, r=r: v.tensor_scalar(out=S8[:r, 1:2], in0=S8[:r, 0:1], scalar1=1.0 / 256, scalar2=EPS, op0=ALU.mult, op1=ALU.add),
                 reads=[S8], writes=[S8])
            P.op("act", lambda a, S8=S8, r=r: a.activation(out=S8[:r, 1:2], in_=S8[:r, 1:2], func=AF.Sqrt), reads=[S8], writes=[S8])
            P.op("dve", lambda v, S8=S8, r=r: v.reciprocal(out=S8[:r, 1:2], in_=S8[:r, 1:2]), reads=[S8], writes=[S8])
            P.op("dve", lambda v, SG=SG, S8=S8, MX=MX, r=r: v.scalar_tensor_tensor(
                out=MX[:r, 256:512], in0=SG[:r, :], scalar=S8[:r, 1:2], in1=snwt[:r, :], op0=ALU.mult, op1=ALU.mult),
                reads=[SG, S8, snwt], writes=[(MX, 1)])
            P.op("pool", lambda g, MI=MI, MX=MX, r=r: g.tensor_copy(out=MX[:r, 0:256], in_=MI[:r, 0, :]), reads=[(MI, 0)], writes=[(MX, 0)])
            P.op("pool", lambda g, MI=MI, MX=MX, r=r: g.tensor_copy(out=MX[:r, 512:768], in_=MI[:r, 1, :]), reads=[(MI, 1)], writes=[(MX, 2)])
            for j, kk in enumerate((0, 1, 2, 3, 6, 7)):
                P.op("pe", lambda pe, j=j, kk=kk, MX=MX, r=r: pe.transpose(pT[:, kk, :r], MX[:r, j * 128:(j + 1) * 128], idb[:r, :r]),
                     reads=[MX, idb], writes=[pT])
            P.op("act", lambda a, MT=MT, r=r: a.copy(out=MT[:, 0:4, :r], in_=pT[:, 0:4, :r]), reads=[pT], writes=[(MT, 0)])
            P.op("act", lambda a, MT=MT, r=r: a.copy(out=MT[:, 6:8, :r], in_=pT[:, 6:8, :r]), reads=[pT], writes=[(MT, 2)])
            SL = sgl[b]
            for fo in range(2):
                for k in range(2):
                    P.op("pe", lambda pe, fo=fo, k=k, S5=S5, r=r: pe.matmul(pgl[:, fo * 128:fo * 128 + r], lhsT=wg[:, k, fo * 128:(fo + 1) * 128],
                                                                         rhs=S5[:, k, :r], start=(k == 0), stop=(k == 1)),
                         reads=[wg, S5], writes=[pgl])
                P.op("act", lambda a, fo=fo, SL=SL, r=r: a.activation(out=SL[:, fo, :r], in_=pgl[:, fo * 128:fo * 128 + r], func=AF.Sigmoid,
                                                                      bias=bg[:, fo:fo + 1]), reads=[pgl, bg], writes=[SL])
            P.op("dve", lambda v, SL=SL, S5=S5, MT=MT, r=r: v.tensor_tensor(out=MT[:, 4:6, :r], in0=SL[:, :, :r], in1=S5[:, :, :r], op=ALU.mult),
                 reads=[SL, S5], writes=[(MT, 1)])
            X1 = x1t[b]
            for ct in range(2):
                ps = po[ct]
                for k in range(8):
                    P.op("pe", lambda pe, ps=ps, k=k, ct=ct, MT=MT, r=r: pe.matmul(ps[:r, :], lhsT=MT[:, k, :r], rhs=wob[:, k, ct * 512:(ct + 1) * 512],
                                                                               start=(k == 0), stop=(k == 7)),
                         reads=[MT, (wob, k)], writes=[ps])
                cs = slice(ct * 512, (ct + 1) * 512)
                P.op("dve", lambda v, ps=ps, cs=cs, X1=X1, r=r, ic=ic: v.tensor_tensor(out=X1[:r, cs], in0=ps[:r, :], in1=g1[ic][:r, cs], op=ALU.mult),
                     reads=[ps, g1[ic]], writes=[(X1, ct)])
                P.op("pool", lambda g, cs=cs, X1=X1, X=X, r=r: g.tensor_tensor(out=X1[:r, cs], in0=X1[:r, cs], in1=X[:r, cs], op=ALU.add),
                     reads=[(X1, ct), X], writes=[(X1, ct)])
            P.dma("pool", x1_out[rows, :], X1[:r, :], reads=[X1], is_output=True)
            HF, HB = hmf[b], hmb[b]
            P.op("act", lambda a, X1=X1, S8=S8, r=r: a.activation(out=junk[:r, :], in_=X1[:r, :], func=AF.Square, accum_out=S8[:r, 2:3]),
                 reads=[X1], writes=[junk, S8])
            P.op("dve", lambda v, S8=S8, r=r: v.tensor_scalar(out=S8[:r, 3:4], in0=S8[:r, 2:3], scalar1=1.0 / D, scalar2=EPS, op0=ALU.mult, op1=ALU.add),
                 reads=[S8], writes=[S8])
            P.op("act", lambda a, S8=S8, r=r: a.activation(out=S8[:r, 3:4], in_=S8[:r, 3:4], func=AF.Sqrt), reads=[S8], writes=[S8])
            P.op("dve", lambda v, S8=S8, r=r: v.reciprocal(out=S8[:r, 3:4], in_=S8[:r, 3:4]), reads=[S8], writes=[S8])
            P.op("dve", lambda v, X1=X1, S8=S8, HF=HF, r=r, ic=ic: v.scalar_tensor_tensor(
                out=HF[:r, :], in0=X1[:r, :], scalar=S8[:r, 3:4], in1=w2[ic][:r, :], op0=ALU.mult, op1=ALU.mult),
                reads=[X1, S8, w2[ic]], writes=[HF])
            P.op("pool", lambda g, HF=HF, r=r, ic=ic: g.tensor_tensor(out=HF[:r, :], in0=HF[:r, :], in1=sh2[ic][:r, :], op=ALU.add),
                 reads=[HF, sh2[ic]], writes=[HF])
            P.op("pool", lambda g, HF=HF, HB=HB, r=r: g.tensor_copy(out=HB[:r, :], in_=HF[:r, :]), reads=[HF], writes=[HB])
            HTB, HTF = hmTb[b], hmTf[b]
            for k in range(8):
                P.op("pe", lambda pe, k=k, HB=HB, r=r: pe.transpose(pT[:, k, :r], HB[:r, k * 128:(k + 1) * 128], idb[:r, :r]),
                     reads=[HB, idb], writes=[pT])
            P.op("act", lambda a, HTB=HTB, r=r: a.copy(out=HTB[:, :, :r], in_=pT[:, :, :r]), reads=[pT], writes=[HTB])
            P.dma("pool", hmT_out[:, :, t * 128:t * 128 + r], HTB[:, :, :r], reads=[HTB], is_output=True)
            for k in range(8):
                pp = pTf[k // 4]
                P.op("pe", lambda pe, k=k, pp=pp, HF=HF, r=r: pe.transpose(pp[:, k % 4, :r], HF[:r, k * 128:(k + 1) * 128], idf[:r, :r]),
                     reads=[HF, idf], writes=[pp])
            for hh in range(2):
                P.op("dve", lambda v, hh=hh, HTF=HTF, r=r: v.tensor_copy(out=HTF[:, hh * 4:(hh + 1) * 4, :r], in_=pTf[hh][:, :, :r]),
                     reads=[pTf[hh]], writes=[(HTF, hh)])
            for k in range(8):
                P.op("pe", lambda pe, k=k, HTF=HTF, r=r: pe.matmul(plg[:r, 0:32], lhsT=HTF[:, k, :r], rhs=wrt[:, k, :], start=(k == 0), stop=(k == 7)),
                     reads=[HTF, wrt], writes=[plg])
            LG, T8, MK, GT = lg[b], t8[b], msk[b], gt[b]
            P.op("dve", lambda v, LG=LG, r=r: v.tensor_tensor(out=LG[:r, :], in0=plg[:r, 0:32], in1=brt[:r, :], op=ALU.add), reads=[plg, brt], writes=[LG])
            P.op("dve", lambda v, LG=LG, T8=T8, r=r: v.max(out=T8[:r, :], in_=LG[:r, :]), reads=[LG], writes=[T8])
            P.op("dve", lambda v, LG=LG, T8=T8, MK=MK, r=r: v.tensor_scalar(out=MK[:r, :], in0=LG[:r, :], scalar1=T8[:r, 3:4], scalar2=None, op0=ALU.is_ge),
                 reads=[LG, T8], writes=[MK])
            P.op("dve", lambda v, LG=LG, T8=T8, r=r: v.tensor_scalar(out=LG[:r, :], in0=LG[:r, :], scalar1=T8[:r, 0:1], scalar2=None, op0=ALU.subtract),
                 reads=[LG, T8], writes=[LG])
            P.op("act", lambda a, LG=LG, r=r: a.activation(out=LG[:r, :], in_=LG[:r, :], func=AF.Exp), reads=[LG], writes=[LG])
            P.op("dve", lambda v, LG=LG, MK=MK, r=r: v.tensor_tensor(out=LG[:r, :], in0=LG[:r, :], in1=MK[:r, :], op=ALU.mult), reads=[LG, MK], writes=[LG])
            P.op("dve", lambda v, LG=LG, T8=T8, r=r: v.tensor_reduce(out=T8[:r, 4:5], in_=LG[:r, :], op=ALU.add, axis=AX.X), reads=[LG], writes=[T8])
            P.op("dve", lambda v, T8=T8, r=r: v.reciprocal(out=T8[:r, 4:5], in_=T8[:r, 4:5]), reads=[T8], writes=[T8])
            P.op("dve", lambda v, LG=LG, T8=T8, GT=GT, r=r: v.tensor_scalar(out=GT[:r, :], in0=LG[:r, :], scalar1=T8[:r, 4:5], scalar2=None, op0=ALU.mult),
                 reads=[LG, T8], writes=[GT])
            P.dma("pool", gates_out[rows, :], GT[:r, :], reads=[GT], is_output=True)
        P.finish()
    return nc


NE = 32
HALVES = [(0, 8), (8, 17)]


def build_phase_d(final=False, n_experts=NE):
    nc = _new_nc()
    x1 = _din(nc, "x1", [TOK_PC, D])
    hmT = _din(nc, "hmT", [128, 8, TOK_PC], BF16)
    gates = _din(nc, "gates", [TOK_PC, 32])
    modl = _din(nc, "modl", [6 * D])
    modc = _din(nc, "modc", [6 * D])
    wgate = _din(nc, "wgate", [NE, D, D])
    wup = _din(nc, "wup", [NE, D, D])
    wdown = _din(nc, "wdown", [NE, D, D])
    bgate = _din(nc, "bgate", [128, NE, 8])
    bup = _din(nc, "bup", [128, NE, 8])
    bdown = _din(nc, "bdown", [NE, D])
    identf_d = _din(nc, "identf", [128, 128])
    fnw = _din(nc, "fnw", [D])
    x2_out = _dout(nc, "x2", [TOK_PC, D])
    with ExitStack() as st:
        P = Prog(nc, st)
        idf = P.sb([128, 128], F32)
        P.dma("sp", idf[:, :], identf_d, writes=[idf])
        g2 = [P.sb([128, D], F32) for _ in range(2)]
        _load_bcast(P, "sp", g2[0], modl[5 * D:6 * D], D)
        _load_bcast(P, "sp", g2[1], modc[5 * D:6 * D], D)
        if final:
            fw = P.sb([128, D], F32)
            _load_bcast(P, "sp", fw, fnw, D)
        bgt = P.sb([128, NE, 8], F32)
        but = P.sb([128, NE, 8], F32)
        P.dma("sp", bgt[:, :, :], bgate, writes=[bgt])
        P.dma("sp", but[:, :, :], bup, writes=[but])
        bdt = P.sb([32, D], F32)
        P.dma("sp", bdt[:, :], bdown, writes=[bdt])
        NRING = 14
        ring = [P.sb([128, 2, D], BF16) for _ in range(NRING)]
        stg = [P.sb([128, 2, D], F32) for _ in range(2)]
        hm = P.sb([128, 8, 1056], BF16)
        acc = P.sb([128, 9, D], F32)
        gts = P.sb([128, 9, 32], F32)
        gT = P.sb([32, 128], F32)
        actT = [P.sb([128, 8, 512], BF16) for _ in range(2)]
        gp = [P.sb([128, 512], F32) for _ in range(2)]
        sg = [P.sb([128, 512], BF16) for _ in range(2)]
        up = [P.sb([128, 512], F32) for _ in range(2)]
        xt = [P.sb([128, D], F32) for _ in range(2)]
        junk = P.sb([128, D], F32)
        s8 = [P.sb([128, 4], F32) for _ in range(2)]
        pg = [P.ps([128, 512]) for _ in range(2)]
        pu = [P.ps([128, 512]) for _ in range(2)]
        po = [P.ps([128, 512]) for _ in range(4)]
        nring = 0
        nstg = 0
        nact = 0
        ngu = 0
        npo = 0

        def load_w(src_e):
            nonlocal nring, nstg
            pieces = []
            for j in range(4):
                S = stg[nstg % 2]
                nstg += 1
                R = ring[nring % NRING]
                nring += 1
                P.dma("sp", S[:, :, :], src_e[j * 256:(j + 1) * 256, :].rearrange("(k p) n -> p k n", p=128), writes=[S])
                P.op("pool", lambda g, S=S, R=R: g.tensor_copy(out=R[:, :, :], in_=S[:, :, :]), reads=[S], writes=[R])
                pieces += [(R, 0), (R, 1)]
            return pieces

        for (ta, tb) in HALVES:
            tok0 = ta * 128
            ntok = sum(128 if t < 16 else CTX_PC for t in range(ta, tb))
            nt = tb - ta
            P.dma("sp", hm[:, :, 0:ntok], hmT[:, :, tok0:tok0 + ntok], writes=[hm])
            for t in range(ta, tb):
                r = 128 if t < 16 else CTX_PC
                P.dma("sp", gts[:r, t - ta, :], gates[t * 128:t * 128 + r, :], writes=[(gts, t - ta)])
            for t in range(ta, tb):
                r = 128 if t < 16 else CTX_PC
                lt = t - ta
                pp = po[npo % 4]
                npo += 1
                P.op("pe", lambda pe, pp=pp, lt=lt, r=r: pe.transpose(pp[0:32, 0:r], gts[:r, lt, :], idf[:r, :r]), reads=[(gts, lt), idf], writes=[pp])
                P.op("act", lambda a, pp=pp, r=r: a.copy(out=gT[:, :r], in_=pp[0:32, 0:r]), reads=[pp], writes=[gT])
                for ct in range(2):
                    pq = po[npo % 4]
                    npo += 1
                    P.op("pe", lambda pe, pq=pq, ct=ct, r=r: pe.matmul(pq[:r, :], lhsT=gT[:, :r], rhs=bdt[:, ct * 512:(ct + 1) * 512], start=True, stop=True),
                         reads=[gT, bdt], writes=[pq])
                    P.op("act", lambda a, pq=pq, ct=ct, lt=lt, r=r: a.copy(out=acc[:r, lt, ct * 512:(ct + 1) * 512], in_=pq[:r, :]),
                         reads=[pq], writes=[(acc, (lt, ct))])
            ttiles = []
            o = 0
            while o < ntok:
                w = min(512, ntok - o)
                ttiles.append((o, w))
                o += w
            for e in range(n_experts):
                Wg = load_w(wgate[e])
                Wu = load_w(wup[e])
                Wd = load_w(wdown[e])
                for (o, w) in ttiles:
                    A = actT[nact % 2]
                    nact += 1
                    for c in range(8):
                        PG, PU = pg[ngu % 2], pu[ngu % 2]
                        GP, SG, UP = gp[ngu % 2], sg[ngu % 2], up[ngu % 2]
                        ngu += 1
                        for k in range(8):
                            R, kk = Wg[k]
                            P.op("pe", lambda pe, PG=PG, R=R, kk=kk, k=k, c=c, o=o, w=w: pe.matmul(
                                PG[:, :w], lhsT=R[:, kk, c * 128:(c + 1) * 128], rhs=hm[:, k, o:o + w], start=(k == 0), stop=(k == 7)),
                                reads=[R, hm], writes=[PG])
                        for k in range(8):
                            R, kk = Wu[k]
                            P.op("pe", lambda pe, PU=PU, R=R, kk=kk, k=k, c=c, o=o, w=w: pe.matmul(
                                PU[:, :w], lhsT=R[:, kk, c * 128:(c + 1) * 128], rhs=hm[:, k, o:o + w], start=(k == 0), stop=(k == 7)),
                                reads=[R, hm], writes=[PU])
                        P.op("dve", lambda v, PG=PG, GP=GP, e=e, c=c, w=w: v.tensor_scalar(
                            out=GP[:, :w], in0=PG[:, :w], scalar1=bgt[:, e, c:c + 1], scalar2=7.0, op0=ALU.add, op1=ALU.min),
                            reads=[PG, bgt], writes=[GP])
                        P.op("act", lambda a, GP=GP, SG=SG, w=w: a.activation(out=SG[:, :w], in_=GP[:, :w], func=AF.Sigmoid, scale=1.702),
                             reads=[GP], writes=[SG])
                        P.op("dve", lambda v, PU=PU, UP=UP, e=e, c=c, w=w: v.tensor_scalar(
                            out=UP[:, :w], in0=PU[:, :w], scalar1=but[:, e, c:c + 1], scalar2=7.0, op0=ALU.add, op1=ALU.min),
                            reads=[PU, but], writes=[UP])
                        P.op("dve", lambda v, UP=UP, w=w: v.tensor_scalar(out=UP[:, :w], in0=UP[:, :w], scalar1=-7.0, scalar2=1.0, op0=ALU.max, op1=ALU.add),
                             reads=[UP], writes=[UP])
                        P.op("pool", lambda g, GP=GP, SG=SG, w=w: g.tensor_tensor(out=GP[:, :w], in0=GP[:, :w], in1=SG[:, :w], op=ALU.mult),
                             reads=[GP, SG], writes=[GP])
                        P.op("pool", lambda g, GP=GP, UP=UP, A=A, c=c, w=w: g.tensor_tensor(out=A[:, c, :w], in0=GP[:, :w], in1=UP[:, :w], op=ALU.mult),
                             reads=[GP, UP], writes=[(A, c)])
                    nsub = (w + 127) // 128
                    for j in range(nsub):
                        ww = min(128, w - j * 128)
                        lt = (o + j * 128) // 128
                        for ct in range(2):
                            PO = po[npo % 4]
                            npo += 1
                            for c in range(8):
                                R, kk = Wd[c]
                                P.op("pe", lambda pe, PO=PO, A=A, R=R, kk=kk, c=c, j=j, ww=ww, ct=ct: pe.matmul(
                                    PO[:ww, :], lhsT=A[:, c, j * 128:j * 128 + ww], rhs=R[:, kk, ct * 512:(ct + 1) * 512], start=(c == 0), stop=(c == 7)),
                                    reads=[(A, c), R], writes=[PO])
                            P.op("dve", lambda v, PO=PO, lt=lt, ct=ct, ww=ww, e=e: v.scalar_tensor_tensor(
                                out=acc[:ww, lt, ct * 512:(ct + 1) * 512], in0=PO[:ww, :], scalar=gts[:ww, lt, e:e + 1],
                                in1=acc[:ww, lt, ct * 512:(ct + 1) * 512], op0=ALU.mult, op1=ALU.add),
                                reads=[PO, (gts, lt), (acc, (lt, ct))], writes=[(acc, (lt, ct))])
            for t in range(ta, tb):
                r = 128 if t < 16 else CTX_PC
                ic = 0 if t < 16 else 1
                lt = t - ta
                X = xt[t % 2]
                S8 = s8[t % 2]
                P.dma("sp", X[:r, :], x1[t * 128:t * 128 + r, :], writes=[X])
                P.op("dve", lambda v, lt=lt, r=r, ic=ic: v.tensor_tensor(out=acc[:r, lt, :], in0=acc[:r, lt, :], in1=g2[ic][:r, :], op=ALU.mult),
                     reads=[(acc, (lt, 0)), (acc, (lt, 1)), g2[ic]], writes=[(acc, (lt, 0)), (acc, (lt, 1))])
                P.op("pool", lambda g, X=X, lt=lt, r=r: g.tensor_tensor(out=X[:r, :], in0=X[:r, :], in1=acc[:r, lt, :], op=ALU.add),
                     reads=[X, (acc, (lt, 0)), (acc, (lt, 1))], writes=[X])
                if final:
                    P.op("act", lambda a, X=X, S8=S8, r=r: a.activation(out=junk[:r, :], in_=X[:r, :], func=AF.Square, accum_out=S8[:r, 0:1]),
                         reads=[X], writes=[junk, S8])
                    _rms_rstd(P, S8, Tile_col(S8, 1), r, D)
                    P.op("dve", lambda v, X=X, S8=S8, r=r: v.scalar_tensor_tensor(out=X[:r, :], in0=X[:r, :], scalar=S8[:r, 1:2], in1=fw[:r, :],
                                                                                 op0=ALU.mult, op1=ALU.mult), reads=[X, S8, fw], writes=[X])
                P.dma("pool", x2_out[t * 128:t * 128 + r, :], X[:r, :], reads=[X], is_output=True)
        P.finish()
    return nc


class Tile_col(Tile):
    def __init__(self, t, c):
        self.t, self.c = t, c
        self.h, self.psum, self.bufs = t.h, t.psum, t.bufs

    def __getitem__(self, idx):
        rows, cols = idx
        return self.t.h[rows, self.c + (cols.start or 0):self.c + cols.stop]


def moe_inputs(p):
    wgu = p["moe_w_gate_up"]
    bgu = p["moe_b_gate_up"]
    return {"wgate": np.ascontiguousarray(wgu[:, :, 0::2]), "wup": np.ascontiguousarray(wgu[:, :, 1::2]),
            "wdown": p["moe_w_down"],
            "bgate": np.ascontiguousarray(bgu[:, 0::2].reshape(NE, 8, 128).transpose(2, 0, 1)),
            "bup": np.ascontiguousarray(bgu[:, 1::2].reshape(NE, 8, 128).transpose(2, 0, 1)),
            "bdown": p["moe_b_down"]}


EPC = NE // NCORES
NTT = (NTOK + 511) // 512


def build_phase_d2(n_experts=EPC):
    nc = _new_nc()
    hmT = _din(nc, "hmT", [128, 8, NTOK], BF16)
    gates = _din(nc, "gates", [128, NKC, EPC])
    wgate = _din(nc, "wgate", [EPC, D, D])
    wup = _din(nc, "wup", [EPC, D, D])
    wdown = _din(nc, "wdown", [EPC, D, D])
    bgate = _din(nc, "bgate", [128, EPC, 8])
    bup = _din(nc, "bup", [128, EPC, 8])
    part = _dout(nc, "part", [NTOK, D])
    with ExitStack() as st:
        P = Prog(nc, st)
        bgt = P.sb([128, EPC, 8], F32)
        but = P.sb([128, EPC, 8], F32)
        P.dma("sp", bgt[:, :, :], bgate, writes=[bgt])
        P.dma("sp", but[:, :, :], bup, writes=[but])
        gts = P.sb([128, NKC, EPC], F32)
        P.dma("sp", gts[:, :, :], gates, writes=[gts])
        NRING = 20
        ring = [P.sb([128, 2, D], BF16) for _ in range(NRING)]
        stg = [P.sb([128, 2, D], F32) for _ in range(3)]
        hm = [P.sb([128, 8, 512], BF16) for _ in range(2)]
        actT = [P.sb([128, 8, 512], BF16) for _ in range(2)]
        gp = [P.sb([128, 512], F32) for _ in range(2)]
        sg = [P.sb([128, 512], BF16) for _ in range(2)]
        up = [P.sb([128, 512], F32) for _ in range(2)]
        ot = [P.sb([128, D], F32) for _ in range(3)]
        pv = [P.sb([128, D], F32) for _ in range(3)]
        pg = [P.ps([128, 512]) for _ in range(2)]
        pu = [P.ps([128, 512]) for _ in range(2)]
        po = [P.ps([128, 512]) for _ in range(4)]
        dblk = [Tile(None) for _ in range(NKC)]
        cnt = {"ring": 0, "stg": 0, "gu": 0, "po": 0, "ot": 0}

        def load_w(src_e):
            pieces = []
            for j in range(4):
                S = stg[cnt["stg"] % 3]
                cnt["stg"] += 1
                R = ring[cnt["ring"] % NRING]
                cnt["ring"] += 1
                P.dma("sp", S[:, :, :], src_e[j * 256:(j + 1) * 256, :].rearrange("(k p) n -> p k n", p=128), writes=[S])
                P.op("pool", lambda g, S=S, R=R: g.tensor_copy(out=R[:, :, :], in_=S[:, :, :]), reads=[S], writes=[R])
                pieces += [(R, 0), (R, 1)]
            return pieces

        for e in range(n_experts):
            Wg = load_w(wgate[e])
            Wu = load_w(wup[e])
            Wd = load_w(wdown[e])
            for ti in range(NTT):
                o = ti * 512
                w = min(512, NTOK - o)
                H = hm[ti % 2]
                A = actT[ti % 2]
                P.dma("sp", H[:, :, :w], hmT[:, :, o:o + w], writes=[H])
                for c in range(8):
                    b2 = cnt["gu"] % 2
                    cnt["gu"] += 1
                    PG, PU, GP, SG, UP = pg[b2], pu[b2], gp[b2], sg[b2], up[b2]
                    for k in range(8):
                        R, kk = Wg[k]
                        P.op("pe", lambda pe, PG=PG, R=R, kk=kk, k=k, c=c, H=H, w=w: pe.matmul(
                            PG[:, :w], lhsT=R[:, kk, c * 128:(c + 1) * 128], rhs=H[:, k, :w], start=(k == 0), stop=(k == 7)),
                            reads=[R, H], writes=[PG])
                    for k in range(8):
                        R, kk = Wu[k]
                        P.op("pe", lambda pe, PU=PU, R=R, kk=kk, k=k, c=c, H=H, w=w: pe.matmul(
                            PU[:, :w], lhsT=R[:, kk, c * 128:(c + 1) * 128], rhs=H[:, k, :w], start=(k == 0), stop=(k == 7)),
                            reads=[R, H], writes=[PU])
                    P.op("dve", lambda v, PG=PG, GP=GP, e=e, c=c, w=w: v.tensor_scalar(
                        out=GP[:, :w], in0=PG[:, :w], scalar1=bgt[:, e, c:c + 1], scalar2=7.0, op0=ALU.add, op1=ALU.min),
                        reads=[PG, bgt], writes=[GP])
                    P.op("act", lambda a, GP=GP, SG=SG, w=w: a.activation(out=SG[:, :w], in_=GP[:, :w], func=AF.Sigmoid, scale=1.702),
                         reads=[GP], writes=[SG])
                    P.op("dve", lambda v, PU=PU, UP=UP, e=e, c=c, w=w: v.tensor_scalar(
                        out=UP[:, :w], in0=PU[:, :w], scalar1=but[:, e, c:c + 1], scalar2=7.0, op0=ALU.add, op1=ALU.min),
                        reads=[PU, but], writes=[UP])
                    P.op("dve", lambda v, UP=UP, w=w: v.tensor_scalar(out=UP[:, :w], in0=UP[:, :w], scalar1=-7.0, scalar2=1.0, op0=ALU.max, op1=ALU.add),
                         reads=[UP], writes=[UP])
                    P.op("pool", lambda g, GP=GP, SG=SG, w=w: g.tensor_tensor(out=GP[:, :w], in0=GP[:, :w], in1=SG[:, :w], op=ALU.mult),
                         reads=[GP, SG], writes=[GP])
                    P.op("pool", lambda g, GP=GP, UP=UP, A=A, c=c, w=w: g.tensor_tensor(out=A[:, c, :w], in0=GP[:, :w], in1=UP[:, :w], op=ALU.mult),
                         reads=[GP, UP], writes=[(A, c)])
                for j in range(w // 128):
                    blk = o // 128 + j
                    OT = ot[cnt["ot"] % 3]
                    PV = pv[cnt["ot"] % 3]
                    cnt["ot"] += 1
                    if e > 0:
                        P.dma("sp", PV[:, :], part[blk * 128:(blk + 1) * 128, :], reads=[dblk[blk]], writes=[PV])
                    for ct in range(2):
                        PO = po[cnt["po"] % 4]
                        cnt["po"] += 1
                        for c in range(8):
                            R, kk = Wd[c]
                            P.op("pe", lambda pe, PO=PO, A=A, R=R, kk=kk, c=c, j=j, ct=ct: pe.matmul(
                                PO[:, :], lhsT=A[:, c, j * 128:(j + 1) * 128], rhs=R[:, kk, ct * 512:(ct + 1) * 512], start=(c == 0), stop=(c == 7)),
                                reads=[(A, c), R], writes=[PO])
                        cs = slice(ct * 512, (ct + 1) * 512)
                        if e == 0:
                            P.op("dve", lambda v, PO=PO, OT=OT, cs=cs, blk=blk, e=e: v.tensor_scalar(
                                out=OT[:, cs], in0=PO[:, :], scalar1=gts[:, blk, e:e + 1], scalar2=None, op0=ALU.mult),
                                reads=[PO, gts], writes=[(OT, ct)])
                        else:
                            P.op("dve", lambda v, PO=PO, OT=OT, PV=PV, cs=cs, blk=blk, e=e: v.scalar_tensor_tensor(
                                out=OT[:, cs], in0=PO[:, :], scalar=gts[:, blk, e:e + 1], in1=PV[:, cs], op0=ALU.mult, op1=ALU.add),
                                reads=[PO, gts, PV], writes=[(OT, ct)])
                    P.dma("pool", part[blk * 128:(blk + 1) * 128, :], OT[:, :], reads=[OT], writes=[dblk[blk]], is_output=True)
        P.finish()
    return nc


def build_phase_e(final=False):
    nc = _new_nc()
    x1 = _din(nc, "x1", [TOK_PC, D])
    parts = _din(nc, "parts", [NCORES, TOK_PC, D])
    gates = _din(nc, "gates", [TOK_PC, 32])
    modl = _din(nc, "modl", [6 * D])
    modc = _din(nc, "modc", [6 * D])
    bdown = _din(nc, "bdown", [NE, D])
    identf_d = _din(nc, "identf", [128, 128])
    fnw = _din(nc, "fnw", [D])
    x2_out = _dout(nc, "x2", [TOK_PC, D])
    with ExitStack() as st:
        P = Prog(nc, st)
        idf = P.sb([128, 128], F32)
        P.dma("sp", idf[:, :], identf_d, writes=[idf])
        g2 = [P.sb([128, D], F32) for _ in range(2)]
        _load_bcast(P, "sp", g2[0], modl[5 * D:6 * D], D)
        _load_bcast(P, "sp", g2[1], modc[5 * D:6 * D], D)
        fw = P.sb([128, D], F32)
        _load_bcast(P, "sp", fw, fnw, D)
        bdt = P.sb([32, D], F32)
        P.dma("sp", bdt[:, :], bdown, writes=[bdt])
        pt = [P.sb([128, NCORES, D], F32) for _ in range(2)]
        xt = [P.sb([128, D], F32) for _ in range(2)]
        gt = [P.sb([128, 32], F32) for _ in range(2)]
        gT = [P.sb([32, 128], F32) for _ in range(2)]
        acc = [P.sb([128, D], F32) for _ in range(2)]
        acc2 = [P.sb([128, D], F32) for _ in range(2)]
        junk = P.sb([128, D], F32)
        s8 = [P.sb([128, 4], F32) for _ in range(2)]
        pT = P.ps([128, 512])
        pb = [P.ps([128, 512]) for _ in range(2)]
        for t in range(NT):
            r = 128 if t < 16 else CTX_PC
            ic = 0 if t < 16 else 1
            b = t % 2
            rows = slice(t * 128, t * 128 + r)
            PT, X, G, GT, A, A2, S8 = pt[b], xt[b], gt[b], gT[b], acc[b], acc2[b], s8[b]
            for c in range(NCORES):
                P.dma("sp", PT[:r, c, :], parts[c, rows, :], writes=[(PT, c)])
            P.dma("sp", X[:r, :], x1[rows, :], writes=[X])
            P.dma("sp", G[:r, :], gates[rows, :], writes=[G])
            P.op("pe", lambda pe, G=G, r=r: pe.transpose(pT[0:32, 0:r], G[:r, :], idf[:r, :r]), reads=[G, idf], writes=[pT])
            P.op("act", lambda a, GT=GT, r=r: a.copy(out=GT[:, :r], in_=pT[0:32, 0:r]), reads=[pT], writes=[GT])
            for ct in range(2):
                P.op("pe", lambda pe, ct=ct, GT=GT, r=r: pe.matmul(pb[ct][:r, :], lhsT=GT[:, :r], rhs=bdt[:, ct * 512:(ct + 1) * 512], start=True, stop=True),
                     reads=[GT, bdt], writes=[pb[ct]])
            P.op("dve", lambda v, PT=PT, A=A, r=r: v.tensor_tensor(out=A[:r, :], in0=PT[:r, 0, :], in1=PT[:r, 1, :], op=ALU.add),
                 reads=[(PT, 0), (PT, 1)], writes=[A])
            P.op("pool", lambda g, PT=PT, A2=A2, r=r: g.tensor_tensor(out=A2[:r, :], in0=PT[:r, 2, :], in1=PT[:r, 3, :], op=ALU.add),
                 reads=[(PT, 2), (PT, 3)], writes=[A2])
            P.op("dve", lambda v, PT=PT, A=A, r=r: v.tensor_tensor(out=A[:r, :], in0=A[:r, :], in1=PT[:r, 4, :], op=ALU.add),
                 reads=[(PT, 4), A], writes=[A])
            P.op("pool", lambda g, PT=PT, A2=A2, r=r: g.tensor_tensor(out=A2[:r, :], in0=A2[:r, :], in1=PT[:r, 5, :], op=ALU.add),
                 reads=[(PT, 5), A2], writes=[A2])
            P.op("dve", lambda v, PT=PT, A=A, r=r: v.tensor_tensor(out=A[:r, :], in0=A[:r, :], in1=PT[:r, 6, :], op=ALU.add),
                 reads=[(PT, 6), A], writes=[A])
            P.op("pool", lambda g, PT=PT, A2=A2, r=r: g.tensor_tensor(out=A2[:r, :], in0=A2[:r, :], in1=PT[:r, 7, :], op=ALU.add),
                 reads=[(PT, 7), A2], writes=[A2])
            P.op("dve", lambda v, A=A, A2=A2, r=r: v.tensor_tensor(out=A[:r, :], in0=A[:r, :], in1=A2[:r, :], op=ALU.add), reads=[A, A2], writes=[A])
            for ct in range(2):
                cs = slice(ct * 512, (ct + 1) * 512)
                P.op("dve", lambda v, A=A, ct=ct, cs=cs, r=r: v.tensor_tensor(out=A[:r, cs], in0=A[:r, cs], in1=pb[ct][:r, :], op=ALU.add),
                     reads=[A, pb[ct]], writes=[A])
            P.op("pool", lambda g, A=A, r=r, ic=ic: g.tensor_tensor(out=A[:r, :], in0=A[:r, :], in1=g2[ic][:r, :], op=ALU.mult), reads=[A, g2[ic]], writes=[A])
            P.op("dve", lambda v, A=A, X=X, r=r: v.tensor_tensor(out=X[:r, :], in0=X[:r, :], in1=A[:r, :], op=ALU.add), reads=[A, X], writes=[X])
            if final:
                P.op("act", lambda a, X=X, S8=S8, r=r: a.activation(out=junk[:r, :], in_=X[:r, :], func=AF.Square, accum_out=S8[:r, 0:1]),
                     reads=[X], writes=[junk, S8])
                _rms_rstd(P, S8, Tile_col(S8, 1), r, D)
                P.op("dve", lambda v, X=X, S8=S8, r=r: v.scalar_tensor_tensor(out=X[:r, :], in0=X[:r, :], scalar=S8[:r, 1:2], in1=fw[:r, :],
                                                                             op0=ALU.mult, op1=ALU.mult), reads=[X, S8, fw], writes=[X])
            P.dma("pool", x2_out[rows, :], X[:r, :], reads=[X], is_output=True)
        P.finish()
    return nc


_PROGS = {}


def _prog(name, builder):
    if name not in _PROGS:
        _PROGS[name] = builder()
    return _PROGS[name]


def _run(nc, maps):
    return run_bass_kernel_spmd(nc, maps, core_ids=list(range(NCORES))).results


def kernel(**inp):
    f32 = np.float32
    inp = {k: np.asarray(v) for k, v in inp.items()}
    identb = np.eye(128).astype(ml_dtypes.bfloat16)
    identf = np.eye(128, dtype=f32)
    ropd, ropg = rope_tables(32), rope_tables(64)
    c, cc = inp["c"], inp["c_ctx"]
    cT = np.ascontiguousarray(np.stack([c[0], cc], axis=-1).reshape(8, 128, 2).transpose(1, 0, 2)).astype(f32)
    maps = []
    for i in range(NCORES):
        maps.append({"cT": cT, "wada": np.ascontiguousarray(inp["w_ada"][:, :, 768 * i:768 * (i + 1)]),
                     "bada": np.ascontiguousarray(np.broadcast_to(inp["b_ada"][:, None, 768 * i:768 * (i + 1)], (DEPTH, 2, 768)))})
    res = _run(_prog("m", build_phase_m), maps)
    mod = np.concatenate([r["mod"] for r in res], axis=-1)

    xg = np.concatenate([inp["ctx"][0], inp["x"][0]], axis=0).astype(f32)
    x_cores = [_core_rows(xg, i) for i in range(NCORES)]
    for l in range(DEPTH):
        p = {k: v[l] for k, v in inp.items() if v.ndim >= 1 and v.shape[0] == DEPTH and k not in ("x", "c", "ctx", "c_ctx", "final_norm_w")}
        qkw = np.concatenate([np.tile(p["gqa_q_norm_w"], 4), np.tile(p["gqa_k_norm_w"], 2)]).astype(f32)
        maps = []
        for i in range(NCORES):
            maps.append({"x": x_cores[i], "modl": mod[l, 0], "modc": mod[l, 1], "n1w": p["norm1_w"], "w_in": p["w_in"],
                         "ident": identb, "ropd": ropd[LAT_PC * i:LAT_PC * (i + 1)], "ropg": ropg[LAT_PC * i:LAT_PC * (i + 1)], "qkw": qkw})
        ra = _run(_prog("a", build_phase_a), maps)
        u_mid = _gather_tok([r["u"] for r in ra])
        u_glob = np.zeros((NTOK, D_IN), f32)
        u_glob[:, C_SZ:C_GQ] = u_mid
        lamv = np.stack([p["diff_lq1"], p["diff_lk1"], p["diff_lq2"], p["diff_lk2"]]).astype(f32)
        laminit = np.array([0.8 - 0.6 * math.exp(-0.3 * l)], f32)
        maps = attn_inputs([r["qkd"] for r in ra], [r["qkg"] for r in ra], [r["vv"] for r in ra], lamv, laminit, p["diff_subln_w"])
        rb = _run(_prog("b_attn", build_phase_b_attn), maps)
        maps = [ssd_inputs(u_glob, p, i) for i in range(NCORES)]
        rs = _run(_prog("b_ssd", build_phase_b_ssd), maps)

        def unrev(y):
            return np.concatenate([y[:CTX][::-1], y[CTX:][::-1]], axis=0)
        ysf = np.concatenate([rs[h]["s_y"] for h in range(4)], axis=1)
        ysb = np.concatenate([unrev(rs[4 + h]["s_y"]) for h in range(4)], axis=1)
        maps = [s5_inputs(u_glob, p, i) for i in range(NCORES)]
        rc5 = _run(_prog("b_s5", build_phase_b_s5), maps)
        s5yT = np.concatenate([r["c_yT"] for r in rc5], axis=0)
        s5y = np.ascontiguousarray(s5yT.T)
        zz = u_glob[:, C_SZ:C_SZ + 256]
        maps = []
        for i in range(NCORES):
            maps.append({"x": x_cores[i], "ad": rb[i]["od"], "ag": rb[i]["og"], "ysf": _core_rows(ysf, i), "ysb": _core_rows(ysb, i),
                         "zz": _core_rows(zz, i), "s5T": np.ascontiguousarray(_core_rows(s5y, i).T), "modl": mod[l, 0], "modc": mod[l, 1],
                         "n2w": p["norm2_w"], "snw": p["ssd_norm_w"], "wglu": p["s5_w_glu"],
                         "bglu": np.ascontiguousarray(p["s5_b_glu"].reshape(2, 128).T), "wout": p["w_out"], "wr": p["moe_w_router"],
                         "br": p["moe_b_router"], "identb": identb, "identf": identf})
        rcc = _run(_prog("c", build_phase_c), maps)
        mi = moe_inputs(p)
        hm_all = np.concatenate([r["hmT"] for r in rcc], axis=2)
        g_all = np.concatenate([r["gates"] for r in rcc], axis=0)
        maps = []
        for i in range(NCORES):
            es = slice(EPC * i, EPC * (i + 1))
            maps.append({"hmT": hm_all, "gates": np.ascontiguousarray(g_all[:, es].reshape(NKC, 128, EPC).transpose(1, 0, 2)),
                         "wgate": mi["wgate"][es], "wup": mi["wup"][es], "wdown": mi["wdown"][es],
                         "bgate": np.ascontiguousarray(mi["bgate"][:, es]), "bup": np.ascontiguousarray(mi["bup"][:, es])})
        rd = _run(_prog("d2", build_phase_d2), maps)
        final = (l == DEPTH - 1)
        maps = []
        for i in range(NCORES):
            parts = np.stack([rd[c_]["part"][i * TOK_PC:(i + 1) * TOK_PC] for c_ in range(NCORES)])
            maps.append({"x1": rcc[i]["x1"], "parts": parts, "gates": rcc[i]["gates"], "modl": mod[l, 0], "modc": mod[l, 1],
                         "bdown": mi["bdown"], "identf": identf, "fnw": inp["final_norm_w"]})
        re_ = _run(_prog("e_final" if final else "e", (lambda: build_phase_e(True)) if final else (lambda: build_phase_e(False))), maps)
        x_cores = [r["x2"] for r in re_]
    out = np.concatenate([xc_[:LAT_PC] for xc_ in x_cores], axis=0)
    return out.reshape(1, SEQ, D).astype(f32)
```

```python
from contextlib import ExitStack
import math
import numpy as np
import ml_dtypes
import concourse.bass as bass
import concourse.mybir as mybir
from concourse.bass_utils import run_bass_kernel_spmd

F32 = mybir.dt.float32
BF16 = mybir.dt.bfloat16
U32 = mybir.dt.uint32
AF = mybir.ActivationFunctionType
ALU = mybir.AluOpType
AX = mybir.AxisListType

NCORES = 8
D = 1024
SEQ = 16384
CTX = 256
NTOK = SEQ + CTX
LAT_PC = SEQ // NCORES
CTX_PC = CTX // NCORES
TOK_PC = LAT_PC + CTX_PC
NT = 17
D_IN = 2568
EPS = 1e-6
DEPTH = 4


class _Buf:
    __slots__ = ("w", "r")

    def __init__(self):
        self.w = None
        self.r = {}


class Tile:
    def __init__(self, h, psum=False):
        self.h = h
        self.psum = psum
        self.bufs = {None: _Buf()}

    def __getitem__(self, idx):
        return self.h[idx]


class _Eng:
    def __init__(self, name, h, sem, sem_id):
        self.name, self.h, self.sem, self.sem_id = name, h, sem, sem_id
        self.count = 0
        self.seen = {}


class Prog:
    def __init__(self, nc, stack, n_dma_sems=6):
        self.nc = nc
        self.stack = stack
        self.sems = []
        self.engs = {}
        for name, h in (("pe", nc.tensor), ("act", nc.scalar), ("dve", nc.vector),
                        ("pool", nc.gpsimd), ("sp", nc.sync)):
            s = stack.enter_context(nc.semaphore("sem_" + name))
            self.sems.append(s)
            self.engs[name] = _Eng(name, h, s, len(self.sems) - 1)
        self.dma_sems = {}
        for q in ("sp", "pool", "act"):
            lst = []
            for i in range(n_dma_sems):
                s = stack.enter_context(nc.semaphore(f"dsem_{q}{i}"))
                self.sems.append(s)
                lst.append([len(self.sems) - 1, 0])
            self.dma_sems[q] = [lst, 0]
        self.out_events = []
        self.ntiles = 0

    def sb(self, shape, dtype, name=None):
        self.ntiles += 1
        h = self.stack.enter_context(self.nc.sbuf_tensor(name or f"t{self.ntiles}", list(shape), dtype))
        return Tile(h)

    def ps(self, shape, dtype=F32, name=None):
        self.ntiles += 1
        h = self.stack.enter_context(self.nc.psum_tensor(name or f"p{self.ntiles}", list(shape), dtype))
        return Tile(h, psum=True)

    @staticmethod
    def _split(ref):
        if isinstance(ref, Tile):
            return ref, None
        return ref

    def _check_bufs(self, ref):
        t, k = self._split(ref)
        if k is None:
            return list(t.bufs.values())
        b = t.bufs.get(k)
        if b is None:
            b = t.bufs[k] = _Buf()
        return [t.bufs[None], b]

    def _rec_buf(self, ref):
        t, k = self._split(ref)
        b = t.bufs.get(k)
        if b is None:
            b = t.bufs[k] = _Buf()
        return b

    def _collect(self, reads, writes):
        waits = {}

        def add(ev):
            if ev is None:
                return
            s, v = ev
            if waits.get(s, 0) < v:
                waits[s] = v
        for ref in reads:
            for b in self._check_bufs(ref):
                add(b.w)
        for ref in writes:
            for b in self._check_bufs(ref):
                add(b.w)
                for ev in b.r.items():
                    add(ev)
        return waits

    def _emit_waits(self, E, waits, skip_own=False):
        for s, v in waits.items():
            if E.seen.get(s, 0) >= v:
                continue
            if skip_own and s == E.sem_id:
                continue
            E.h.wait_ge(self.sems[s], v)
            E.seen[s] = v

    def _record(self, ev, reads, writes):
        s, v = ev
        for ref in reads:
            b = self._rec_buf(ref)
            if b.r.get(s, 0) < v:
                b.r[s] = v
        for ref in writes:
            b = self._rec_buf(ref)
            b.w = ev
            b.r = {}
            t, k = self._split(ref)
            if k is None:
                for kk, bb in t.bufs.items():
                    if kk is not None:
                        bb.w = ev
                        bb.r = {}

    def op(self, e, fn, reads=(), writes=()):
        E = self.engs[e]
        pr = [r for r in reads if self._split(r)[0].psum]
        if pr:
            reads = [r for r in reads if not self._split(r)[0].psum]
            writes = list(writes) + pr
        waits = self._collect(reads, writes)
        self._emit_waits(E, waits, skip_own=(e == "pe"))
        inst = fn(E.h)
        E.count += 1
        inst.then_inc(E.sem, 1)
        self._record((E.sem_id, E.count), reads, writes)
        return inst

    def dma(self, q, out, in_, reads=(), writes=(), is_output=False, **kw):
        E = self.engs[q]
        lst, rr = self.dma_sems[q]
        slot = lst[rr]
        self.dma_sems[q][1] = (rr + 1) % len(lst)
        waits = self._collect(reads, writes)
        if slot[1] > 0:
            if waits.get(slot[0], 0) < slot[1]:
                waits[slot[0]] = slot[1]
        self._emit_waits(E, waits)
        inst = E.h.dma_start(out=out, in_=in_, **kw)
        slot[1] += 16
        inst.then_inc(self.sems[slot[0]], 16)
        ev = (slot[0], slot[1])
        self._record(ev, reads, writes)
        if is_output:
            self.out_events.append(ev)
        return inst

    def finish(self, e="sp"):
        E = self.engs[e]
        waits = {}
        for s, v in self.out_events:
            if waits.get(s, 0) < v:
                waits[s] = v
        for q, (lst, _) in self.dma_sems.items():
            for sid, tot in lst:
                if tot > 0 and waits.get(sid, 0) < tot:
                    waits[sid] = tot
        for name, EE in self.engs.items():
            if EE.count > 0 and name != e:
                waits[EE.sem_id] = EE.count
        self._emit_waits(E, waits)


def _bf16(a):
    return np.asarray(a).astype(ml_dtypes.bfloat16)


def _new_nc():
    return bass.Bass("TRN2", target_bir_lowering=False)


def _din(nc, name, shape, dtype=F32):
    return nc.dram_tensor(name, list(shape), dtype, kind="ExternalInput").ap()


def _dout(nc, name, shape, dtype=F32):
    return nc.dram_tensor(name, list(shape), dtype, kind="ExternalOutput").ap()


def build_phase_m():
    nc = _new_nc()
    cT = _din(nc, "cT", [128, 8, 2])
    wada = _din(nc, "wada", [DEPTH, 1024, 768])
    bada = _din(nc, "bada", [DEPTH, 2, 768])
    mod = _dout(nc, "mod", [DEPTH, 2, 768])
    with ExitStack() as st:
        P = Prog(nc, st)
        ct = P.sb([128, 8, 2], F32)
        sc = P.sb([128, 8, 2], F32)
        P.dma("sp", ct[:, :, :], cT[:, :, :], writes=[ct])
        P.op("act", lambda a: a.activation(out=sc[:, :, :], in_=ct[:, :, :], func=AF.Sigmoid), reads=[ct], writes=[sc])
        P.op("dve", lambda v: v.tensor_tensor(out=sc[:, :, :], in0=sc[:, :, :], in1=ct[:, :, :], op=ALU.mult),
             reads=[sc, ct], writes=[sc])
        wt = [P.sb([128, 8, 768], F32) for _ in range(2)]
        bt = [P.sb([2, 768], F32) for _ in range(2)]
        ot = [P.sb([2, 768], F32) for _ in range(2)]
        pss = [P.ps([128, 512]) for _ in range(2)]
        for l in range(DEPTH):
            w = wt[l % 2]
            P.dma("sp", w[:, :, :], wada[l].rearrange("(k p) n -> p k n", p=128), writes=[w])
            P.dma("sp", bt[l % 2][:, :], bada[l], writes=[bt[l % 2]])
            for h in range(2):
                ps = pss[h]
                for k in range(8):
                    P.op("pe", lambda t, k=k, h=h, w=w, ps=ps: t.matmul(ps[0:2, 0:384], lhsT=sc[:, k, :], rhs=w[:, k, h * 384:(h + 1) * 384],
                                                                     start=(k == 0), stop=(k == 7)),
                         reads=[sc, w], writes=[ps])
                P.op("dve", lambda v, h=h, ps=ps, l=l: v.tensor_tensor(out=ot[l % 2][:, h * 384:(h + 1) * 384], in0=ps[0:2, 0:384],
                                                                      in1=bt[l % 2][:, h * 384:(h + 1) * 384], op=ALU.add),
                     reads=[ps, bt[l % 2]], writes=[ot[l % 2]])
            P.dma("sp", mod[l], ot[l % 2][:, :], reads=[ot[l % 2]], is_output=True)
        P.finish()
    return nc


C_DQ, C_DK, C_DV = 0, 256, 512
C_SZ, C_SX, C_SDT = 768, 1024, 1792
C_S5 = 1800
C_GQ, C_GK, C_GV = 2056, 2312, 2440


def _load_bcast(P, q, dst, src_ap, n):
    P.dma(q, dst[:, 0:n], src_ap.partition_broadcast(128), writes=[dst])


def _rms_rstd(P, ss, rstd, r, n, k=1):
    P.op("dve", lambda v: v.tensor_scalar(out=rstd[:r, 0:k], in0=ss[:r, 0:k], scalar1=1.0 / n, scalar2=EPS,
                                          op0=ALU.mult, op1=ALU.add), reads=[ss], writes=[rstd])
    P.op("act", lambda a: a.activation(out=rstd[:r, 0:k], in_=rstd[:r, 0:k], func=AF.Sqrt), reads=[rstd], writes=[rstd])
    P.op("dve", lambda v: v.reciprocal(out=rstd[:r, 0:k], in_=rstd[:r, 0:k]), reads=[rstd], writes=[rstd])


def _rope(P, eng, src, dst, tmp, c0, ngrp, hd, cos_ap, sin_ap, r):
    j = hd // 4
    n = ngrp * hd
    sv = src[:r, c0:c0 + n].rearrange("p (g a h j) -> p g a h j", g=ngrp, a=2, h=2, j=j)
    dv = dst[:r, c0:c0 + n].rearrange("p (g a h j) -> p g a h j", g=ngrp, a=2, h=2, j=j)
    t1 = tmp[:r, 0:n // 2].rearrange("p (g a j) -> p g a j", g=ngrp, a=2, j=j)
    t2 = tmp[:r, n // 2:n].rearrange("p (g a j) -> p g a j", g=ngrp, a=2, j=j)
    cb = cos_ap.unsqueeze(1).broadcast_to([r, ngrp, 2, j])
    sb_ = sin_ap.unsqueeze(1).broadcast_to([r, ngrp, 2, j])
    x0 = sv[:, :, :, 0, :]
    x1 = sv[:, :, :, 1, :]
    tt = lambda o, a, b, op: P.op(eng, lambda v: v.tensor_tensor(out=o, in0=a, in1=b, op=op),
                                  reads=[src, tmp], writes=[tmp, dst])
    tt(t1, x0, cb, ALU.mult)
    tt(t2, x1, sb_, ALU.mult)
    tt(dv[:, :, :, 0, :], t1, t2, ALU.subtract)
    tt(t1, x0, sb_, ALU.mult)
    tt(t2, x1, cb, ALU.mult)
    tt(dv[:, :, :, 1, :], t1, t2, ALU.add)


def build_phase_a():
    nc = _new_nc()
    x = _din(nc, "x", [TOK_PC, D])
    modl = _din(nc, "modl", [6 * D])
    modc = _din(nc, "modc", [6 * D])
    n1w = _din(nc, "n1w", [D])
    w_in = _din(nc, "w_in", [D, D_IN])
    ident = _din(nc, "ident", [128, 128], BF16)
    ropd = _din(nc, "ropd", [LAT_PC, 2, 16])
    ropg = _din(nc, "ropg", [LAT_PC, 2, 32])
    qkw = _din(nc, "qkw", [384])
    u_out = _dout(nc, "u", [TOK_PC, C_GQ - C_SZ])
    qkd_out = _dout(nc, "qkd", [TOK_PC, 512], BF16)
    qkg_out = _dout(nc, "qkg", [TOK_PC, 384], BF16)
    vv_out = _dout(nc, "vv", [TOK_PC, 384], BF16)
    with ExitStack() as st:
        P = Prog(nc, st)
        idt = P.sb([128, 128], BF16)
        P.dma("sp", idt[:, :], ident, writes=[idt])
        w1 = [P.sb([128, D], F32) for _ in range(2)]
        sh1 = [P.sb([128, D], F32) for _ in range(2)]
        n1 = P.sb([128, D], F32)
        _load_bcast(P, "sp", n1, n1w, D)
        for i, m in enumerate((modl, modc)):
            _load_bcast(P, "sp", sh1[i], m[0:D], D)
            _load_bcast(P, "sp", w1[i], m[D:2 * D], D)
            P.op("dve", lambda v, i=i: v.scalar_tensor_tensor(out=w1[i][:, :], in0=w1[i][:, :], scalar=1.0, in1=n1[:, :],
                                                              op0=ALU.add, op1=ALU.mult), reads=[w1[i], n1], writes=[w1[i]])
        qkwt = P.sb([128, 384], F32)
        _load_bcast(P, "sp", qkwt, qkw, 384)
        rd = P.sb([128, 16, 32], F32)
        rg = P.sb([128, 16, 64], F32)
        P.dma("sp", rd[:, :, :], ropd.rearrange("(t p) c j -> p t (c j)", p=128), writes=[rd])
        P.dma("sp", rg[:, :, :], ropg.rearrange("(t p) c j -> p t (c j)", p=128), writes=[rg])
        wbf = P.sb([128, 8, D_IN], BF16)
        stg = [P.sb([128, D_IN], F32) for _ in range(2)]
        for k in range(8):
            s = stg[k % 2]
            P.dma("sp", s[:, :], w_in[k * 128:(k + 1) * 128, :], writes=[s])
            P.op("pool", lambda g, k=k, s=s: g.tensor_copy(out=wbf[:, k, :], in_=s[:, :]), reads=[s], writes=[(wbf, k)])

        xt = [P.sb([128, D], F32) for _ in range(3)]
        junk = P.sb([128, D], F32)
        ss = [P.sb([128, 8], F32) for _ in range(2)]
        rstd = [P.sb([128, 8], F32) for _ in range(2)]
        hf = [P.sb([128, D], F32) for _ in range(2)]
        hb = [P.sb([128, D], BF16) for _ in range(2)]
        hT = [P.sb([128, 8, 128], BF16) for _ in range(2)]
        pT = [P.ps([128, 8, 128], BF16) for _ in range(2)]
        pu = [P.ps([128, 512]) for _ in range(4)]
        ut = [P.sb([128, D_IN], F32) for _ in range(2)]
        qd = [P.sb([128, 512], BF16) for _ in range(2)]
        qg = [P.sb([128, 384], BF16) for _ in range(2)]
        gn = [P.sb([128, 384], F32) for _ in range(2)]
        vvt = [P.sb([128, 384], BF16) for _ in range(2)]
        tmp = [P.sb([128, 512], F32) for _ in range(2)]
        coltiles = [(c, min(512, D_IN - c)) for c in range(0, D_IN, 512)]
        npu = 0
        for t in range(NT):
            r = 128 if t < 16 else CTX_PC
            ic = 0 if t < 16 else 1
            X = xt[t % 3]
            P.dma("sp", X[:r, :], x[t * 128:t * 128 + r, :], writes=[X])
            S, R = ss[t % 2], rstd[t % 2]
            P.op("act", lambda a, X=X, S=S, r=r: a.activation(out=junk[:r, :], in_=X[:r, :], func=AF.Square, accum_out=S[:r, 0:1]),
                 reads=[X], writes=[junk, S])
            _rms_rstd(P, S, R, r, D)
            H, HB = hf[t % 2], hb[t % 2]
            P.op("dve", lambda v, X=X, R=R, H=H, r=r, ic=ic: v.scalar_tensor_tensor(
                out=H[:r, :], in0=X[:r, :], scalar=R[:r, 0:1], in1=w1[ic][:r, :], op0=ALU.mult, op1=ALU.mult),
                reads=[X, R, w1[ic]], writes=[H])
            P.op("dve", lambda g, H=H, HB=HB, r=r, ic=ic: g.tensor_tensor(out=HB[:r, :], in0=H[:r, :], in1=sh1[ic][:r, :], op=ALU.add),
                 reads=[H, sh1[ic]], writes=[HB])
            PT, HT = pT[t % 2], hT[t % 2]
            for k in range(8):
                P.op("pe", lambda pe, k=k, HB=HB, PT=PT, r=r: pe.transpose(PT[:, k, :r], HB[:r, k * 128:(k + 1) * 128], idt[:r, :r]),
                     reads=[HB, idt], writes=[PT])
            P.op("act", lambda a, PT=PT, HT=HT, r=r: a.copy(out=HT[:, :, :r], in_=PT[:, :, :r]), reads=[PT], writes=[HT])
            U = ut[t % 2]
            for ci, (c0, cw) in enumerate(coltiles):
                ps = pu[npu % 4]
                npu += 1
                for k in range(8):
                    P.op("pe", lambda pe, k=k, ps=ps, HT=HT, r=r, c0=c0, cw=cw: pe.matmul(
                        ps[:r, :cw], lhsT=HT[:, k, :r], rhs=wbf[:, k, c0:c0 + cw], start=(k == 0), stop=(k == 7)),
                        reads=[HT, (wbf, k)], writes=[ps])
                e = "act" if ci % 2 == 0 else "dve"
                if e == "act":
                    P.op("act", lambda a, ps=ps, U=U, r=r, c0=c0, cw=cw: a.copy(out=U[:r, c0:c0 + cw], in_=ps[:r, :cw]),
                         reads=[ps], writes=[(U, ci)])
                else:
                    P.op("dve", lambda v, ps=ps, U=U, r=r, c0=c0, cw=cw: v.tensor_copy(out=U[:r, c0:c0 + cw], in_=ps[:r, :cw]),
                         reads=[ps], writes=[(U, ci)])
            P.dma("sp", u_out[t * 128:t * 128 + r, :], U[:r, C_SZ:C_GQ], reads=[U], is_output=True)
            VV = vvt[t % 2]
            P.op("dve", lambda g, U=U, VV=VV, r=r: g.tensor_copy(out=VV[:r, 0:256], in_=U[:r, C_DV:C_DV + 256]), reads=[U], writes=[VV])
            P.op("dve", lambda g, U=U, VV=VV, r=r: g.tensor_copy(out=VV[:r, 256:384], in_=U[:r, C_GV:C_GV + 128]), reads=[U], writes=[VV])
            P.dma("sp", vv_out[t * 128:t * 128 + r, :], VV[:r, :], reads=[VV], is_output=True)
            QD, QG, GN, TM = qd[t % 2], qg[t % 2], gn[t % 2], tmp[t % 2]
            if t < 16:
                cosd = rd[:r, t, 0:16].rearrange("p (a j) -> p a j", a=2)
                sind = rd[:r, t, 16:32].rearrange("p (a j) -> p a j", a=2)
                _rope(P, "dve", U, QD, TM, 0, 16, 32, cosd, sind, r)
            else:
                P.op("dve", lambda v, U=U, QD=QD, r=r: v.tensor_copy(out=QD[:r, :], in_=U[:r, 0:512]), reads=[U], writes=[QD])
            P.dma("sp", qkd_out[t * 128:t * 128 + r, :], QD[:r, :], reads=[QD], is_output=True)
            S2, R2 = ss[t % 2], rstd[t % 2]
            P.op("dve", lambda g, U=U, GN=GN, r=r: g.tensor_tensor(out=GN[:r, :], in0=U[:r, C_GQ:C_GQ + 384], in1=U[:r, C_GQ:C_GQ + 384],
                                                                  op=ALU.mult), reads=[U], writes=[GN])
            P.op("dve", lambda v, GN=GN, S2=S2, r=r: v.tensor_reduce(out=S2[:r, 1:7], in_=GN[:r, :].rearrange("p (g d) -> p g d", g=6),
                                                                    op=ALU.add, axis=AX.X), reads=[GN], writes=[S2])
            P.op("dve", lambda v, S2=S2, R2=R2, r=r: v.tensor_scalar(out=R2[:r, 1:7], in0=S2[:r, 1:7], scalar1=1.0 / 64, scalar2=EPS,
                                                                    op0=ALU.mult, op1=ALU.add), reads=[S2], writes=[R2])
            P.op("act", lambda a, R2=R2, r=r: a.activation(out=R2[:r, 1:7], in_=R2[:r, 1:7], func=AF.Sqrt), reads=[R2], writes=[R2])
            P.op("dve", lambda v, R2=R2, r=r: v.reciprocal(out=R2[:r, 1:7], in_=R2[:r, 1:7]), reads=[R2], writes=[R2])
            P.op("dve", lambda v, U=U, GN=GN, R2=R2, r=r: v.tensor_tensor(
                out=GN[:r, :].rearrange("p (g d) -> p g d", g=6), in0=U[:r, C_GQ:C_GQ + 384].rearrange("p (g d) -> p g d", g=6),
                in1=R2[:r, 1:7].unsqueeze(2).broadcast_to([r, 6, 64]), op=ALU.mult), reads=[U, R2], writes=[GN])
            if t < 16:
                P.op("dve", lambda g, GN=GN, r=r: g.tensor_tensor(out=GN[:r, :], in0=GN[:r, :], in1=qkwt[:r, :], op=ALU.mult),
                     reads=[GN, qkwt], writes=[GN])
                cosg = rg[:r, t, 0:32].rearrange("p (a j) -> p a j", a=2)
                sing = rg[:r, t, 32:64].rearrange("p (a j) -> p a j", a=2)
                _rope(P, "dve", GN, QG, TM, 0, 6, 64, cosg, sing, r)
            else:
                P.op("dve", lambda g, GN=GN, QG=QG, r=r: g.tensor_tensor(out=QG[:r, :], in0=GN[:r, :], in1=qkwt[:r, :], op=ALU.mult),
                     reads=[GN, qkwt], writes=[QG])
            P.dma("sp", qkg_out[t * 128:t * 128 + r, :], QG[:r, :], reads=[QG], is_output=True)
        P.finish()
    return nc


def rope_tables(head_dim):
    axis_dim = head_dim // 2
    inv = (10000.0 ** (-np.arange(0, axis_dim, 2, dtype=np.float32) / axis_dim)).astype(np.float32)
    t = np.arange(SEQ)
    row = (t // 64).astype(np.float32)[:, None] * inv
    col = (t % 64).astype(np.float32)[:, None] * inv
    ang = np.stack([row, col], axis=1).astype(np.float32)
    return np.stack([np.cos(ang), np.sin(ang)], axis=1).reshape(SEQ, 2, -1).astype(np.float32)


NKC = NTOK // 128


def emit_attention(P, io):
    lamv = P.sb([128, 4, 32], F32)
    P.dma("sp", lamv[:, :, :].rearrange("p a b -> p (a b)"), io["lamv"].rearrange("a b -> (a b)").partition_broadcast(128), writes=[lamv])
    lami = P.sb([128, 1], F32)
    P.dma("sp", lami[:, :], io["laminit"].partition_broadcast(128), writes=[lami])
    subw = P.sb([128, 64], F32)
    P.dma("sp", subw[:, :], io["subw"].partition_broadcast(128), writes=[subw])
    sm = P.sb([128, 8], F32)
    lj = P.sb([128, 32], F32)
    for i in range(2):
        P.op("dve", lambda v, i=i: v.tensor_tensor(out=lj[:, :], in0=lamv[:, 2 * i, :], in1=lamv[:, 2 * i + 1, :], op=ALU.mult),
             reads=[lamv], writes=[lj])
        P.op("dve", lambda v, i=i: v.tensor_reduce(out=sm[:, i:i + 1], in_=lj[:, :], op=ALU.add, axis=AX.X), reads=[lj], writes=[sm])
    P.op("act", lambda a: a.activation(out=sm[:, 0:2], in_=sm[:, 0:2], func=AF.Exp), reads=[sm], writes=[sm])
    P.op("dve", lambda v: v.tensor_tensor(out=sm[:, 2:3], in0=sm[:, 0:1], in1=sm[:, 1:2], op=ALU.subtract), reads=[sm], writes=[sm])
    P.op("dve", lambda v: v.tensor_tensor(out=sm[:, 2:3], in0=sm[:, 2:3], in1=lami[:, 0:1], op=ALU.add), reads=[sm, lami], writes=[sm])
    P.op("dve", lambda v: v.tensor_scalar(out=sm[:, 3:4], in0=sm[:, 2:3], scalar1=-1.0, scalar2=None, op0=ALU.mult), reads=[sm], writes=[sm])
    P.op("dve", lambda v: v.tensor_scalar(out=sm[:, 4:5], in0=lami[:, 0:1], scalar1=-1.0, scalar2=1.0, op0=ALU.mult, op1=ALU.add),
         reads=[lami], writes=[sm])
    P.op("dve", lambda v: v.tensor_scalar(out=subw[:, :], in0=subw[:, :], scalar1=sm[:, 4:5], scalar2=None, op0=ALU.mult),
         reads=[subw, sm], writes=[subw])

    kt = [P.sb([128, NTOK + 256], BF16) for _ in range(2)]
    vt = [P.sb([128, NKC, 65], BF16) for _ in range(2)]
    qt = [P.sb([128, TOK_PC], BF16) for _ in range(2)]
    pt = [P.sb([128, 1024], BF16) for _ in range(4)]
    pss = [P.ps([128, 1024]) for _ in range(3)]
    pso2 = [P.ps([128, 512]) for _ in range(2)]
    class _Acc:
        def __init__(self, t, off):
            self.t, self.off = t, off
        def ap(self, w, c0, c1):
            return self.t[:w, self.off + c0:self.off + c1]
    pso = [_Acc(pso2[j // 2], 128 * (j % 2)) for j in range(4)]
    n0 = P.sb([128, NT, 64], F32)
    o1 = [P.sb([128, 64], F32) for _ in range(2)]
    rz = P.sb([128, 8], F32)
    s2 = [P.sb([128, 2], F32) for _ in range(2)]
    res = [P.sb([128, NT, 64], F32) for _ in range(2)]
    junk = P.sb([128, 64], F32)
    qtiles = [(i * 512, 512, 0, NKC) for i in range(4)] + [(LAT_PC, CTX_PC, 0, 2)]

    jobs = []
    for h in range(4):
        for m in range(2):
            jobs.append(("d", h, m, io["qTd"][2 * h + m], io["kTd"][2 * h + m], io["vd"][h], 32, 32 ** -0.5))
    for h in range(4):
        jobs.append(("g", h, 0, io["qTg"][h], io["kTg"][h // 2], io["vg"][h // 2], 64, 64 ** -0.5))
    nS = 0
    nres = 0
    nv = 0
    ne = 0
    VT = None
    RES = None
    for ji, (kind, h, m, qsrc, ksrc, vsrc, dk, scale) in enumerate(jobs):
        KT, QT = kt[ji % 2], qt[ji % 2]
        G = _rg(dk)
        NJ = (NKC + G - 1) // G
        P.dma("sp", KT[:, 0:NJ * 128], ksrc, writes=[KT])
        for g in range(G):
            P.dma("sp", QT[64 * g:64 * g + dk, :], qsrc, writes=[QT])
        if (kind == "d" and m == 0) or (kind == "g" and h % 2 == 0):
            VT = vt[nv % 2]
            nv += 1
            P.dma("sp", VT[:, :, :].rearrange("p k e -> p (k e)"), vsrc, writes=[VT])
        if (kind == "d" and m == 1) or kind == "g":
            RES = res[nres % 2]
            nres += 1
        for (q0, qw, c_lo, c_hi) in qtiles:
            nsub = (qw + 127) // 128
            pend = []
            npairs = (c_hi - c_lo + 1) // 2
            LOOKP = 2
            for pstep in range(npairs + LOOKP):
                if pstep < npairs:
                    ps = pss[nS % 3]
                    PT = pt[nS % 4]
                    nS += 1
                    cs_ = [c for c in (c_lo + 2 * pstep, c_lo + 2 * pstep + 1) if c < c_hi]
                    for hf, c in enumerate(cs_):
                        P.op("pe", lambda pe, ps=ps, KT=KT, QT=QT, c=c, q0=q0, qw=qw, dk=dk, G=G, hf=hf: pe.matmul(
                            ps[:, hf * 512:hf * 512 + qw], lhsT=KT[64 * (c % G):64 * (c % G) + dk, (c // G) * 128:(c // G + 1) * 128],
                            rhs=QT[64 * (c % G):64 * (c % G) + dk, q0:q0 + qw], start=True, stop=True),
                            reads=[KT, QT], writes=[ps])
                    if qw == 512 and len(cs_) == 2:
                        P.op("act", lambda a, ps=ps, PT=PT, scale=scale: a.activation(out=PT[:, :], in_=ps[:, :], func=AF.Exp, scale=scale),
                             reads=[ps], writes=[PT])
                    else:
                        for hf, c in enumerate(cs_):
                            P.op("act", lambda a, ps=ps, PT=PT, qw=qw, scale=scale, hf=hf: a.activation(
                                out=PT[:, hf * 512:hf * 512 + qw], in_=ps[:, hf * 512:hf * 512 + qw], func=AF.Exp, scale=scale),
                                reads=[ps], writes=[PT])
                    pend.append([(c, PT, hf) for hf, c in enumerate(cs_)])
                if pstep >= LOOKP:
                    for (c, PT, hf) in pend.pop(0):
                        for j in range(nsub):
                            w = min(128, qw - j * 128)
                            first = (c == c_lo)
                            P.op("pe", lambda pe, j=j, w=w, PT=PT, VT=VT, c=c, hf=hf, first=first, c_hi=c_hi: pe.matmul(
                                pso[j].ap(w, 0, 65), lhsT=PT[:, hf * 512 + j * 128:hf * 512 + j * 128 + w], rhs=VT[:, c, :],
                                start=(first and j % 2 == 0), stop=(c == c_hi - 1), skip_group_check=True),
                                reads=[PT, VT], writes=[pso[j].t])
            assert not pend
            for j in range(nsub):
                w = min(128, qw - j * 128)
                tix = (q0 // 128) + j
                P.op("dve", lambda v, j=j, w=w: v.reciprocal(out=rz[:w, j:j + 1], in_=pso[j].ap(w, 64, 65)), reads=[pso[j].t], writes=[(rz, j)])
                if kind == "g":
                    P.op("dve", lambda v, j=j, w=w, RES=RES, tix=tix: v.tensor_scalar(
                        out=RES[:w, tix, :], in0=pso[j].ap(w, 0, 64), scalar1=rz[:w, j:j + 1], scalar2=None, op0=ALU.mult),
                        reads=[pso[j].t, (rz, j)], writes=[(RES, tix)])
                elif m == 0:
                    P.op("dve", lambda v, j=j, w=w, tix=tix: v.tensor_scalar(
                        out=n0[:w, tix, :], in0=pso[j].ap(w, 0, 64), scalar1=rz[:w, j:j + 1], scalar2=None, op0=ALU.mult),
                        reads=[pso[j].t, (rz, j)], writes=[(n0, tix)])
                else:
                    O1 = o1[ne % 2]
                    S2 = s2[ne % 2]
                    ne += 1
                    P.op("dve", lambda v, j=j, w=w, O1=O1: v.tensor_scalar(
                        out=O1[:w, :], in0=pso[j].ap(w, 0, 64), scalar1=rz[:w, j:j + 1], scalar2=None, op0=ALU.mult),
                        reads=[pso[j].t, (rz, j)], writes=[O1])
                    P.op("dve", lambda v, w=w, O1=O1, tix=tix: v.scalar_tensor_tensor(
                        out=O1[:w, :], in0=O1[:w, :], scalar=sm[:w, 3:4], in1=n0[:w, tix, :], op0=ALU.mult, op1=ALU.add),
                        reads=[O1, sm, (n0, tix)], writes=[O1])
                    P.op("pool", lambda g, w=w, O1=O1: g.tensor_tensor(out=junk[:w, :], in0=O1[:w, :], in1=O1[:w, :], op=ALU.mult),
                         reads=[O1], writes=[junk])
                    P.op("dve", lambda v, w=w, S2=S2: v.tensor_reduce(out=S2[:w, 0:1], in_=junk[:w, :], op=ALU.add, axis=AX.X),
                         reads=[junk], writes=[S2])
                    P.op("dve", lambda v, w=w, S2=S2: v.tensor_scalar(out=S2[:w, 1:2], in0=S2[:w, 0:1], scalar1=1.0 / 64, scalar2=EPS,
                                                                     op0=ALU.mult, op1=ALU.add), reads=[S2], writes=[S2])
                    P.op("act", lambda a, w=w, S2=S2: a.activation(out=S2[:w, 1:2], in_=S2[:w, 1:2], func=AF.Sqrt), reads=[S2], writes=[S2])
                    P.op("dve", lambda v, w=w, S2=S2: v.reciprocal(out=S2[:w, 1:2], in_=S2[:w, 1:2]), reads=[S2], writes=[S2])
                    P.op("dve", lambda v, w=w, S2=S2, O1=O1, RES=RES, tix=tix: v.scalar_tensor_tensor(
                        out=RES[:w, tix, :], in0=O1[:w, :], scalar=S2[:w, 1:2], in1=subw[:w, :], op0=ALU.mult, op1=ALU.mult),
                        reads=[O1, S2, subw], writes=[(RES, tix)])
        if (kind == "d" and m == 1) or kind == "g":
            dst = io["od"] if kind == "d" else io["og"]
            P.dma("pool", dst[0:LAT_PC, h * 64:(h + 1) * 64].rearrange("(t p) e -> p t e", p=128), RES[:, 0:16, :],
                  reads=[RES], is_output=True)
            P.dma("pool", dst[LAT_PC:TOK_PC, h * 64:(h + 1) * 64], RES[:CTX_PC, 16, :], reads=[RES], is_output=True)


def build_phase_b_attn():
    nc = _new_nc()
    io = {
        "qTd": _din(nc, "qTd", [8, 32, TOK_PC], BF16), "kTd": _din(nc, "kTd", [8, 128, 65 * 128], BF16),
        "vd": _din(nc, "vd", [4, 128, NKC * 65], BF16),
        "qTg": _din(nc, "qTg", [4, 64, TOK_PC], BF16), "kTg": _din(nc, "kTg", [2, 128, 65 * 128], BF16),
        "vg": _din(nc, "vg", [2, 128, NKC * 65], BF16),
        "lamv": _din(nc, "lamv", [4, 32]), "laminit": _din(nc, "laminit", [1]), "subw": _din(nc, "subw", [64]),
        "od": _dout(nc, "od", [TOK_PC, 256]), "og": _dout(nc, "og", [TOK_PC, 256]),
    }
    with ExitStack() as st:
        P = Prog(nc, st)
        emit_attention(P, io)
        P.finish()
    return nc


def _gather_tok(per_core):
    lat = np.concatenate([a[:LAT_PC] for a in per_core], axis=0)
    ctx = np.concatenate([a[LAT_PC:] for a in per_core], axis=0)
    return np.concatenate([ctx, lat], axis=0)


def _core_rows(glob, i):
    return np.concatenate([glob[CTX + LAT_PC * i:CTX + LAT_PC * (i + 1)], glob[CTX_PC * i:CTX_PC * (i + 1)]], axis=0)


def _v_aug(v):
    n, e = v.shape
    va = np.concatenate([v, np.ones((n, 1), v.dtype)], axis=1)
    return np.ascontiguousarray(va.reshape(NKC, 128, e + 1).transpose(1, 0, 2).reshape(128, NKC * (e + 1)))


RG = {32: 2, 64: 2}


def _rg(dk):
    return RG[dk]


def _row_group_layout(kT, dk):
    nm = kT.shape[0]
    G = _rg(dk)
    NJ = (NKC + G - 1) // G
    pad = np.zeros((nm, dk, NJ * G * 128), kT.dtype)
    pad[:, :, :NTOK] = kT
    v = pad.reshape(nm, dk, NJ, G, 128).transpose(0, 3, 1, 2, 4)
    out = np.zeros((nm, 128, NJ * 128), kT.dtype)
    for g in range(G):
        out[:, 64 * g:64 * g + dk] = v[:, g].reshape(nm, dk, NJ * 128)
    return out


def attn_inputs(qkd, qkg, vv, lamv, laminit, subw):
    gd = _gather_tok(qkd)
    gg = _gather_tok(qkg)
    gv = _gather_tok(vv)
    kTd = _row_group_layout(gd[:, 256:512].reshape(NTOK, 8, 32).transpose(1, 2, 0), 32)
    kTg = _row_group_layout(gg[:, 256:384].reshape(NTOK, 2, 64).transpose(1, 2, 0), 64)
    vd = np.stack([_v_aug(gv[:, h * 64:(h + 1) * 64]) for h in range(4)])
    vg = np.stack([_v_aug(gv[:, 256 + h * 64:256 + (h + 1) * 64]) for h in range(2)])
    maps = []
    for i in range(NCORES):
        qTd = np.ascontiguousarray(qkd[i][:, 0:256].reshape(TOK_PC, 8, 32).transpose(1, 2, 0))
        qTg = np.ascontiguousarray(qkg[i][:, 0:256].reshape(TOK_PC, 4, 64).transpose(1, 2, 0))
        maps.append({"qTd": qTd, "kTd": kTd, "vd": vd, "qTg": qTg, "kTg": kTg, "vg": vg,
                     "lamv": lamv, "laminit": laminit, "subw": subw})
    return maps


XPAD = NTOK + 4


def _xpad_col(tok):
    return tok if tok < CTX else tok + 2


def emit_ssd(P, io):
    nc = P.nc
    triu = P.sb([128, 128], F32)
    ones = P.sb([128, 128], F32)
    identf = P.sb([128, 128], F32)
    negm = P.sb([128, 128], F32)
    identb = P.sb([128, 128], BF16)
    for tl, nm in ((triu, "triu"), (ones, "ones"), (identf, "identf"), (negm, "negm"), (identb, "identb")):
        P.dma("sp", tl[:, :], io[nm], writes=[tl])
    cw = P.sb([128, 3, 4], F32)
    P.dma("sp", cw[0:64, 0, :], io["convw"][0:64, :], writes=[cw])
    P.dma("sp", cw[:, 1, :], io["convw"][64:192, :], writes=[cw])
    P.dma("sp", cw[:, 2, :], io["convw"][192:320, :], writes=[cw])
    import os
    _pre = int(os.environ.get("SSD_PRE", "99"))
    if _pre <= 0:
        return
    scal = P.sb([128, 8], F32)
    P.dma("sp", scal[:, 0:3], io["scal"].partition_broadcast(128), writes=[scal])
    P.op("act", lambda a: a.activation(out=scal[:, 3:4], in_=scal[:, 1:2], func=AF.Exp), reads=[scal], writes=[scal])
    P.op("dve", lambda v: v.tensor_scalar(out=scal[:, 3:4], in0=scal[:, 3:4], scalar1=-1.0, scalar2=None, op0=ALU.mult),
         reads=[scal], writes=[scal])
    P.op("dve", lambda v: v.tensor_scalar(out=scal[:, 4:5], in0=scal[:, 2:3], scalar1=0.5, scalar2=None, op0=ALU.mult),
         reads=[scal], writes=[scal])
    if _pre <= 1:
        return
    dt = P.sb([128, NKC], F32)
    adt = P.sb([128, NKC], F32)
    P.dma("sp", dt[:, :], io["dtraw"], writes=[dt])
    P.op("act", lambda a: a.activation(out=dt[:, :], in_=dt[:, :], func=AF.Exp, bias=scal[:, 0:1]), reads=[dt, scal], writes=[dt])
    P.op("dve", lambda v: v.tensor_scalar(out=dt[:, :], in0=dt[:, :], scalar1=1.0, scalar2=None, op0=ALU.add), reads=[dt], writes=[dt])
    P.op("act", lambda a: a.activation(out=dt[:, :], in_=dt[:, :], func=AF.Ln), reads=[dt], writes=[dt])
    P.op("dve", lambda v: v.tensor_scalar(out=adt[:, :], in0=dt[:, :], scalar1=scal[:, 3:4], scalar2=None, op0=ALU.mult),
         reads=[dt, scal], writes=[adt])
    if _pre <= 2:
        return
    pX, pB, pA, pG, pY, pS, pO = (P.ps([128, 512]) for _ in range(7))
    pcs, ptot = pA, pG
    P.op("pe", lambda pe: pe.matmul(pcs[:, 0:NKC], lhsT=triu[:, :], rhs=adt[:, :], start=True, stop=True), reads=[triu, adt], writes=[pcs])
    P.op("pe", lambda pe: pe.matmul(ptot[:, 0:NKC], lhsT=ones[:, :], rhs=adt[:, :], start=True, stop=True), reads=[ones, adt], writes=[ptot])
    if _pre <= 3:
        return
    nacum = P.sb([128, NKC], F32)
    eacum = P.sb([128, NKC], F32)
    dtd = P.sb([128, NKC], F32)
    etot = P.sb([128, NKC], F32)
    P.op("dve", lambda v: v.tensor_scalar(out=nacum[:, :], in0=pcs[:, 0:NKC], scalar1=-1.0, scalar2=None, op0=ALU.mult), reads=[pcs], writes=[nacum])
    P.op("act", lambda a: a.activation(out=eacum[:, :], in_=pcs[:, 0:NKC], func=AF.Exp), reads=[pcs], writes=[eacum])
    P.op("dve", lambda v: v.tensor_tensor(out=dtd[:, :], in0=ptot[:, 0:NKC], in1=nacum[:, :], op=ALU.add), reads=[ptot, nacum], writes=[dtd])
    P.op("act", lambda a: a.activation(out=dtd[:, :], in_=dtd[:, :], func=AF.Exp), reads=[dtd], writes=[dtd])
    P.op("dve", lambda v: v.tensor_tensor(out=dtd[:, :], in0=dtd[:, :], in1=dt[:, :], op=ALU.mult), reads=[dtd, dt], writes=[dtd])
    P.op("act", lambda a: a.activation(out=etot[:, :], in_=ptot[:, 0:NKC], func=AF.Exp), reads=[ptot], writes=[etot])

    BLK = 8
    raw = [[P.sb([128, BLK * 128 + 2], F32) for _ in range(3)] for _ in range(2)]
    ctmp = [P.sb([128, BLK * 128], F32) for _ in range(2)]
    xT = [P.sb([64, BLK * 128], F32) for _ in range(2)]
    BT = [P.sb([128, BLK * 128], BF16) for _ in range(2)]
    CT = [P.sb([128, BLK * 128], BF16) for _ in range(2)]
    Yb = [P.sb([128, BLK, 64], F32) for _ in range(2)]
    xtm = [P.sb([128, 64], F32) for _ in range(2)]
    xdt = [P.sb([128, 64], BF16) for _ in range(2)]
    xdd = [P.sb([128, 64], BF16) for _ in range(2)]
    btm = [P.sb([128, 128], BF16) for _ in range(2)]
    adtb = [P.sb([128, 128], F32) for _ in range(2)]
    Lt = [P.sb([128, 128], F32) for _ in range(2)]
    Mt = [P.sb([128, 128], BF16) for _ in range(2)]
    ysb = [P.sb([128, 64], F32) for _ in range(2)]
    Hf = P.sb([128, 64], F32)
    Hb = [P.sb([128, 64], BF16) for _ in range(3)]
    P.op("dve", lambda v: v.memset(Hf[:, :], 0.0), writes=[Hf])

    blocks = [(0, 2)] + [(2 + i * BLK, BLK) for i in range(16)]
    import os
    _lvl = int(os.environ.get("SSD_LVL", "9"))
    if _lvl <= 1:
        blocks = []
    for bi, (c0, nch) in enumerate(blocks):
        n = nch * 128
        R = raw[bi % 2]
        col = _xpad_col(c0 * 128)
        src = io["xbcT"]
        P.dma("sp", R[0][0:64, 0:n + 2], src[0:64, col:col + n + 2], writes=[R[0]])
        P.dma("sp", R[1][:, 0:n + 2], src[64:192, col:col + n + 2], writes=[R[1]])
        P.dma("sp", R[2][:, 0:n + 2], src[192:320, col:col + n + 2], writes=[R[2]])
        T = ctmp[bi % 2]
        outs = (xT[bi % 2], BT[bi % 2], CT[bi % 2])
        for gi in range(3):
            rows = 64 if gi == 0 else 128
            e = "dve" if gi != 1 else "pool"
            Rg = R[gi]
            if e == "dve":
                P.op("dve", lambda v, Rg=Rg, T=T, rows=rows, n=n, gi=gi: v.tensor_scalar(
                    out=T[:rows, 0:n], in0=Rg[:rows, 0:n], scalar1=cw[:rows, gi, 0:1], scalar2=cw[:rows, gi, 3:4], op0=ALU.mult, op1=ALU.add),
                    reads=[Rg, cw], writes=[T])
                for tap in (1, 2):
                    P.op("dve", lambda v, Rg=Rg, T=T, rows=rows, n=n, gi=gi, tap=tap: v.scalar_tensor_tensor(
                        out=T[:rows, 0:n], in0=Rg[:rows, tap:tap + n], scalar=cw[:rows, gi, tap:tap + 1], in1=T[:rows, 0:n],
                        op0=ALU.mult, op1=ALU.add), reads=[Rg, cw, T], writes=[T])
            else:
                P.op("pool", lambda g, Rg=Rg, T=T, rows=rows, n=n, gi=gi: g.tensor_scalar(
                    out=T[:rows, 0:n], in0=Rg[:rows, 0:n], scalar1=cw[:rows, gi, 0:1], scalar2=cw[:rows, gi, 3:4], op0=ALU.mult, op1=ALU.add),
                    reads=[Rg, cw], writes=[T])
                for tap in (1, 2):
                    P.op("dve", lambda v, Rg=Rg, T=T, rows=rows, n=n, gi=gi, tap=tap: v.scalar_tensor_tensor(
                        out=T[:rows, 0:n], in0=Rg[:rows, tap:tap + n], scalar=cw[:rows, gi, tap:tap + 1], in1=T[:rows, 0:n],
                        op0=ALU.mult, op1=ALU.add), reads=[Rg, cw, T], writes=[T])
            O = outs[gi]
            P.op("act", lambda a, O=O, T=T, rows=rows, n=n: a.activation(out=O[:rows, 0:n], in_=T[:rows, 0:n], func=AF.Silu),
                 reads=[T], writes=[O])
        XT, BTt, CTt, Y = outs[0], outs[1], outs[2], Yb[bi % 2]
        if _lvl <= 2 or (_lvl == 3 and bi > 0) or (_lvl == 4 and bi > 2):
            continue
        for k in range(nch):
            c = c0 + k
            sl = slice(k * 128, (k + 1) * 128)
            X, XD, XDD, BM = xtm[c % 2], xdt[c % 2], xdd[c % 2], btm[c % 2]
            P.op("pe", lambda pe, XT=XT, sl=sl: pe.transpose(pX[:, 0:64], XT[0:64, sl], identf[0:64, 0:64]), reads=[XT, identf], writes=[pX])
            P.op("act", lambda a, X=X: a.copy(out=X[:, :], in_=pX[:, 0:64]), reads=[pX], writes=[X])
            P.op("dve", lambda v, X=X, XD=XD, c=c: v.tensor_scalar(out=XD[:, :], in0=X[:, :], scalar1=dt[:, c:c + 1], scalar2=None, op0=ALU.mult),
                 reads=[X, dt], writes=[XD])
            P.op("dve", lambda v, X=X, XDD=XDD, c=c: v.tensor_scalar(out=XDD[:, :], in0=X[:, :], scalar1=dtd[:, c:c + 1], scalar2=None, op0=ALU.mult),
                 reads=[X, dtd], writes=[XDD])
            pBv = pB.h[:, 0:256].bitcast(BF16)
            P.op("pe", lambda pe, BTt=BTt, sl=sl, pBv=pBv: pe.transpose(pBv[:, 0:128], BTt[:, sl], identb[:, :]), reads=[BTt, identb], writes=[pB])
            P.op("act", lambda a, BM=BM, pBv=pBv: a.copy(out=BM[:, :], in_=pBv[:, 0:128]), reads=[pB], writes=[BM])
            AB = adtb[c % 2]
            P.op("pool", lambda g, AB=AB, c=c: g.tensor_copy(out=AB[:, :], in_=adt[:, c:c + 1].broadcast_to([128, 128])), reads=[adt], writes=[AB])
            P.op("pe", lambda pe, AB=AB: pe.matmul(pA[:, 0:128], lhsT=AB[:, :], rhs=triu[:, :], start=True, stop=False), reads=[AB, triu], writes=[pA])
            P.op("pe", lambda pe: pe.matmul(pA[:, 0:128], lhsT=identf[:, :], rhs=negm[:, :], start=False, stop=True), reads=[identf, negm], writes=[pA])
            L, M = Lt[c % 2], Mt[c % 2]
            P.op("act", lambda a, L=L, c=c: a.activation(out=L[:, :], in_=pA[:, 0:128], func=AF.Exp, bias=nacum[:, c:c + 1]),
                 reads=[pA, nacum], writes=[L])
            P.op("pe", lambda pe, BTt=BTt, CTt=CTt, sl=sl: pe.matmul(pG[:, 0:128], lhsT=BTt[:, sl], rhs=CTt[:, sl], start=True, stop=True),
                 reads=[BTt, CTt], writes=[pG])
            P.op("dve", lambda v, L=L, M=M: v.tensor_tensor(out=M[:, :], in0=pG[:, 0:128], in1=L[:, :], op=ALU.mult), reads=[pG, L], writes=[M])
            P.op("pe", lambda pe, M=M, XD=XD: pe.matmul(pY[:, 0:64], lhsT=M[:, :], rhs=XD[:, :], start=True, stop=True), reads=[M, XD], writes=[pY])
            YS = ysb[c % 2]
            P.op("act", lambda a, YS=YS: a.copy(out=YS[:, :], in_=pY[:, 0:64]), reads=[pY], writes=[YS])
            if c > 0:
                HP = Hb[(c - 1) % 3]
                P.op("pe", lambda pe, CTt=CTt, sl=sl, HP=HP: pe.matmul(pO[:, 0:64], lhsT=CTt[:, sl], rhs=HP[:, :], start=True, stop=True),
                     reads=[CTt, HP], writes=[pO])
                P.op("dve", lambda v, YS=YS, c=c: v.scalar_tensor_tensor(out=YS[:, :], in0=pO[:, 0:64], scalar=eacum[:, c:c + 1], in1=YS[:, :],
                                                                        op0=ALU.mult, op1=ALU.add), reads=[pO, eacum, YS], writes=[YS])
            P.op("dve", lambda v, YS=YS, X=X, Y=Y, k=k: v.scalar_tensor_tensor(out=Y[:, k, :], in0=X[:, :], scalar=scal[:, 4:5], in1=YS[:, :],
                                                                              op0=ALU.mult, op1=ALU.add), reads=[X, scal, YS], writes=[(Y, k)])
            P.op("pe", lambda pe, BM=BM, XDD=XDD: pe.matmul(pS[:, 0:64], lhsT=BM[:, :], rhs=XDD[:, :], start=True, stop=True),
                 reads=[BM, XDD], writes=[pS])
            P.op("dve", lambda v, c=c: v.scalar_tensor_tensor(out=Hf[:, :], in0=Hf[:, :], scalar=etot[:, c:c + 1], in1=pS[:, 0:64],
                                                             op0=ALU.mult, op1=ALU.add), reads=[Hf, etot, pS], writes=[Hf])
            HN = Hb[c % 3]
            P.op("pool", lambda g, HN=HN: g.tensor_copy(out=HN[:, :], in_=Hf[:, :]), reads=[Hf], writes=[HN])
        P.dma("pool", io["y"][c0 * 128:(c0 + nch) * 128, :].rearrange("(k p) e -> p k e", p=128), Y[:, 0:nch, :], reads=[Y], is_output=True)


def ssd_consts():
    s = np.arange(128)[:, None]
    l = np.arange(128)[None, :]
    return {"triu": (s <= l).astype(np.float32), "ones": np.ones((128, 128), np.float32),
            "identf": np.eye(128, dtype=np.float32), "negm": np.where(s > l, -30000.0, 0.0).astype(np.float32),
            "identb": np.eye(128).astype(ml_dtypes.bfloat16)}


def ssd_io(nc):
    io = {"xbcT": _din(nc, "s_xbcT", [320, XPAD]), "convw": _din(nc, "s_convw", [320, 4]), "dtraw": _din(nc, "s_dtraw", [128, NKC]),
          "scal": _din(nc, "s_scal", [3]), "y": _dout(nc, "s_y", [NTOK, 64])}
    for nm in ("triu", "ones", "identf", "negm"):
        io[nm] = _din(nc, "s_" + nm, [128, 128])
    io["identb"] = _din(nc, "s_identb", [128, 128], BF16)
    return io


def ssd_inputs(u_glob, p, i):
    h, d = i % 4, i // 4
    g = h // 2
    cols = np.concatenate([np.arange(C_SX + h * 64, C_SX + (h + 1) * 64),
                           np.arange(C_SX + 256 + g * 128, C_SX + 256 + (g + 1) * 128),
                           np.arange(C_SX + 512 + g * 128, C_SX + 512 + (g + 1) * 128)])
    uc, ul = u_glob[:CTX], u_glob[CTX:]
    taps = [0, 1, 2]
    if d == 1:
        uc, ul = uc[::-1], ul[::-1]
        taps = [2, 1, 0]
    xp = np.zeros((320, XPAD), np.float32)
    xp[:, 1:1 + CTX] = uc[:, cols].T
    xp[:, 3 + CTX:3 + CTX + SEQ] = ul[:, cols].T
    ccols = cols - C_SX
    convw = np.stack([p["ssd_conv_w"][taps[0], ccols], p["ssd_conv_w"][taps[1], ccols], p["ssd_conv_w"][taps[2], ccols],
                      p["ssd_conv_b"][ccols]], axis=1).astype(np.float32)
    dcol = C_SDT + d * 4 + h
    dts = np.concatenate([uc[:, dcol], ul[:, dcol]])
    dtraw = np.ascontiguousarray(dts.reshape(NKC, 128).T)
    scal = np.array([p["ssd_dt_bias"][d, h], p["ssd_a_log"][d, h], p["ssd_d"][h]], np.float32)
    m = {"s_xbcT": xp, "s_convw": np.ascontiguousarray(convw), "s_dtraw": dtraw, "s_scal": scal}
    for k, v in ssd_consts().items():
        m["s_" + k] = v
    return m


def build_phase_b_ssd():
    nc = _new_nc()
    io = ssd_io(nc)
    with ExitStack() as st:
        P = Prog(nc, st)
        emit_ssd(P, io)
        P.finish()
    return nc


I32 = mybir.dt.int32
TWO_PI = 2.0 * math.pi
PI_SAFE = 3.1415925


def _sincos(P, ang, n, sin_t, cos_t, tmpf, tmpi, msk):
    A = ang
    P.op("dve", lambda v: v.tensor_scalar(out=tmpf[:, :n], in0=A[:, :n], scalar1=1.0 / TWO_PI, scalar2=None, op0=ALU.mult),
         reads=[A], writes=[tmpf])
    P.op("dve", lambda v: v.tensor_copy(out=tmpi[:, :n], in_=tmpf[:, :n]), reads=[tmpf], writes=[tmpi])
    P.op("dve", lambda v: v.tensor_copy(out=tmpf[:, :n], in_=tmpi[:, :n]), reads=[tmpi], writes=[tmpf])
    P.op("dve", lambda v: v.scalar_tensor_tensor(out=sin_t[:, :n], in0=tmpf[:, :n], scalar=-TWO_PI, in1=A[:, :n], op0=ALU.mult, op1=ALU.add),
         reads=[tmpf, A], writes=[sin_t])

    def wrap(T):
        P.op("dve", lambda v: v.tensor_scalar(out=msk[:, :n], in0=T[:, :n], scalar1=math.pi, scalar2=-TWO_PI, op0=ALU.is_gt, op1=ALU.mult),
             reads=[T], writes=[msk])
        P.op("dve", lambda v: v.tensor_tensor(out=T[:, :n], in0=T[:, :n], in1=msk[:, :n], op=ALU.add), reads=[T, msk], writes=[T])
        P.op("dve", lambda v: v.tensor_scalar(out=msk[:, :n], in0=T[:, :n], scalar1=-math.pi, scalar2=TWO_PI, op0=ALU.is_lt, op1=ALU.mult),
             reads=[T], writes=[msk])
        P.op("dve", lambda v: v.tensor_tensor(out=T[:, :n], in0=T[:, :n], in1=msk[:, :n], op=ALU.add), reads=[T, msk], writes=[T])
        P.op("dve", lambda v: v.tensor_scalar(out=T[:, :n], in0=T[:, :n], scalar1=PI_SAFE, scalar2=-PI_SAFE, op0=ALU.min, op1=ALU.max),
             reads=[T], writes=[T])
    wrap(sin_t)
    P.op("dve", lambda v: v.tensor_scalar(out=cos_t[:, :n], in0=sin_t[:, :n], scalar1=math.pi / 2, scalar2=None, op0=ALU.add),
         reads=[sin_t], writes=[cos_t])
    wrap(cos_t)
    P.op("act", lambda a: a.activation(out=sin_t[:, :n], in_=sin_t[:, :n], func=AF.Sin), reads=[sin_t], writes=[sin_t])
    P.op("act", lambda a: a.activation(out=cos_t[:, :n], in_=cos_t[:, :n], func=AF.Sin), reads=[cos_t], writes=[cos_t])


S5L = 512


def emit_s5(P, io):
    identf = P.sb([128, 128], F32)
    P.dma("sp", identf[:, :], io["identf"], writes=[identf])
    iot = P.sb([128, S5L], F32)
    P.dma("sp", iot[:, :], io["iota"].partition_broadcast(128), writes=[iot])
    dsk = P.sb([32, 1], F32)
    P.dma("sp", dsk[:, :], io["dskip"], writes=[dsk])
    Yacc = P.sb([32, NTOK], F32)
    prm = P.sb([128, 2, 4], F32)
    P.dma("sp", prm[:, :, :], io["prm"], writes=[prm])
    Bre = P.sb([128, 32], F32)
    Bim = P.sb([128, 32], F32)
    P.dma("sp", Bre[:, :], io["bre"], writes=[Bre])
    P.dma("sp", Bim[:, :], io["bim"], writes=[Bim])
    tf = P.sb([128, S5L], F32)
    ti = P.sb([128, S5L], I32)
    mk = P.sb([128, S5L], F32)
    sc = P.sb([128, 24], F32)
    ang = P.sb([128, S5L], F32)
    pbu = [[P.ps([128, 512]) for _ in range(2)] for _ in range(2)]
    py = [P.ps([128, 512]) for _ in range(2)]
    pT = P.ps([128, 512])
    uT = [P.sb([32, S5L], F32) for _ in range(3)]
    t1 = [P.sb([128, S5L], F32) for _ in range(2)]
    t2 = [P.sb([128, S5L], F32) for _ in range(2)]
    bre_t = [P.sb([128, S5L], F32) for _ in range(2)]
    bim_t = [P.sb([128, S5L], F32) for _ in range(2)]
    gre = [P.sb([128, S5L], F32) for _ in range(2)]
    gim = [P.sb([128, S5L], F32) for _ in range(2)]
    hre = [P.sb([128, S5L], F32) for _ in range(2)]
    him = [P.sb([128, S5L], F32) for _ in range(2)]
    Rt = P.sb([128, S5L], F32)
    COS = P.sb([128, S5L], F32)
    SIN = P.sb([128, S5L], F32)
    BbT = [P.sb([32, 128], F32) for _ in range(2)]
    Bb = [P.sb([128, 32], F32) for _ in range(2)]
    CT = [P.sb([128, 32], F32) for _ in range(2)]
    h0 = P.sb([128, 2], F32)
    th = P.sb([128, 1], F32)
    sn = P.sb([128, 1], F32)
    cs = P.sb([128, 1], F32)
    ts = lambda o, i, s1, s2, o0, o1=None, rd=(), wr=(): P.op(
        "dve", lambda v: v.tensor_scalar(out=o, in0=i, scalar1=s1, scalar2=s2, op0=o0, **({"op1": o1} if o1 is not None else {})),
        reads=list(rd), writes=list(wr))
    tt = lambda o, a, b, op, rd=(), wr=(), e="dve": P.op(e, lambda v: v.tensor_tensor(out=o, in0=a, in1=b, op=op), reads=list(rd), writes=list(wr))
    chunks = [(0, CTX)] + [(CTX + i * S5L, S5L) for i in range(SEQ // S5L)]
    nchunk = 0
    for d in range(2):
        c = lambda j: sc[:, j:j + 1]
        P.op("act", lambda a: a.activation(out=c(0), in_=prm[:, d, 2:3], func=AF.Exp), reads=[prm], writes=[sc])
        tt(c(1), prm[:, d, 0:1], c(0), ALU.mult, [prm, sc], [sc])
        P.op("act", lambda a: a.activation(out=c(2), in_=c(1), func=AF.Exp), reads=[sc], writes=[sc])
        tt(th[:, 0:1], prm[:, d, 1:2], c(0), ALU.mult, [prm, sc], [th])
        _sincos(P, th, 1, sn, cs, tf, ti, mk)
        tt(c(6), c(2), cs[:, 0:1], ALU.mult, [sc, cs], [sc])
        tt(c(7), c(2), sn[:, 0:1], ALU.mult, [sc, sn], [sc])
        ts(c(6), c(6), -1.0, None, ALU.add, rd=[sc], wr=[sc])
        tt(c(8), prm[:, d, 0:1], prm[:, d, 0:1], ALU.mult, [prm], [sc])
        tt(c(9), prm[:, d, 1:2], prm[:, d, 1:2], ALU.mult, [prm], [sc])
        tt(c(8), c(8), c(9), ALU.add, [sc], [sc])
        P.op("dve", lambda v: v.reciprocal(out=c(8), in_=c(8)), reads=[sc], writes=[sc])
        tt(c(10), c(6), prm[:, d, 0:1], ALU.mult, [sc, prm], [sc])
        tt(c(11), c(7), prm[:, d, 1:2], ALU.mult, [sc, prm], [sc])
        tt(c(10), c(10), c(11), ALU.add, [sc], [sc])
        tt(c(10), c(10), c(8), ALU.mult, [sc], [sc])
        tt(c(12), c(7), prm[:, d, 0:1], ALU.mult, [sc, prm], [sc])
        tt(c(13), c(6), prm[:, d, 1:2], ALU.mult, [sc, prm], [sc])
        tt(c(12), c(12), c(13), ALU.subtract, [sc], [sc])
        tt(c(12), c(12), c(8), ALU.mult, [sc], [sc])
        ts(c(14), c(12), -1.0, None, ALU.mult, rd=[sc], wr=[sc])
        ts(Bb[0][:, :], Bre[:, :], c(10), None, ALU.mult, rd=[Bre, sc], wr=[Bb[0]])
        P.op("dve", lambda v: v.scalar_tensor_tensor(out=Bb[0][:, :], in0=Bim[:, :], scalar=c(14), in1=Bb[0][:, :], op0=ALU.mult, op1=ALU.add),
             reads=[Bim, sc, Bb[0]], writes=[Bb[0]])
        ts(Bb[1][:, :], Bim[:, :], c(10), None, ALU.mult, rd=[Bim, sc], wr=[Bb[1]])
        P.op("dve", lambda v: v.scalar_tensor_tensor(out=Bb[1][:, :], in0=Bre[:, :], scalar=c(12), in1=Bb[1][:, :], op0=ALU.mult, op1=ALU.add),
             reads=[Bre, sc, Bb[1]], writes=[Bb[1]])
        for q in range(2):
            P.op("pe", lambda pe, q=q: pe.transpose(pT[0:32, 0:128], Bb[q][:, :], identf[:, :]), reads=[Bb[q], identf], writes=[pT])
            P.op("act", lambda a, q=q: a.copy(out=BbT[q][:, :], in_=pT[0:32, 0:128]), reads=[pT], writes=[BbT[q]])
        P.dma("sp", CT[0][:, :], io["ctre"][d], writes=[CT[0]])
        P.dma("sp", CT[1][:, :], io["ctim"][d], writes=[CT[1]])
        ts(CT[1][:, :], CT[1][:, :], -1.0, None, ALU.mult, rd=[CT[1]], wr=[CT[1]])
        ts(ang[:, :], iot[:, :], th[:, 0:1], None, ALU.mult, rd=[iot, th], wr=[ang])
        _sincos(P, ang, S5L, SIN, COS, tf, ti, mk)
        ts(Rt[:, :], iot[:, :], 0.0, c(2), ALU.mult, ALU.add, rd=[iot, sc], wr=[Rt])
        P.op("dve", lambda v: v.memset(h0[:, :], 0.0), writes=[h0])
        prev = (h0[:, 0:1], h0[:, 1:2], [h0])
        src = io["uT"][d]
        for ci, (t0, n) in enumerate(chunks):
            k = nchunk % 2
            nchunk += 1
            U = uT[nchunk % 3]
            P.dma("sp", U[:, :n], src[:, t0:t0 + n], writes=[U])
            pr, pi = pbu[k]
            P.op("pe", lambda pe, U=U, pr=pr, n=n: pe.matmul(pr[:, :n], lhsT=BbT[0][:, :], rhs=U[:, :n], start=True, stop=True),
                 reads=[BbT[0], U], writes=[pr])
            P.op("pe", lambda pe, U=U, pi=pi, n=n: pe.matmul(pi[:, :n], lhsT=BbT[1][:, :], rhs=U[:, :n], start=True, stop=True),
                 reads=[BbT[1], U], writes=[pi])
            T1, T2, BR, BI, GR, GI, HR, HI = t1[k], t2[k], bre_t[k], bim_t[k], gre[k], gim[k], hre[k], him[k]
            tt(T1[:, :n], pr[:, :n], COS[:, :n], ALU.mult, [pr, COS], [T1])
            tt(T2[:, :n], pi[:, :n], SIN[:, :n], ALU.mult, [pi, SIN], [T2])
            tt(BR[:, :n], T1[:, :n], T2[:, :n], ALU.add, [T1, T2], [BR])
            tt(T1[:, :n], pi[:, :n], COS[:, :n], ALU.mult, [pi, COS], [T1])
            tt(T2[:, :n], pr[:, :n], SIN[:, :n], ALU.mult, [pr, SIN], [T2])
            tt(BI[:, :n], T1[:, :n], T2[:, :n], ALU.subtract, [T1, T2], [BI])
            P.op("dve", lambda v, GR=GR, BR=BR, n=n, prev=prev: v.tensor_tensor_scan(
                out=GR[:, :n], data0=Rt[:, :n], data1=BR[:, :n], initial=prev[0], op0=ALU.mult, op1=ALU.add),
                reads=[Rt, BR] + prev[2], writes=[GR])
            P.op("dve", lambda v, GI=GI, BI=BI, n=n, prev=prev: v.tensor_tensor_scan(
                out=GI[:, :n], data0=Rt[:, :n], data1=BI[:, :n], initial=prev[1], op0=ALU.mult, op1=ALU.add),
                reads=[Rt, BI] + prev[2], writes=[GI])
            tt(T1[:, :n], GR[:, :n], COS[:, :n], ALU.mult, [GR, COS], [T1])
            tt(T2[:, :n], GI[:, :n], SIN[:, :n], ALU.mult, [GI, SIN], [T2])
            tt(HR[:, :n], T1[:, :n], T2[:, :n], ALU.subtract, [T1, T2], [HR])
            tt(T1[:, :n], GR[:, :n], SIN[:, :n], ALU.mult, [GR, SIN], [T1])
            tt(T2[:, :n], GI[:, :n], COS[:, :n], ALU.mult, [GI, COS], [T2])
            tt(HI[:, :n], T1[:, :n], T2[:, :n], ALU.add, [T1, T2], [HI])
            PY = py[k]
            P.op("pe", lambda pe, PY=PY, HR=HR, n=n: pe.matmul(PY[0:32, :n], lhsT=CT[0][:, :], rhs=HR[:, :n], start=True, stop=False),
                 reads=[CT[0], HR], writes=[PY])
            P.op("pe", lambda pe, PY=PY, HI=HI, n=n: pe.matmul(PY[0:32, :n], lhsT=CT[1][:, :], rhs=HI[:, :n], start=False, stop=True),
                 reads=[CT[1], HI], writes=[PY])
            if d == 0:
                P.op("act", lambda a, PY=PY, t0=t0, n=n: a.copy(out=Yacc[:, t0:t0 + n], in_=PY[0:32, :n]), reads=[PY], writes=[(Yacc, ci)])
            else:
                if ci == 0:
                    lo = 0
                else:
                    lo = CTX + SEQ - (t0 - CTX) - n
                dst = Yacc[:, lo:lo + n][:, ::-1]
                cj = 0 if ci == 0 else len(chunks) - ci
                P.op("dve", lambda v, PY=PY, dst=dst, n=n: v.tensor_tensor(out=dst, in0=PY[0:32, :n], in1=dst, op=ALU.add),
                     reads=[PY, (Yacc, cj)], writes=[(Yacc, cj)])
            prev = (HR[:, n - 1:n], HI[:, n - 1:n], [HR, HI])
    GW = 2048
    ub = [P.sb([32, GW], F32) for _ in range(2)]
    g1 = [P.sb([32, GW], F32) for _ in range(2)]
    g2 = [P.sb([32, GW], F32) for _ in range(2)]
    for gi, t0 in enumerate(range(0, NTOK, GW)):
        n = min(GW, NTOK - t0)
        UB, G1, G2 = ub[gi % 2], g1[gi % 2], g2[gi % 2]
        P.dma("sp", UB[:, :n], io["uT"][0][:, t0:t0 + n], writes=[UB])
        Y = Yacc[:, t0:t0 + n]
        P.op("dve", lambda v, UB=UB, Y=Y, n=n: v.scalar_tensor_tensor(out=Y, in0=UB[:, :n], scalar=dsk[:, 0:1], in1=Y, op0=ALU.mult, op1=ALU.add),
             reads=[UB, dsk, Yacc], writes=[Yacc])
        P.op("dve", lambda g, Y=Y, G1=G1, n=n: g.tensor_tensor(out=G1[:, :n], in0=Y, in1=Y, op=ALU.mult), reads=[Yacc], writes=[G1])
        P.op("dve", lambda v, G1=G1, n=n: v.tensor_scalar(out=G1[:, :n], in0=G1[:, :n], scalar1=0.044715, scalar2=1.0, op0=ALU.mult, op1=ALU.add),
             reads=[G1], writes=[G1])
        P.op("dve", lambda g, Y=Y, G1=G1, n=n: g.tensor_tensor(out=G1[:, :n], in0=G1[:, :n], in1=Y, op=ALU.mult), reads=[Yacc, G1], writes=[G1])
        P.op("act", lambda a, G1=G1, G2=G2, n=n: a.activation(out=G2[:, :n], in_=G1[:, :n], func=AF.Sigmoid, scale=1.5957691216057308),
             reads=[G1], writes=[G2])
        P.op("dve", lambda v, Y=Y, G2=G2, n=n: v.tensor_tensor(out=G2[:, :n], in0=G2[:, :n], in1=Y, op=ALU.mult), reads=[Yacc, G2], writes=[G2])
        P.dma("pool", io["yT"][:, t0:t0 + n], G2[:, :n], reads=[G2], is_output=True)


def s5_io(nc):
    return {"identf": _din(nc, "c_identf", [128, 128]), "iota": _din(nc, "c_iota", [S5L]), "dskip": _din(nc, "c_dskip", [32, 1]),
            "prm": _din(nc, "c_prm", [128, 2, 4]), "bre": _din(nc, "c_bre", [128, 32]), "bim": _din(nc, "c_bim", [128, 32]),
            "ctre": _din(nc, "c_ctre", [2, 128, 32]), "ctim": _din(nc, "c_ctim", [2, 128, 32]),
            "uT": _din(nc, "c_uT", [2, 32, NTOK]), "yT": _dout(nc, "c_yT", [32, NTOK])}


def s5_inputs(u_glob, p, i):
    g0 = 2 * i
    cols = np.arange(C_S5 + g0 * 16, C_S5 + g0 * 16 + 32)
    uf = np.ascontiguousarray(u_glob[:, cols].T)
    ub = np.concatenate([uf[:, :CTX][:, ::-1], uf[:, CTX:][:, ::-1]], axis=1)
    prm = np.zeros((128, 2, 4), np.float32)
    bre = np.zeros((128, 32), np.float32)
    bim = np.zeros((128, 32), np.float32)
    ctre = np.zeros((2, 128, 32), np.float32)
    ctim = np.zeros((2, 128, 32), np.float32)
    for uidx in range(2):
        g = g0 + uidx
        rows = slice(uidx * 64, (uidx + 1) * 64)
        cc = slice(uidx * 16, (uidx + 1) * 16)
        bre[rows, cc] = p["s5_b_re"][g]
        bim[rows, cc] = p["s5_b_im"][g]
        for d in range(2):
            prm[rows, d, 0] = p["s5_lam_re"][d, g]
            prm[rows, d, 1] = p["s5_lam_im"][d, g]
            prm[rows, d, 2] = p["s5_log_dt"][d, g]
            ctre[d, rows, cc] = p["s5_c_re"][d, g].T
            ctim[d, rows, cc] = p["s5_c_im"][d, g].T
    return {"c_identf": np.eye(128, dtype=np.float32), "c_iota": np.arange(1, S5L + 1, dtype=np.float32),
            "c_dskip": np.ascontiguousarray(p["s5_d"][g0 * 16:g0 * 16 + 32].reshape(32, 1)), "c_prm": prm, "c_bre": bre, "c_bim": bim,
            "c_ctre": ctre, "c_ctim": ctim, "c_uT": np.ascontiguousarray(np.stack([uf, ub]))}


def build_phase_b_s5():
    nc = _new_nc()
    io = s5_io(nc)
    with ExitStack() as st:
        P = Prog(nc, st)
        emit_s5(P, io)
        P.finish()
    return nc


def build_phase_c():
    nc = _new_nc()
    x = _din(nc, "x", [TOK_PC, D])
    ad = _din(nc, "ad", [TOK_PC, 256])
    ag = _din(nc, "ag", [TOK_PC, 256])
    ysf = _din(nc, "ysf", [TOK_PC, 256])
    ysb = _din(nc, "ysb", [TOK_PC, 256])
    zz = _din(nc, "zz", [TOK_PC, 256])
    s5T = _din(nc, "s5T", [256, TOK_PC])
    modl = _din(nc, "modl", [6 * D])
    modc = _din(nc, "modc", [6 * D])
    n2w = _din(nc, "n2w", [D])
    snw = _din(nc, "snw", [256])
    wglu = _din(nc, "wglu", [256, 256])
    bglu = _din(nc, "bglu", [128, 2])
    wout = _din(nc, "wout", [D, D])
    wr = _din(nc, "wr", [D, 32])
    br = _din(nc, "br", [32])
    identb_d = _din(nc, "identb", [128, 128], BF16)
    identf_d = _din(nc, "identf", [128, 128])
    x1_out = _dout(nc, "x1", [TOK_PC, D])
    hmT_out = _dout(nc, "hmT", [128, 8, TOK_PC], BF16)
    gates_out = _dout(nc, "gates", [TOK_PC, 32])
    with ExitStack() as st:
        P = Prog(nc, st)
        idb = P.sb([128, 128], BF16)
        idf = P.sb([128, 128], F32)
        P.dma("sp", idb[:, :], identb_d, writes=[idb])
        P.dma("sp", idf[:, :], identf_d, writes=[idf])
        g1 = [P.sb([128, D], F32) for _ in range(2)]
        w2 = [P.sb([128, D], F32) for _ in range(2)]
        sh2 = [P.sb([128, D], F32) for _ in range(2)]
        n2 = P.sb([128, D], F32)
        _load_bcast(P, "sp", n2, n2w, D)
        for i, m in enumerate((modl, modc)):
            _load_bcast(P, "sp", g1[i], m[2 * D:3 * D], D)
            _load_bcast(P, "sp", sh2[i], m[3 * D:4 * D], D)
            _load_bcast(P, "sp", w2[i], m[4 * D:5 * D], D)
            P.op("dve", lambda v, i=i: v.scalar_tensor_tensor(out=w2[i][:, :], in0=w2[i][:, :], scalar=1.0, in1=n2[:, :],
                                                              op0=ALU.add, op1=ALU.mult), reads=[w2[i], n2], writes=[w2[i]])
        snwt = P.sb([128, 256], F32)
        _load_bcast(P, "sp", snwt, snw, 256)
        brt = P.sb([128, 32], F32)
        _load_bcast(P, "sp", brt, br, 32)
        wg = P.sb([128, 2, 256], F32)
        P.dma("sp", wg[:, :, :], wglu.rearrange("(k p) n -> p k n", p=128), writes=[wg])
        bg = P.sb([128, 2], F32)
        P.dma("sp", bg[:, :], bglu, writes=[bg])
        wrt = P.sb([128, 8, 32], F32)
        P.dma("sp", wrt[:, :, :], wr.rearrange("(k p) n -> p k n", p=128), writes=[wrt])
        wob = P.sb([128, 8, D], BF16)
        stg = [P.sb([128, D], F32) for _ in range(2)]
        for k in range(8):
            s = stg[k % 2]
            P.dma("sp", s[:, :], wout[k * 128:(k + 1) * 128, :], writes=[s])
            P.op("pool", lambda g, k=k, s=s: g.tensor_copy(out=wob[:, k, :], in_=s[:, :]), reads=[s], writes=[(wob, k)])

        NB = 2
        xt = [P.sb([128, D], F32) for _ in range(NB)]
        mixin = [P.sb([128, 5, 256], F32) for _ in range(NB)]
        s5t = [P.sb([128, 2, 128], F32) for _ in range(NB)]
        mix = [P.sb([128, 768], BF16) for _ in range(NB)]
        mixT = [P.sb([128, 8, 128], BF16) for _ in range(NB)]
        sg = [P.sb([128, 256], F32) for _ in range(NB)]
        sq = [P.sb([128, 256], F32) for _ in range(NB)]
        st8 = [P.sb([128, 8], F32) for _ in range(NB)]
        x1t = [P.sb([128, D], F32) for _ in range(NB)]
        junk = P.sb([128, D], F32)
        hmf = [P.sb([128, D], F32) for _ in range(NB)]
        hmb = [P.sb([128, D], BF16) for _ in range(NB)]
        hmTb = [P.sb([128, 8, 128], BF16) for _ in range(NB)]
        hmTf = [P.sb([128, 8, 128], F32) for _ in range(NB)]
        sgl = [P.sb([128, 2, 128], F32) for _ in range(NB)]
        lg = [P.sb([128, 32], F32) for _ in range(NB)]
        t8 = [P.sb([128, 8], F32) for _ in range(NB)]
        msk = [P.sb([128, 32], F32) for _ in range(NB)]
        gt = [P.sb([128, 32], F32) for _ in range(NB)]
        pT = P.ps([128, 8, 128], BF16)
        pTf = [P.ps([128, 4, 128]) for _ in range(2)]
        pgl = P.ps([128, 512])
        po = [P.ps([128, 512]) for _ in range(2)]
        plg = P.ps([128, 512])
        for t in range(NT):
            r = 128 if t < 16 else CTX_PC
            ic = 0 if t < 16 else 1
            b = t % NB
            rows = slice(t * 128, t * 128 + r)
            X, MI, S5, MX, MT = xt[b], mixin[b], s5t[b], mix[b], mixT[b]
            P.dma("sp", X[:r, :], x[rows, :], writes=[X])
            for j, src in enumerate((ad, ag, ysf, ysb, zz)):
                P.dma("sp", MI[:r, j, :], src[rows, :], writes=[(MI, j)])
            P.dma("sp", S5[:, :, :r], s5T[:, t * 128:t * 128 + r].rearrange("(k p) n -> p k n", p=128), writes=[S5])
            SG, SQ, S8 = sg[b], sq[b], st8[b]
            P.op("act", lambda a, MI=MI, SG=SG, r=r: a.activation(out=SG[:r, :], in_=MI[:r, 4, :], func=AF.Silu), reads=[(MI, 4)], writes=[SG])
            P.op("dve", lambda g, MI=MI, SQ=SQ, r=r: g.tensor_tensor(out=SQ[:r, :], in0=MI[:r, 2, :], in1=MI[:r, 3, :], op=ALU.add),
                 reads=[(MI, 2), (MI, 3)], writes=[SQ])
            P.op("dve", lambda v, SG=SG, SQ=SQ, r=r: v.tensor_tensor(out=SG[:r, :], in0=SG[:r, :], in1=SQ[:r, :], op=ALU.mult),
                 reads=[SG, SQ], writes=[SG])
            P.op("act", lambda a, SG=SG, SQ=SQ, S8=S8, r=r: a.activation(out=SQ[:r, :], in_=SG[:r, :], func=AF.Square, accum_out=S8[:r, 0:1]),
                 reads=[SG], writes=[SQ, S8])
            P.op("dve", lambda v, S8=S8, r=r: v.tensor_scalar(out=S8[:r, 1:2], in0=S8[:r, 0:1], scalar1=1.0 / 256, scalar2=EPS, op0=ALU.mult, op1=ALU.add),
                 reads=[S8], writes=[S8])
            P.op("act", lambda a, S8=S8, r=r: a.activation(out=S8[:r, 1:2], in_=S8[:r, 1:2], func=AF.Sqrt), reads=[S8], writes=[S8])
            P.op("dve", lambda v, S8=S8, r=r: v.reciprocal(out=S8[:r, 1:2], in_=S8[:r, 1:2]), reads=[S8], writes=[S8])
            P.op("dve", lambda v, SG=SG, S8=S8, MX=MX, r=r: v.scalar_tensor_tensor(
                out=MX[:r, 256:512], in0=SG[:r, :], scalar=S8[:r, 1:2], in1=snwt[:r, :], op0=ALU.mult, op1=ALU.mult),
                reads=[SG, S8, snwt], writes=[(MX, 1)])
            P.op("dve", lambda g, MI=MI, MX=MX, r=r: g.tensor_copy(out=MX[:r, 0:256], in_=MI[:r, 0, :]), reads=[(MI, 0)], writes=[(MX, 0)])
            P.op("dve", lambda g, MI=MI, MX=MX, r=r: g.tensor_copy(out=MX[:r, 512:768], in_=MI[:r, 1, :]), reads=[(MI, 1)], writes=[(MX, 2)])
            for j, kk in enumerate((0, 1, 2, 3, 6, 7)):
                P.op("pe", lambda pe, j=j, kk=kk, MX=MX, r=r: pe.transpose(pT[:, kk, :r], MX[:r, j * 128:(j + 1) * 128], idb[:r, :r]),
                     reads=[MX, idb], writes=[pT])
            P.op("act", lambda a, MT=MT, r=r: a.copy(out=MT[:, 0:4, :r], in_=pT[:, 0:4, :r]), reads=[pT], writes=[(MT, 0)])
            P.op("act", lambda a, MT=MT, r=r: a.copy(out=MT[:, 6:8, :r], in_=pT[:, 6:8, :r]), reads=[pT], writes=[(MT, 2)])
            SL = sgl[b]
            for fo in range(2):
                for k in range(2):
                    P.op("pe", lambda pe, fo=fo, k=k, S5=S5, r=r: pe.matmul(pgl[:, fo * 128:fo * 128 + r], lhsT=wg[:, k, fo * 128:(fo + 1) * 128],
                                                                         rhs=S5[:, k, :r], start=(k == 0), stop=(k == 1)),
                         reads=[wg, S5], writes=[pgl])
                P.op("act", lambda a, fo=fo, SL=SL, r=r: a.activation(out=SL[:, fo, :r], in_=pgl[:, fo * 128:fo * 128 + r], func=AF.Sigmoid,
                                                                      bias=bg[:, fo:fo + 1]), reads=[pgl, bg], writes=[SL])
            P.op("dve", lambda v, SL=SL, S5=S5, MT=MT, r=r: v.tensor_tensor(out=MT[:, 4:6, :r], in0=SL[:, :, :r], in1=S5[:, :, :r], op=ALU.mult),
                 reads=[SL, S5], writes=[(MT, 1)])
            X1 = x1t[b]
            for ct in range(2):
                ps = po[ct]
                for k in range(8):
                    P.op("pe", lambda pe, ps=ps, k=k, ct=ct, MT=MT, r=r: pe.matmul(ps[:r, :], lhsT=MT[:, k, :r], rhs=wob[:, k, ct * 512:(ct + 1) * 512],
                                                                               start=(k == 0), stop=(k == 7)),
                         reads=[MT, (wob, k)], writes=[ps])
                cs = slice(ct * 512, (ct + 1) * 512)
                P.op("dve", lambda v, ps=ps, cs=cs, X1=X1, r=r, ic=ic: v.tensor_tensor(out=X1[:r, cs], in0=ps[:r, :], in1=g1[ic][:r, cs], op=ALU.mult),
                     reads=[ps, g1[ic]], writes=[(X1, ct)])
                P.op("dve", lambda g, cs=cs, X1=X1, X=X, r=r: g.tensor_tensor(out=X1[:r, cs], in0=X1[:r, cs], in1=X[:r, cs], op=ALU.add),
                     reads=[(X1, ct), X], writes=[(X1, ct)])
            P.dma("pool", x1_out[rows, :], X1[:r, :], reads=[X1], is_output=True)
            HF, HB = hmf[b], hmb[b]
            P.op("act", lambda a, X1=X1, S8=S8, r=r: a.activation(out=junk[:r, :], in_=X1[:r, :], func=AF.Square, accum_out=S8[:r, 2:3]),
                 reads=[X1], writes=[junk, S8])
            P.op("dve", lambda v, S8=S8, r=r: v.tensor_scalar(out=S8[:r, 3:4], in0=S8[:r, 2:3], scalar1=1.0 / D, scalar2=EPS, op0=ALU.mult, op1=ALU.add),
                 reads=[S8], writes=[S8])
            P.op("act", lambda a, S8=S8, r=r: a.activation(out=S8[:r, 3:4], in_=S8[:r, 3:4], func=AF.Sqrt), reads=[S8], writes=[S8])
            P.op("dve", lambda v, S8=S8, r=r: v.reciprocal(out=S8[:r, 3:4], in_=S8[:r, 3:4]), reads=[S8], writes=[S8])
            P.op("dve", lambda v, X1=X1, S8=S8, HF=HF, r=r, ic=ic: v.scalar_tensor_tensor(
                out=HF[:r, :], in0=X1[:r, :], scalar=S8[:r, 3:4], in1=w2[ic][:r, :], op0=ALU.mult, op1=ALU.mult),
                reads=[X1, S8, w2[ic]], writes=[HF])
            P.op("dve", lambda g, HF=HF, r=r, ic=ic: g.tensor_tensor(out=HF[:r, :], in0=HF[:r, :], in1=sh2[ic][:r, :], op=ALU.add),
                 reads=[HF, sh2[ic]], writes=[HF])
            P.op("dve", lambda g, HF=HF, HB=HB, r=r: g.tensor_copy(out=HB[:r, :], in_=HF[:r, :]), reads=[HF], writes=[HB])
            HTB, HTF = hmTb[b], hmTf[b]
            for k in range(8):
                P.op("pe", lambda pe, k=k, HB=HB, r=r: pe.transpose(pT[:, k, :r], HB[:r, k * 128:(k + 1) * 128], idb[:r, :r]),
                     reads=[HB, idb], writes=[pT])
            P.op("act", lambda a, HTB=HTB, r=r: a.copy(out=HTB[:, :, :r], in_=pT[:, :, :r]), reads=[pT], writes=[HTB])
            P.dma("pool", hmT_out[:, :, t * 128:t * 128 + r], HTB[:, :, :r], reads=[HTB], is_output=True)
            for k in range(8):
                pp = pTf[k // 4]
                P.op("pe", lambda pe, k=k, pp=pp, HF=HF, r=r: pe.transpose(pp[:, k % 4, :r], HF[:r, k * 128:(k + 1) * 128], idf[:r, :r]),
                     reads=[HF, idf], writes=[pp])
            for hh in range(2):
                P.op("dve", lambda v, hh=hh, HTF=HTF, r=r: v.tensor_copy(out=HTF[:, hh * 4:(hh + 1) * 4, :r], in_=pTf[hh][:, :, :r]),
                     reads=[pTf[hh]], writes=[(HTF, hh)])
            for k in range(8):
                P.op("pe", lambda pe, k=k, HTF=HTF, r=r: pe.matmul(plg[:r, 0:32], lhsT=HTF[:, k, :r], rhs=wrt[:, k, :], start=(k == 0), stop=(k == 7)),
                     reads=[HTF, wrt], writes=[plg])
            LG, T8, MK, GT = lg[b], t8[b], msk[b], gt[b]
            P.op("dve", lambda v, LG=LG, r=r: v.tensor_tensor(out=LG[:r, :], in0=plg[:r, 0:32], in1=brt[:r, :], op=ALU.add), reads=[plg, brt], writes=[LG])
            P.op("dve", lambda v, LG=LG, T8=T8, r=r: v.max(out=T8[:r, :], in_=LG[:r, :]), reads=[LG], writes=[T8])
            P.op("dve", lambda v, LG=LG, T8=T8, MK=MK, r=r: v.tensor_scalar(out=MK[:r, :], in0=LG[:r, :], scalar1=T8[:r, 3:4], scalar2=None, op0=ALU.is_ge),
                 reads=[LG, T8], writes=[MK])
            P.op("dve", lambda v, LG=LG, T8=T8, r=r: v.tensor_scalar(out=LG[:r, :], in0=LG[:r, :], scalar1=T8[:r, 0:1], scalar2=None, op0=ALU.subtract),
                 reads=[LG, T8], writes=[LG])
            P.op("act", lambda a, LG=LG, r=r: a.activation(out=LG[:r, :], in_=LG[:r, :], func=AF.Exp), reads=[LG], writes=[LG])
            P.op("dve", lambda v, LG=LG, MK=MK, r=r: v.tensor_tensor(out=LG[:r, :], in0=LG[:r, :], in1=MK[:r, :], op=ALU.mult), reads=[LG, MK], writes=[LG])
            P.op("dve", lambda v, LG=LG, T8=T8, r=r: v.tensor_reduce(out=T8[:r, 4:5], in_=LG[:r, :], op=ALU.add, axis=AX.X), reads=[LG], writes=[T8])
            P.op("dve", lambda v, T8=T8, r=r: v.reciprocal(out=T8[:r, 4:5], in_=T8[:r, 4:5]), reads=[T8], writes=[T8])
            P.op("dve", lambda v, LG=LG, T8=T8, GT=GT, r=r: v.tensor_scalar(out=GT[:r, :], in0=LG[:r, :], scalar1=T8[:r, 4:5], scalar2=None, op0=ALU.mult),
                 reads=[LG, T8], writes=[GT])
            P.dma("pool", gates_out[rows, :], GT[:r, :], reads=[GT], is_output=True)
        P.finish()
    return nc


NE = 32
HALVES = [(0, 8), (8, 17)]


def build_phase_d(final=False, n_experts=NE):
    nc = _new_nc()
    x1 = _din(nc, "x1", [TOK_PC, D])
    hmT = _din(nc, "hmT", [128, 8, TOK_PC], BF16)
    gates = _din(nc, "gates", [TOK_PC, 32])
    modl = _din(nc, "modl", [6 * D])
    modc = _din(nc, "modc", [6 * D])
    wgate = _din(nc, "wgate", [NE, D, D])
    wup = _din(nc, "wup", [NE, D, D])
    wdown = _din(nc, "wdown", [NE, D, D])
    bgate = _din(nc, "bgate", [128, NE, 8])
    bup = _din(nc, "bup", [128, NE, 8])
    bdown = _din(nc, "bdown", [NE, D])
    identf_d = _din(nc, "identf", [128, 128])
    fnw = _din(nc, "fnw", [D])
    x2_out = _dout(nc, "x2", [TOK_PC, D])
    with ExitStack() as st:
        P = Prog(nc, st)
        idf = P.sb([128, 128], F32)
        P.dma("sp", idf[:, :], identf_d, writes=[idf])
        g2 = [P.sb([128, D], F32) for _ in range(2)]
        _load_bcast(P, "sp", g2[0], modl[5 * D:6 * D], D)
        _load_bcast(P, "sp", g2[1], modc[5 * D:6 * D], D)
        if final:
            fw = P.sb([128, D], F32)
            _load_bcast(P, "sp", fw, fnw, D)
        bgt = P.sb([128, NE, 8], F32)
        but = P.sb([128, NE, 8], F32)
        P.dma("sp", bgt[:, :, :], bgate, writes=[bgt])
        P.dma("sp", but[:, :, :], bup, writes=[but])
        bdt = P.sb([32, D], F32)
        P.dma("sp", bdt[:, :], bdown, writes=[bdt])
        NRING = 14
        ring = [P.sb([128, 2, D], BF16) for _ in range(NRING)]
        stg = [P.sb([128, 2, D], F32) for _ in range(2)]
        hm = P.sb([128, 8, 1056], BF16)
        acc = P.sb([128, 9, D], F32)
        gts = P.sb([128, 9, 32], F32)
        gT = P.sb([32, 128], F32)
        actT = [P.sb([128, 8, 512], BF16) for _ in range(2)]
        gp = [P.sb([128, 512], F32) for _ in range(2)]
        sg = [P.sb([128, 512], BF16) for _ in range(2)]
        up = [P.sb([128, 512], F32) for _ in range(2)]
        xt = [P.sb([128, D], F32) for _ in range(2)]
        junk = P.sb([128, D], F32)
        s8 = [P.sb([128, 4], F32) for _ in range(2)]
        pg = [P.ps([128, 512]) for _ in range(2)]
        pu = [P.ps([128, 512]) for _ in range(2)]
        po = [P.ps([128, 512]) for _ in range(4)]
        nring = 0
        nstg = 0
        nact = 0
        ngu = 0
        npo = 0

        def load_w(src_e):
            nonlocal nring, nstg
            pieces = []
            for j in range(4):
                S = stg[nstg % 2]
                nstg += 1
                R = ring[nring % NRING]
                nring += 1
                P.dma("sp", S[:, :, :], src_e[j * 256:(j + 1) * 256, :].rearrange("(k p) n -> p k n", p=128), writes=[S])
                P.op("pool", lambda g, S=S, R=R: g.tensor_copy(out=R[:, :, :], in_=S[:, :, :]), reads=[S], writes=[R])
                pieces += [(R, 0), (R, 1)]
            return pieces

        for (ta, tb) in HALVES:
            tok0 = ta * 128
            ntok = sum(128 if t < 16 else CTX_PC for t in range(ta, tb))
            nt = tb - ta
            P.dma("sp", hm[:, :, 0:ntok], hmT[:, :, tok0:tok0 + ntok], writes=[hm])
            for t in range(ta, tb):
                r = 128 if t < 16 else CTX_PC
                P.dma("sp", gts[:r, t - ta, :], gates[t * 128:t * 128 + r, :], writes=[(gts, t - ta)])
            for t in range(ta, tb):
                r = 128 if t < 16 else CTX_PC
                lt = t - ta
                pp = po[npo % 4]
                npo += 1
                P.op("pe", lambda pe, pp=pp, lt=lt, r=r: pe.transpose(pp[0:32, 0:r], gts[:r, lt, :], idf[:r, :r]), reads=[(gts, lt), idf], writes=[pp])
                P.op("act", lambda a, pp=pp, r=r: a.copy(out=gT[:, :r], in_=pp[0:32, 0:r]), reads=[pp], writes=[gT])
                for ct in range(2):
                    pq = po[npo % 4]
                    npo += 1
                    P.op("pe", lambda pe, pq=pq, ct=ct, r=r: pe.matmul(pq[:r, :], lhsT=gT[:, :r], rhs=bdt[:, ct * 512:(ct + 1) * 512], start=True, stop=True),
                         reads=[gT, bdt], writes=[pq])
                    P.op("act", lambda a, pq=pq, ct=ct, lt=lt, r=r: a.copy(out=acc[:r, lt, ct * 512:(ct + 1) * 512], in_=pq[:r, :]),
                         reads=[pq], writes=[(acc, (lt, ct))])
            ttiles = []
            o = 0
            while o < ntok:
                w = min(512, ntok - o)
                ttiles.append((o, w))
                o += w
            for e in range(n_experts):
                Wg = load_w(wgate[e])
                Wu = load_w(wup[e])
                Wd = load_w(wdown[e])
                for (o, w) in ttiles:
                    A = actT[nact % 2]
                    nact += 1
                    for c in range(8):
                        PG, PU = pg[ngu % 2], pu[ngu % 2]
                        GP, SG, UP = gp[ngu % 2], sg[ngu % 2], up[ngu % 2]
                        ngu += 1
                        for k in range(8):
                            R, kk = Wg[k]
                            P.op("pe", lambda pe, PG=PG, R=R, kk=kk, k=k, c=c, o=o, w=w: pe.matmul(
                                PG[:, :w], lhsT=R[:, kk, c * 128:(c + 1) * 128], rhs=hm[:, k, o:o + w], start=(k == 0), stop=(k == 7)),
                                reads=[R, hm], writes=[PG])
                        for k in range(8):
                            R, kk = Wu[k]
                            P.op("pe", lambda pe, PU=PU, R=R, kk=kk, k=k, c=c, o=o, w=w: pe.matmul(
                                PU[:, :w], lhsT=R[:, kk, c * 128:(c + 1) * 128], rhs=hm[:, k, o:o + w], start=(k == 0), stop=(k == 7)),
                                reads=[R, hm], writes=[PU])
                        P.op("dve", lambda v, PG=PG, GP=GP, e=e, c=c, w=w: v.tensor_scalar(
                            out=GP[:, :w], in0=PG[:, :w], scalar1=bgt[:, e, c:c + 1], scalar2=7.0, op0=ALU.add, op1=ALU.min),
                            reads=[PG, bgt], writes=[GP])
                        P.op("act", lambda a, GP=GP, SG=SG, w=w: a.activation(out=SG[:, :w], in_=GP[:, :w], func=AF.Sigmoid, scale=1.702),
                             reads=[GP], writes=[SG])
                        P.op("dve", lambda v, PU=PU, UP=UP, e=e, c=c, w=w: v.tensor_scalar(
                            out=UP[:, :w], in0=PU[:, :w], scalar1=but[:, e, c:c + 1], scalar2=7.0, op0=ALU.add, op1=ALU.min),
                            reads=[PU, but], writes=[UP])
                        P.op("dve", lambda v, UP=UP, w=w: v.tensor_scalar(out=UP[:, :w], in0=UP[:, :w], scalar1=-7.0, scalar2=1.0, op0=ALU.max, op1=ALU.add),
                             reads=[UP], writes=[UP])
                        P.op("pool", lambda g, GP=GP, SG=SG, w=w: g.tensor_tensor(out=GP[:, :w], in0=GP[:, :w], in1=SG[:, :w], op=ALU.mult),
                             reads=[GP, SG], writes=[GP])
                        P.op("pool", lambda g, GP=GP, UP=UP, A=A, c=c, w=w: g.tensor_tensor(out=A[:, c, :w], in0=GP[:, :w], in1=UP[:, :w], op=ALU.mult),
                             reads=[GP, UP], writes=[(A, c)])
                    nsub = (w + 127) // 128
                    for j in range(nsub):
                        ww = min(128, w - j * 128)
                        lt = (o + j * 128) // 128
                        for ct in range(2):
                            PO = po[npo % 4]
                            npo += 1
                            for c in range(8):
                                R, kk = Wd[c]
                                P.op("pe", lambda pe, PO=PO, A=A, R=R, kk=kk, c=c, j=j, ww=ww, ct=ct: pe.matmul(
                                    PO[:ww, :], lhsT=A[:, c, j * 128:j * 128 + ww], rhs=R[:, kk, ct * 512:(ct + 1) * 512], start=(c == 0), stop=(c == 7)),
                                    reads=[(A, c), R], writes=[PO])
                            P.op("dve", lambda v, PO=PO, lt=lt, ct=ct, ww=ww, e=e: v.scalar_tensor_tensor(
                                out=acc[:ww, lt, ct * 512:(ct + 1) * 512], in0=PO[:ww, :], scalar=gts[:ww, lt, e:e + 1],
                                in1=acc[:ww, lt, ct * 512:(ct + 1) * 512], op0=ALU.mult, op1=ALU.add),
                                reads=[PO, (gts, lt), (acc, (lt, ct))], writes=[(acc, (lt, ct))])
            for t in range(ta, tb):
                r = 128 if t < 16 else CTX_PC
                ic = 0 if t < 16 else 1
                lt = t - ta
                X = xt[t % 2]
                S8 = s8[t % 2]
                P.dma("sp", X[:r, :], x1[t * 128:t * 128 + r, :], writes=[X])
                P.op("dve", lambda v, lt=lt, r=r, ic=ic: v.tensor_tensor(out=acc[:r, lt, :], in0=acc[:r, lt, :], in1=g2[ic][:r, :], op=ALU.mult),
                     reads=[(acc, (lt, 0)), (acc, (lt, 1)), g2[ic]], writes=[(acc, (lt, 0)), (acc, (lt, 1))])
                P.op("pool", lambda g, X=X, lt=lt, r=r: g.tensor_tensor(out=X[:r, :], in0=X[:r, :], in1=acc[:r, lt, :], op=ALU.add),
                     reads=[X, (acc, (lt, 0)), (acc, (lt, 1))], writes=[X])
                if final:
                    P.op("act", lambda a, X=X, S8=S8, r=r: a.activation(out=junk[:r, :], in_=X[:r, :], func=AF.Square, accum_out=S8[:r, 0:1]),
                         reads=[X], writes=[junk, S8])
                    _rms_rstd(P, S8, Tile_col(S8, 1), r, D)
                    P.op("dve", lambda v, X=X, S8=S8, r=r: v.scalar_tensor_tensor(out=X[:r, :], in0=X[:r, :], scalar=S8[:r, 1:2], in1=fw[:r, :],
                                                                                 op0=ALU.mult, op1=ALU.mult), reads=[X, S8, fw], writes=[X])
                P.dma("pool", x2_out[t * 128:t * 128 + r, :], X[:r, :], reads=[X], is_output=True)
        P.finish()
    return nc


class Tile_col(Tile):
    def __init__(self, t, c):
        self.t, self.c = t, c
        self.h, self.psum, self.bufs = t.h, t.psum, t.bufs

    def __getitem__(self, idx):
        rows, cols = idx
        return self.t.h[rows, self.c + (cols.start or 0):self.c + cols.stop]


def moe_inputs(p):
    wgu = p["moe_w_gate_up"]
    bgu = p["moe_b_gate_up"]
    return {"wgate": np.ascontiguousarray(wgu[:, :, 0::2]), "wup": np.ascontiguousarray(wgu[:, :, 1::2]),
            "wdown": p["moe_w_down"],
            "bgate": np.ascontiguousarray(bgu[:, 0::2].reshape(NE, 8, 128).transpose(2, 0, 1)),
            "bup": np.ascontiguousarray(bgu[:, 1::2].reshape(NE, 8, 128).transpose(2, 0, 1)),
            "bdown": p["moe_b_down"]}


EPC = NE // NCORES
NTT = (NTOK + 511) // 512


def build_phase_d2(n_experts=EPC):
    nc = _new_nc()
    hmT = _din(nc, "hmT", [128, 8, NTOK], BF16)
    gates = _din(nc, "gates", [128, NKC, EPC])
    wgate = _din(nc, "wgate", [EPC, D, D])
    wup = _din(nc, "wup", [EPC, D, D])
    wdown = _din(nc, "wdown", [EPC, D, D])
    bgate = _din(nc, "bgate", [128, EPC, 8])
    bup = _din(nc, "bup", [128, EPC, 8])
    part = _dout(nc, "part", [NTOK, D])
    with ExitStack() as st:
        P = Prog(nc, st)
        bgt = P.sb([128, EPC, 8], F32)
        but = P.sb([128, EPC, 8], F32)
        P.dma("sp", bgt[:, :, :], bgate, writes=[bgt])
        P.dma("sp", but[:, :, :], bup, writes=[but])
        gts = P.sb([128, NKC, EPC], F32)
        P.dma("sp", gts[:, :, :], gates, writes=[gts])
        NRING = 20
        ring = [P.sb([128, 2, D], BF16) for _ in range(NRING)]
        stg = [P.sb([128, 2, D], F32) for _ in range(3)]
        hm = [P.sb([128, 8, 512], BF16) for _ in range(2)]
        actT = [P.sb([128, 8, 512], BF16) for _ in range(2)]
        gp = [P.sb([128, 512], F32) for _ in range(2)]
        sg = [P.sb([128, 512], BF16) for _ in range(2)]
        up = [P.sb([128, 512], F32) for _ in range(2)]
        ot = [P.sb([128, D], F32) for _ in range(3)]
        pv = [P.sb([128, D], F32) for _ in range(3)]
        pg = [P.ps([128, 512]) for _ in range(2)]
        pu = [P.ps([128, 512]) for _ in range(2)]
        po = [P.ps([128, 512]) for _ in range(4)]
        dblk = [Tile(None) for _ in range(NKC)]
        cnt = {"ring": 0, "stg": 0, "gu": 0, "po": 0, "ot": 0}

        def load_w(src_e):
            pieces = []
            for j in range(4):
                S = stg[cnt["stg"] % 3]
                cnt["stg"] += 1
                R = ring[cnt["ring"] % NRING]
                cnt["ring"] += 1
                P.dma("sp", S[:, :, :], src_e[j * 256:(j + 1) * 256, :].rearrange("(k p) n -> p k n", p=128), writes=[S])
                P.op("pool", lambda g, S=S, R=R: g.tensor_copy(out=R[:, :, :], in_=S[:, :, :]), reads=[S], writes=[R])
                pieces += [(R, 0), (R, 1)]
            return pieces

        for e in range(n_experts):
            Wg = load_w(wgate[e])
            Wu = load_w(wup[e])
            Wd = load_w(wdown[e])
            for ti in range(NTT):
                o = ti * 512
                w = min(512, NTOK - o)
                H = hm[ti % 2]
                A = actT[ti % 2]
                P.dma("sp", H[:, :, :w], hmT[:, :, o:o + w], writes=[H])
                for c in range(8):
                    b2 = cnt["gu"] % 2
                    cnt["gu"] += 1
                    PG, PU, GP, SG, UP = pg[b2], pu[b2], gp[b2], sg[b2], up[b2]
                    for k in range(8):
                        R, kk = Wg[k]
                        P.op("pe", lambda pe, PG=PG, R=R, kk=kk, k=k, c=c, H=H, w=w: pe.matmul(
                            PG[:, :w], lhsT=R[:, kk, c * 128:(c + 1) * 128], rhs=H[:, k, :w], start=(k == 0), stop=(k == 7)),
                            reads=[R, H], writes=[PG])
                    for k in range(8):
                        R, kk = Wu[k]
                        P.op("pe", lambda pe, PU=PU, R=R, kk=kk, k=k, c=c, H=H, w=w: pe.matmul(
                            PU[:, :w], lhsT=R[:, kk, c * 128:(c + 1) * 128], rhs=H[:, k, :w], start=(k == 0), stop=(k == 7)),
                            reads=[R, H], writes=[PU])
                    P.op("dve", lambda v, PG=PG, GP=GP, e=e, c=c, w=w: v.tensor_scalar(
                        out=GP[:, :w], in0=PG[:, :w], scalar1=bgt[:, e, c:c + 1], scalar2=7.0, op0=ALU.add, op1=ALU.min),
                        reads=[PG, bgt], writes=[GP])
                    P.op("act", lambda a, GP=GP, SG=SG, w=w: a.activation(out=SG[:, :w], in_=GP[:, :w], func=AF.Sigmoid, scale=1.702),
                         reads=[GP], writes=[SG])
                    P.op("dve", lambda v, PU=PU, UP=UP, e=e, c=c, w=w: v.tensor_scalar(
                        out=UP[:, :w], in0=PU[:, :w], scalar1=but[:, e, c:c + 1], scalar2=7.0, op0=ALU.add, op1=ALU.min),
                        reads=[PU, but], writes=[UP])
                    P.op("dve", lambda v, UP=UP, w=w: v.tensor_scalar(out=UP[:, :w], in0=UP[:, :w], scalar1=-7.0, scalar2=1.0, op0=ALU.max, op1=ALU.add),
                         reads=[UP], writes=[UP])
                    P.op("pool", lambda g, GP=GP, SG=SG, w=w: g.tensor_tensor(out=GP[:, :w], in0=GP[:, :w], in1=SG[:, :w], op=ALU.mult),
                         reads=[GP, SG], writes=[GP])
                    P.op("pool", lambda g, GP=GP, UP=UP, A=A, c=c, w=w: g.tensor_tensor(out=A[:, c, :w], in0=GP[:, :w], in1=UP[:, :w], op=ALU.mult),
                         reads=[GP, UP], writes=[(A, c)])
                for j in range(w // 128):
                    blk = o // 128 + j
                    OT = ot[cnt["ot"] % 3]
                    PV = pv[cnt["ot"] % 3]
                    cnt["ot"] += 1
                    if e > 0:
                        P.dma("sp", PV[:, :], part[blk * 128:(blk + 1) * 128, :], reads=[dblk[blk]], writes=[PV])
                    for ct in range(2):
                        PO = po[cnt["po"] % 4]
                        cnt["po"] += 1
                        for c in range(8):
                            R, kk = Wd[c]
                            P.op("pe", lambda pe, PO=PO, A=A, R=R, kk=kk, c=c, j=j, ct=ct: pe.matmul(
                                PO[:, :], lhsT=A[:, c, j * 128:(j + 1) * 128], rhs=R[:, kk, ct * 512:(ct + 1) * 512], start=(c == 0), stop=(c == 7)),
                                reads=[(A, c), R], writes=[PO])
                        cs = slice(ct * 512, (ct + 1) * 512)
                        if e == 0:
                            P.op("dve", lambda v, PO=PO, OT=OT, cs=cs, blk=blk, e=e: v.tensor_scalar(
                                out=OT[:, cs], in0=PO[:, :], scalar1=gts[:, blk, e:e + 1], scalar2=None, op0=ALU.mult),
                                reads=[PO, gts], writes=[(OT, ct)])
                        else:
                            P.op("dve", lambda v, PO=PO, OT=OT, PV=PV, cs=cs, blk=blk, e=e: v.scalar_tensor_tensor(
                                out=OT[:, cs], in0=PO[:, :], scalar=gts[:, blk, e:e + 1], in1=PV[:, cs], op0=ALU.mult, op1=ALU.add),
                                reads=[PO, gts, PV], writes=[(OT, ct)])
                    P.dma("pool", part[blk * 128:(blk + 1) * 128, :], OT[:, :], reads=[OT], writes=[dblk[blk]], is_output=True)
        P.finish()
    return nc


def build_phase_e(final=False):
    nc = _new_nc()
    x1 = _din(nc, "x1", [TOK_PC, D])
    parts = _din(nc, "parts", [NCORES, TOK_PC, D])
    gates = _din(nc, "gates", [TOK_PC, 32])
    modl = _din(nc, "modl", [6 * D])
    modc = _din(nc, "modc", [6 * D])
    bdown = _din(nc, "bdown", [NE, D])
    identf_d = _din(nc, "identf", [128, 128])
    fnw = _din(nc, "fnw", [D])
    x2_out = _dout(nc, "x2", [TOK_PC, D])
    with ExitStack() as st:
        P = Prog(nc, st)
        idf = P.sb([128, 128], F32)
        P.dma("sp", idf[:, :], identf_d, writes=[idf])
        g2 = [P.sb([128, D], F32) for _ in range(2)]
        _load_bcast(P, "sp", g2[0], modl[5 * D:6 * D], D)
        _load_bcast(P, "sp", g2[1], modc[5 * D:6 * D], D)
        fw = P.sb([128, D], F32)
        _load_bcast(P, "sp", fw, fnw, D)
        bdt = P.sb([32, D], F32)
        P.dma("sp", bdt[:, :], bdown, writes=[bdt])
        pt = [P.sb([128, NCORES, D], F32) for _ in range(2)]
        xt = [P.sb([128, D], F32) for _ in range(2)]
        gt = [P.sb([128, 32], F32) for _ in range(2)]
        gT = [P.sb([32, 128], F32) for _ in range(2)]
        acc = [P.sb([128, D], F32) for _ in range(2)]
        acc2 = [P.sb([128, D], F32) for _ in range(2)]
        junk = P.sb([128, D], F32)
        s8 = [P.sb([128, 4], F32) for _ in range(2)]
        pT = P.ps([128, 512])
        pb = [P.ps([128, 512]) for _ in range(2)]
        for t in range(NT):
            r = 128 if t < 16 else CTX_PC
            ic = 0 if t < 16 else 1
            b = t % 2
            rows = slice(t * 128, t * 128 + r)
            PT, X, G, GT, A, A2, S8 = pt[b], xt[b], gt[b], gT[b], acc[b], acc2[b], s8[b]
            for c in range(NCORES):
                P.dma("sp", PT[:r, c, :], parts[c, rows, :], writes=[(PT, c)])
            P.dma("sp", X[:r, :], x1[rows, :], writes=[X])
            P.dma("sp", G[:r, :], gates[rows, :], writes=[G])
            P.op("pe", lambda pe, G=G, r=r: pe.transpose(pT[0:32, 0:r], G[:r, :], idf[:r, :r]), reads=[G, idf], writes=[pT])
            P.op("act", lambda a, GT=GT, r=r: a.copy(out=GT[:, :r], in_=pT[0:32, 0:r]), reads=[pT], writes=[GT])
            for ct in range(2):
                P.op("pe", lambda pe, ct=ct, GT=GT, r=r: pe.matmul(pb[ct][:r, :], lhsT=GT[:, :r], rhs=bdt[:, ct * 512:(ct + 1) * 512], start=True, stop=True),
                     reads=[GT, bdt], writes=[pb[ct]])
            P.op("dve", lambda v, PT=PT, A=A, r=r: v.tensor_tensor(out=A[:r, :], in0=PT[:r, 0, :], in1=PT[:r, 1, :], op=ALU.add),
                 reads=[(PT, 0), (PT, 1)], writes=[A])
            P.op("pool", lambda g, PT=PT, A2=A2, r=r: g.tensor_tensor(out=A2[:r, :], in0=PT[:r, 2, :], in1=PT[:r, 3, :], op=ALU.add),
                 reads=[(PT, 2), (PT, 3)], writes=[A2])
            P.op("dve", lambda v, PT=PT, A=A, r=r: v.tensor_tensor(out=A[:r, :], in0=A[:r, :], in1=PT[:r, 4, :], op=ALU.add),
                 reads=[(PT, 4), A], writes=[A])
            P.op("pool", lambda g, PT=PT, A2=A2, r=r: g.tensor_tensor(out=A2[:r, :], in0=A2[:r, :], in1=PT[:r, 5, :], op=ALU.add),
                 reads=[(PT, 5), A2], writes=[A2])
            P.op("dve", lambda v, PT=PT, A=A, r=r: v.tensor_tensor(out=A[:r, :], in0=A[:r, :], in1=PT[:r, 6, :], op=ALU.add),
                 reads=[(PT, 6), A], writes=[A])
            P.op("pool", lambda g, PT=PT, A2=A2, r=r: g.tensor_tensor(out=A2[:r, :], in0=A2[:r, :], in1=PT[:r, 7, :], op=ALU.add),
                 reads=[(PT, 7), A2], writes=[A2])
            P.op("dve", lambda v, A=A, A2=A2, r=r: v.tensor_tensor(out=A[:r, :], in0=A[:r, :], in1=A2[:r, :], op=ALU.add), reads=[A, A2], writes=[A])
            for ct in range(2):
                cs = slice(ct * 512, (ct + 1) * 512)
                P.op("dve", lambda v, A=A, ct=ct, cs=cs, r=r: v.tensor_tensor(out=A[:r, cs], in0=A[:r, cs], in1=pb[ct][:r, :], op=ALU.add),
                     reads=[A, pb[ct]], writes=[A])
            P.op("pool", lambda g, A=A, r=r, ic=ic: g.tensor_tensor(out=A[:r, :], in0=A[:r, :], in1=g2[ic][:r, :], op=ALU.mult), reads=[A, g2[ic]], writes=[A])
            P.op("dve", lambda v, A=A, X=X, r=r: v.tensor_tensor(out=X[:r, :], in0=X[:r, :], in1=A[:r, :], op=ALU.add), reads=[A, X], writes=[X])
            if final:
                P.op("act", lambda a, X=X, S8=S8, r=r: a.activation(out=junk[:r, :], in_=X[:r, :], func=AF.Square, accum_out=S8[:r, 0:1]),
                     reads=[X], writes=[junk, S8])
                _rms_rstd(P, S8, Tile_col(S8, 1), r, D)
                P.op("dve", lambda v, X=X, S8=S8, r=r: v.scalar_tensor_tensor(out=X[:r, :], in0=X[:r, :], scalar=S8[:r, 1:2], in1=fw[:r, :],
                                                                             op0=ALU.mult, op1=ALU.mult), reads=[X, S8, fw], writes=[X])
            P.dma("pool", x2_out[rows, :], X[:r, :], reads=[X], is_output=True)
        P.finish()
    return nc


_PROGS = {}


def _prog(name, builder):
    if name not in _PROGS:
        _PROGS[name] = builder()
    return _PROGS[name]


def _run(nc, maps):
    return run_bass_kernel_spmd(nc, maps, core_ids=list(range(NCORES))).results


def kernel(**inp):
    f32 = np.float32
    inp = {k: np.asarray(v) for k, v in inp.items()}
    identb = np.eye(128).astype(ml_dtypes.bfloat16)
    identf = np.eye(128, dtype=f32)
    ropd, ropg = rope_tables(32), rope_tables(64)
    c, cc = inp["c"], inp["c_ctx"]
    cT = np.ascontiguousarray(np.stack([c[0], cc], axis=-1).reshape(8, 128, 2).transpose(1, 0, 2)).astype(f32)
    maps = []
    for i in range(NCORES):
        maps.append({"cT": cT, "wada": np.ascontiguousarray(inp["w_ada"][:, :, 768 * i:768 * (i + 1)]),
                     "bada": np.ascontiguousarray(np.broadcast_to(inp["b_ada"][:, None, 768 * i:768 * (i + 1)], (DEPTH, 2, 768)))})
    res = _run(_prog("m", build_phase_m), maps)
    mod = np.concatenate([r["mod"] for r in res], axis=-1)

    xg = np.concatenate([inp["ctx"][0], inp["x"][0]], axis=0).astype(f32)
    x_cores = [_core_rows(xg, i) for i in range(NCORES)]
    for l in range(DEPTH):
        p = {k: v[l] for k, v in inp.items() if v.ndim >= 1 and v.shape[0] == DEPTH and k not in ("x", "c", "ctx", "c_ctx", "final_norm_w")}
        qkw = np.concatenate([np.tile(p["gqa_q_norm_w"], 4), np.tile(p["gqa_k_norm_w"], 2)]).astype(f32)
        maps = []
        for i in range(NCORES):
            maps.append({"x": x_cores[i], "modl": mod[l, 0], "modc": mod[l, 1], "n1w": p["norm1_w"], "w_in": p["w_in"],
                         "ident": identb, "ropd": ropd[LAT_PC * i:LAT_PC * (i + 1)], "ropg": ropg[LAT_PC * i:LAT_PC * (i + 1)], "qkw": qkw})
        ra = _run(_prog("a", build_phase_a), maps)
        u_mid = _gather_tok([r["u"] for r in ra])
        u_glob = np.zeros((NTOK, D_IN), f32)
        u_glob[:, C_SZ:C_GQ] = u_mid
        lamv = np.stack([p["diff_lq1"], p["diff_lk1"], p["diff_lq2"], p["diff_lk2"]]).astype(f32)
        laminit = np.array([0.8 - 0.6 * math.exp(-0.3 * l)], f32)
        maps = attn_inputs([r["qkd"] for r in ra], [r["qkg"] for r in ra], [r["vv"] for r in ra], lamv, laminit, p["diff_subln_w"])
        rb = _run(_prog("b_attn", build_phase_b_attn), maps)
        maps = [ssd_inputs(u_glob, p, i) for i in range(NCORES)]
        rs = _run(_prog("b_ssd", build_phase_b_ssd), maps)

        def unrev(y):
            return np.concatenate([y[:CTX][::-1], y[CTX:][::-1]], axis=0)
        ysf = np.concatenate([rs[h]["s_y"] for h in range(4)], axis=1)
        ysb = np.concatenate([unrev(rs[4 + h]["s_y"]) for h in range(4)], axis=1)
        maps = [s5_inputs(u_glob, p, i) for i in range(NCORES)]
        rc5 = _run(_prog("b_s5", build_phase_b_s5), maps)
        s5yT = np.concatenate([r["c_yT"] for r in rc5], axis=0)
        s5y = np.ascontiguousarray(s5yT.T)
        zz = u_glob[:, C_SZ:C_SZ + 256]
        maps = []
        for i in range(NCORES):
            maps.append({"x": x_cores[i], "ad": rb[i]["od"], "ag": rb[i]["og"], "ysf": _core_rows(ysf, i), "ysb": _core_rows(ysb, i),
                         "zz": _core_rows(zz, i), "s5T": np.ascontiguousarray(_core_rows(s5y, i).T), "modl": mod[l, 0], "modc": mod[l, 1],
                         "n2w": p["norm2_w"], "snw": p["ssd_norm_w"], "wglu": p["s5_w_glu"],
                         "bglu": np.ascontiguousarray(p["s5_b_glu"].reshape(2, 128).T), "wout": p["w_out"], "wr": p["moe_w_router"],
                         "br": p["moe_b_router"], "identb": identb, "identf": identf})
        rcc = _run(_prog("c", build_phase_c), maps)
        mi = moe_inputs(p)
        hm_all = np.concatenate([r["hmT"] for r in rcc], axis=2)
        g_all = np.concatenate([r["gates"] for r in rcc], axis=0)
        maps = []
        for i in range(NCORES):
            es = slice(EPC * i, EPC * (i + 1))
            maps.append({"hmT": hm_all, "gates": np.ascontiguousarray(g_all[:, es].reshape(NKC, 128, EPC).transpose(1, 0, 2)),
                         "wgate": mi["wgate"][es], "wup": mi["wup"][es], "wdown": mi["wdown"][es],
                         "bgate": np.ascontiguousarray(mi["bgate"][:, es]), "bup": np.ascontiguousarray(mi["bup"][:, es])})
        rd = _run(_prog("d2", build_phase_d2), maps)
        final = (l == DEPTH - 1)
        maps = []
        for i in range(NCORES):
            parts = np.stack([rd[c_]["part"][i * TOK_PC:(i + 1) * TOK_PC] for c_ in range(NCORES)])
            maps.append({"x1": rcc[i]["x1"], "parts": parts, "gates": rcc[i]["gates"], "modl": mod[l, 0], "modc": mod[l, 1],
                         "bdown": mi["bdown"], "identf": identf, "fnw": inp["final_norm_w"]})
        re_ = _run(_prog("e_final" if final else "e", (lambda: build_phase_e(True)) if final else (lambda: build_phase_e(False))), maps)
        x_cores = [r["x2"] for r in re_]
    out = np.concatenate([xc_[:LAT_PC] for xc_ in x_cores], axis=0)
    return out.reshape(1, SEQ, D).astype(f32)
```
